# Optimizing a Trainium2 kernel written in Bass

```python
import jax, jax.numpy as jnp
from jax import lax
import numpy as np

D_MODEL = 1024
BATCH = 16
SEQ = 2048
DEPTH = 1
DEC_BATCH = 32
DEC_SEQ = 64
PAST_LEN = 1024

CHUNK = 64
RET_HEADS = 4
RET_DK = 128
RET_DV = 256
RET_ROPE_BASE = 10000.0
DSA_HEADS = 8
DSA_KV_HEADS = 2
DSA_HEAD_DIM = 64
IDX_HEADS = 8
IDX_DIM = 64
DSA_TOPK = 256
ROPE_THETA = 500000.0
N_EXPERTS = 32
MOE_TOP_K = 4
D_FF = 1024
SWIGLU_ALPHA = 1.702
SWIGLU_LIMIT = 7.0
MOE_BLOCK = 128
LN_EPS = 1e-5
GN_EPS = 1e-6
DEEPNORM_ALPHA = (2.0 * DEPTH) ** 0.25
DEEPNORM_BETA = (8.0 * DEPTH) ** -0.25
PROJ_WIDTHS = (RET_HEADS * RET_DK, RET_HEADS * RET_DK, RET_HEADS * RET_DV, RET_HEADS * RET_DV,
               DSA_HEADS * DSA_HEAD_DIM, DSA_KV_HEADS * DSA_HEAD_DIM, DSA_KV_HEADS * DSA_HEAD_DIM,
               IDX_HEADS * IDX_DIM, IDX_DIM, IDX_HEADS, D_MODEL, D_MODEL)
PROJ_TOTAL = sum(PROJ_WIDTHS)

kernel_name = 'streaming_retention_dsa_moe_step'


def layer_norm(x, g, b):
    xf = x.astype(jnp.float32)
    mu = jnp.mean(xf, -1, keepdims=True)
    var = jnp.mean(jnp.square(xf - mu), -1, keepdims=True)
    return ((xf - mu) * lax.rsqrt(var + LN_EPS) * g + b).astype(x.dtype)


def apply_rotary(x, pos, inv_freq):
    half = inv_freq.shape[0]
    ang = pos.astype(jnp.float32)[:, None] * inv_freq[None, :]
    cos = jnp.cos(ang)[None, :, None, :]
    sin = jnp.sin(ang)[None, :, None, :]
    xf = x.astype(jnp.float32)
    x1, x2, rest = xf[..., :half], xf[..., half:2 * half], xf[..., 2 * half:]
    out = jnp.concatenate([x1 * cos - x2 * sin, x1 * sin + x2 * cos, rest], -1)
    return out.astype(x.dtype)


def retention_inv_freq():
    return RET_ROPE_BASE ** (-jnp.linspace(0.0, 1.0, RET_DK // 2, dtype=jnp.float32))


def partial_inv_freq(head_dim):
    n_rot = head_dim // 4
    return ROPE_THETA ** (-jnp.arange(0, n_rot, 2, dtype=jnp.float32) / n_rot)


def retention_gammas():
    return 1.0 - 2.0 ** (-5.0 - jnp.arange(RET_HEADS, dtype=jnp.float32))


def project_and_split(x, w_in, pos):
    b, t, _ = x.shape
    z = jnp.einsum('btd,de->bte', x, w_in)
    cuts = np.cumsum(PROJ_WIDTHS)[:-1].tolist()
    rq, rk, rv, rg, aq, ak, av, iq, ik, iw, gr, ga = jnp.split(z, cuts, axis=-1)
    ret_f = retention_inv_freq()
    rq = apply_rotary(rq.reshape(b, t, RET_HEADS, RET_DK), pos, ret_f)
    rk = apply_rotary(rk.reshape(b, t, RET_HEADS, RET_DK), pos, ret_f) * (RET_DK ** -0.5)
    rv = rv.reshape(b, t, RET_HEADS, RET_DV)
    att_f = partial_inv_freq(DSA_HEAD_DIM)
    aq = apply_rotary(aq.reshape(b, t, DSA_HEADS, DSA_HEAD_DIM), pos, att_f)
    ak = apply_rotary(ak.reshape(b, t, DSA_KV_HEADS, DSA_HEAD_DIM), pos, att_f)
    av = av.reshape(b, t, DSA_KV_HEADS, DSA_HEAD_DIM)
    idx_f = partial_inv_freq(IDX_DIM)
    iq = apply_rotary(iq.reshape(b, t, IDX_HEADS, IDX_DIM), pos, idx_f)
    ik = apply_rotary(ik.reshape(b, t, 1, IDX_DIM), pos, idx_f)[:, :, 0]
    iw = iw * (IDX_HEADS ** -0.5)
    return (rq, rk, rv, rg, aq, ak, av, iq, ik, iw, gr, ga)


def retention_chunk(q, k, v, s0):
    t = q.shape[1]
    log_g = jnp.log(retention_gammas())
    pos = jnp.arange(t, dtype=jnp.float32)
    diff = pos[:, None] - pos[None, :]
    decay = jnp.where(diff >= 0, jnp.exp(jnp.maximum(diff, 0.0)[None] * log_g[:, None, None]), 0.0)
    scores = jnp.einsum('bthd,bshd->bhts', q, k).astype(jnp.float32) * decay[None]
    o = jnp.einsum('bhts,bshe->bthe', scores, v.astype(jnp.float32))
    xi = jnp.exp((pos + 1.0)[:, None] * log_g[None, :])
    o = o + jnp.einsum('bthd,bhde->bthe', q.astype(jnp.float32), s0) * xi[None, :, :, None]
    zeta = jnp.exp((t - 1.0 - pos)[:, None] * log_g[None, :])
    s_new = (jnp.exp(t * log_g)[None, :, None, None] * s0
             + jnp.einsum('bthd,bthe->bhde', k.astype(jnp.float32) * zeta[None, :, :, None], v.astype(jnp.float32)))
    return o, s_new


def dsa_select_attend(q, iq, iw, k, v, ik, limit, n_sel):
    b, nq = q.shape[:2]
    n_keys = k.shape[1]
    idx = jnp.einsum('bqhd,bld->bqhl', iq, ik).astype(jnp.float32) * (IDX_DIM ** -0.5)
    idx = jnp.einsum('bqhl,bqh->bql', jax.nn.relu(idx), iw.astype(jnp.float32))
    admissible = jnp.arange(n_keys) < limit
    idx = jnp.where(admissible[None, None, :], idx, -jnp.inf)
    top_s, sel = lax.top_k(idx, n_sel)
    valid = jnp.isfinite(top_s)
    take = jax.vmap(lambda rows, ids: rows[ids])
    k_sel = take(k, sel)
    v_sel = take(v, sel)
    qg = q.reshape(b, nq, DSA_KV_HEADS, DSA_HEADS // DSA_KV_HEADS, DSA_HEAD_DIM)
    logits = jnp.einsum('bqgrd,bqngd->bqgrn', qg, k_sel).astype(jnp.float32) * (DSA_HEAD_DIM ** -0.5)
    logits = jnp.where(valid[:, :, None, None, :], logits, -jnp.inf)
    p = jax.nn.softmax(logits, axis=-1)
    out = jnp.einsum('bqgrn,bqngd->bqgrd', p, v_sel.astype(jnp.float32))
    return out.reshape(b, nq, DSA_HEADS * DSA_HEAD_DIM)


def merge_branches(x, o_ret, rg, o_dsa, gr, ga, w_ret_o, w_dsa_o, w_o):
    b, t = x.shape[:2]
    mu = jnp.mean(o_ret, -1, keepdims=True)
    var = jnp.mean(jnp.square(o_ret - mu), -1, keepdims=True)
    gn = (o_ret - mu) * lax.rsqrt(var + GN_EPS)
    ret = gn.reshape(b, t, RET_HEADS * RET_DV) * jax.nn.silu(rg.astype(jnp.float32))
    y_ret = jnp.einsum('bte,ed->btd', ret.astype(x.dtype), w_ret_o)
    y_dsa = jnp.einsum('bte,ed->btd', o_dsa.astype(x.dtype), w_dsa_o)
    merged = jax.nn.sigmoid(gr) * y_ret + jax.nn.sigmoid(ga) * y_dsa
    return jnp.einsum('btd,de->bte', merged, w_o)


def token_mixer_prompt(x, w_in, w_ret_o, w_dsa_o, w_o):
    b, s, _ = x.shape
    nc = s // CHUNK
    pos = jnp.arange(s)
    rq, rk, rv, rg, aq, ak, av, iq, ik, iw, gr, ga = project_and_split(x, w_in, pos)

    def chunks(a):
        return a.reshape(b, nc, CHUNK, *a.shape[2:]).swapaxes(0, 1)

    def unchunk(a):
        return a.swapaxes(0, 1).reshape(b, s, *a.shape[3:])

    def ret_step(state, blk):
        o, state = retention_chunk(blk[0], blk[1], blk[2], state)
        return state, o

    s0 = jnp.zeros((b, RET_HEADS, RET_DK, RET_DV), jnp.float32)
    s_fin, o_ret = lax.scan(ret_step, s0, (chunks(rq), chunks(rk), chunks(rv)))
    o_ret = unchunk(o_ret)
    n_sel = min(DSA_TOPK, s // 4)

    def dsa_block(blk):
        qb, iqb, iwb, ci = blk
        return dsa_select_attend(qb, iqb, iwb, ak, av, ik, (ci + 1) * CHUNK, n_sel)

    o_dsa = unchunk(lax.map(dsa_block, (chunks(aq), chunks(iq), chunks(iw), jnp.arange(nc))))
    y = merge_branches(x, o_ret, rg, o_dsa, gr, ga, w_ret_o, w_dsa_o, w_o)
    return y, s_fin, ak, av, ik


def token_mixer_sample(x, s_ret, past_k, past_v, past_ik, w_in, w_ret_o, w_dsa_o, w_o):
    t = x.shape[1]
    p = past_k.shape[1]
    pos = p + jnp.arange(t)
    rq, rk, rv, rg, aq, ak, av, iq, ik, iw, gr, ga = project_and_split(x, w_in, pos)
    o_ret, s_new = retention_chunk(rq, rk, rv, s_ret.astype(jnp.float32))
    k_all = jnp.concatenate([past_k, ak.astype(past_k.dtype)], axis=1)
    v_all = jnp.concatenate([past_v, av.astype(past_v.dtype)], axis=1)
    ik_all = jnp.concatenate([past_ik, ik.astype(past_ik.dtype)], axis=1)
    n_keys = p + t
    n_sel = min(DSA_TOPK, n_keys // 4)
    o_dsa = dsa_select_attend(aq, iq, iw, k_all, v_all, ik_all, n_keys, n_sel)
    y = merge_branches(x, o_ret, rg, o_dsa, gr, ga, w_ret_o, w_dsa_o, w_o)
    return y, s_new, ak, av, ik


def clamped_swiglu(h):
    h = h.astype(jnp.float32)
    glu = jnp.minimum(h[..., ::2], SWIGLU_LIMIT)
    lin = jnp.clip(h[..., 1::2], -SWIGLU_LIMIT, SWIGLU_LIMIT)
    return glu * jax.nn.sigmoid(SWIGLU_ALPHA * glu) * (lin + 1.0)


def moe_ffn(x, router_w, router_b, w_up, b_up, w_down, b_down):
    lead = x.shape[:-1]
    xf = x.reshape(-1, D_MODEL)
    n = xf.shape[0]
    logits = (xf @ router_w + router_b).astype(jnp.float32)
    top_v, top_e = lax.top_k(logits, MOE_TOP_K)
    gates = jax.nn.softmax(top_v, axis=-1)
    flat_e = top_e.reshape(-1)
    flat_t = jnp.repeat(jnp.arange(n, dtype=jnp.int32), MOE_TOP_K)
    flat_g = gates.reshape(-1)
    order = jnp.argsort(flat_e)
    e_sorted = flat_e[order]
    counts = jnp.zeros((N_EXPERTS,), jnp.int32).at[flat_e].add(1)
    padded = (counts + MOE_BLOCK - 1) // MOE_BLOCK * MOE_BLOCK
    pad_end = jnp.cumsum(padded)
    pad_start = pad_end - padded
    start = jnp.cumsum(counts) - counts
    dest = pad_start[e_sorted] + jnp.arange(n * MOE_TOP_K, dtype=jnp.int32) - start[e_sorted]
    n_blocks = -(-(n * MOE_TOP_K) // MOE_BLOCK) + N_EXPERTS
    rows = n_blocks * MOE_BLOCK
    row_tok = jnp.full((rows,), n, jnp.int32).at[dest].set(flat_t[order])
    row_gate = jnp.zeros((rows,), jnp.float32).at[dest].set(flat_g[order])
    block_e = jnp.minimum(jnp.searchsorted(pad_end, jnp.arange(n_blocks, dtype=jnp.int32) * MOE_BLOCK, side='right'),
                          N_EXPERTS - 1)
    x_pad = jnp.concatenate([xf, jnp.zeros((1, D_MODEL), xf.dtype)], axis=0)
    xb = x_pad[row_tok].reshape(n_blocks, MOE_BLOCK, D_MODEL)

    def expert_block(args):
        xblk, e = args
        h = xblk @ w_up[e] + b_up[e]
        a = clamped_swiglu(h).astype(xblk.dtype)
        return a @ w_down[e] + b_down[e]

    yb = lax.map(expert_block, (xb, block_e)).reshape(rows, D_MODEL)
    y = jnp.zeros((n + 1, D_MODEL), jnp.float32).at[row_tok].add(yb.astype(jnp.float32) * row_gate[:, None])
    return y[:n].astype(x.dtype).reshape(*lead, D_MODEL)


def setup_inputs(seed: int = 0) -> dict:
    key = jax.random.key(seed)
    ks = jax.random.split(key, 20)
    f32 = jnp.float32

    def nrm(k, shape, scale):
        return jax.random.normal(k, shape, f32) * scale

    return {
        'x_prompt': nrm(ks[0], (BATCH, SEQ, D_MODEL), 1.0),
        'x_sample': nrm(ks[1], (DEC_BATCH, DEC_SEQ, D_MODEL), 1.0),
        'state_ret': nrm(ks[2], (DEPTH, DEC_BATCH, RET_HEADS, RET_DK, RET_DV), 0.5),
        'cache_k': nrm(ks[3], (DEPTH, DEC_BATCH, PAST_LEN, DSA_KV_HEADS, DSA_HEAD_DIM), 1.0),
        'cache_v': nrm(ks[4], (DEPTH, DEC_BATCH, PAST_LEN, DSA_KV_HEADS, DSA_HEAD_DIM), 1.0),
        'cache_idx_k': nrm(ks[5], (DEPTH, DEC_BATCH, PAST_LEN, IDX_DIM), 1.0),
        'w_in': nrm(ks[6], (DEPTH, D_MODEL, PROJ_TOTAL), D_MODEL ** -0.5),
        'w_ret_o': nrm(ks[7], (DEPTH, RET_HEADS * RET_DV, D_MODEL), DEEPNORM_BETA * (RET_HEADS * RET_DV) ** -0.5),
        'w_dsa_o': nrm(ks[8], (DEPTH, DSA_HEADS * DSA_HEAD_DIM, D_MODEL), DEEPNORM_BETA * (DSA_HEADS * DSA_HEAD_DIM) ** -0.5),
        'w_o': nrm(ks[9], (DEPTH, D_MODEL, D_MODEL), DEEPNORM_BETA * D_MODEL ** -0.5),
        'ln1_g': 1.0 + nrm(ks[10], (DEPTH, D_MODEL), 0.01),
        'ln1_b': nrm(ks[11], (DEPTH, D_MODEL), 0.01),
        'router_w': nrm(ks[12], (DEPTH, D_MODEL, N_EXPERTS), D_MODEL ** -0.5),
        'router_b': nrm(ks[13], (DEPTH, N_EXPERTS), 0.01),
        'w_up': nrm(ks[14], (DEPTH, N_EXPERTS, D_MODEL, 2 * D_FF), D_MODEL ** -0.5),
        'b_up': nrm(ks[15], (DEPTH, N_EXPERTS, 2 * D_FF), 0.01),
        'w_down': nrm(ks[16], (DEPTH, N_EXPERTS, D_FF, D_MODEL), DEEPNORM_BETA * D_FF ** -0.5),
        'b_down': nrm(ks[17], (DEPTH, N_EXPERTS, D_MODEL), 0.01),
        'ln2_g': 1.0 + nrm(ks[18], (DEPTH, D_MODEL), 0.01),
        'ln2_b': nrm(ks[19], (DEPTH, D_MODEL), 0.01),
    }


def reference(x_prompt, x_sample, state_ret, cache_k, cache_v, cache_idx_k, w_in, w_ret_o, w_dsa_o, w_o,
              ln1_g, ln1_b, router_w, router_b, w_up, b_up, w_down, b_down, ln2_g, ln2_b):
    hp, hs = x_prompt, x_sample
    sp_l, kp_l, vp_l, ikp_l = [], [], [], []
    ss_l, ks_l, vs_l, iks_l = [], [], [], []
    for l in range(DEPTH):
        mix_p, s_p, k_p, v_p, ik_p = token_mixer_prompt(hp, w_in[l], w_ret_o[l], w_dsa_o[l], w_o[l])
        hp = layer_norm(DEEPNORM_ALPHA * hp + mix_p, ln1_g[l], ln1_b[l])
        hp = layer_norm(DEEPNORM_ALPHA * hp + moe_ffn(hp, router_w[l], router_b[l], w_up[l], b_up[l], w_down[l], b_down[l]),
                        ln2_g[l], ln2_b[l])
        mix_s, s_s, k_s, v_s, ik_s = token_mixer_sample(hs, state_ret[l], cache_k[l], cache_v[l], cache_idx_k[l],
                                                        w_in[l], w_ret_o[l], w_dsa_o[l], w_o[l])
        hs = layer_norm(DEEPNORM_ALPHA * hs + mix_s, ln1_g[l], ln1_b[l])
        hs = layer_norm(DEEPNORM_ALPHA * hs + moe_ffn(hs, router_w[l], router_b[l], w_up[l], b_up[l], w_down[l], b_down[l]),
                        ln2_g[l], ln2_b[l])
        sp_l.append(s_p); kp_l.append(k_p); vp_l.append(v_p); ikp_l.append(ik_p)
        ss_l.append(s_s); ks_l.append(k_s); vs_l.append(v_s); iks_l.append(ik_s)
    return (hp, hs, jnp.stack(sp_l), jnp.stack(kp_l), jnp.stack(vp_l), jnp.stack(ikp_l),
            jnp.stack(ss_l), jnp.stack(ks_l), jnp.stack(vs_l), jnp.stack(iks_l))
```

```python
import contextlib
import numpy as np
import concourse.bass as bass
import concourse.mybir as mybir
from concourse.bass_utils import run_bass_kernel_spmd

F32 = mybir.dt.float32
BF16 = mybir.dt.bfloat16
I32 = mybir.dt.int32
U32 = mybir.dt.uint32
ALU = mybir.AluOpType
AF = mybir.ActivationFunctionType
AX = mybir.AxisListType

D = 1024
NDS = 32
NEG = -1.0e30
N_EXP = 32
D_FF = 1024
ALPHA = 2.0 ** 0.25
PROJ = 6472


class Sched:
    ENG = ('pe', 'dve', 'act', 'pool', 'sp')
    DMAQ = ('sp', 'act', 'pool')
    ENGOBJ = {'pe': 'tensor', 'dve': 'vector', 'act': 'scalar', 'pool': 'gpsimd', 'sp': 'sync'}

    def __init__(self, nc, stack):
        self.nc = nc
        self.streams = {e: [] for e in self.ENG}
        self.cnt = {e: 0 for e in self.ENG}
        self.dcnt = {q: [0] * NDS for q in self.DMAQ}
        self.drr = {q: 0 for q in self.DMAQ}
        self.pw = {}
        self.last_w = {}
        self.readers = {}
        self.nops = 0
        self.sems = {}
        for e in self.ENG:
            self.sems[('c', e)] = stack.enter_context(nc.semaphore('c_' + e))
        for q in self.DMAQ:
            for j in range(NDS):
                self.sems[('d', q, j)] = stack.enter_context(nc.semaphore('d_%s_%d' % (q, j)))
        self.fence = {e: stack.enter_context(nc.sbuf_tensor('fence_' + e, [128, 2], F32)) for e in ('dve', 'act', 'pool')}

    def op(self, eng, meth, reads=(), writes=(), dma=False, after=(), **kw):
        fn = (meth, kw)
        deps = set(t for t in after if t is not None)

        for r in reads:
            t = self.last_w.get(r)
            if t is not None:
                deps.add(t)
        for w in writes:
            t = self.last_w.get(w)
            if t is not None:
                deps.add(t)
            for t in self.readers.get(w, ()):
                deps.add(t)
        if dma:
            j = self.drr[eng]
            self.drr[eng] = (j + 1) % NDS
            self.dcnt[eng][j] += 1
            tok = (('d', eng, j), 16 * self.dcnt[eng][j])
        else:
            self.cnt[eng] += 1
            tok = (('c', eng), self.cnt[eng])
        need = {}
        for (sk, v) in deps:
            if sk == ('c', 'pe') and eng == 'pe' and not dma:
                continue
            need[sk] = max(need.get(sk, 0), v)
        waits = []
        for sk, v in need.items():
            if self.pw.get((eng, sk), 0) < v:
                waits.append((sk, v))
                self.pw[(eng, sk)] = v
        for r in reads:
            self.readers.setdefault(r, []).append(tok)
        for w in writes:
            self.last_w[w] = tok
            self.readers[w] = []
        self.streams[eng].append((waits, fn, tok, dma))
        self.nops += 1
        if kw.get('accum_out') is not None and not kw.get('_nofence'):
            if eng == 'act':
                return self.op('act', 'memzero', (), writes, ap=self.fence['act'][:, 0:1])
            return self.op(eng, 'memset', (), writes, ap=self.fence[eng][:, 0:1], constant=0.0)
        return tok

    def barrier(self):
        cur = []
        for e in self.ENG:
            if self.cnt[e]:
                cur.append((('c', e), self.cnt[e]))
        for q in self.DMAQ:
            for j in range(NDS):
                if self.dcnt[q][j]:
                    cur.append((('d', q, j), 16 * self.dcnt[q][j]))
        for e in self.ENG:
            waits = []
            for sk, v in cur:
                if sk == ('c', e):
                    continue
                if self.pw.get((e, sk), 0) < v:
                    waits.append((sk, v))
                    self.pw[(e, sk)] = v
            self.streams[e].append((waits, None, None, False))
        self.last_w = {}
        self.readers = {}

    def flush(self):
        nc = self.nc
        sems = self.sems
        with nc.Block() as block:
            def mk(ename):
                stream = self.streams[ename]

                def body(eng):
                    for (waits, fn, tok, dma) in stream:
                        for (sk, v) in waits:
                            eng.wait_ge(sems[sk], v)
                        if fn is not None:
                            try:
                                inst = getattr(eng, fn[0])(**fn[1])
                            except Exception:
                                print("FAILED OP", ename, fn[0], {k: (getattr(v, 'shape', v), getattr(v, 'name', '')) for k, v in fn[1].items()})
                                raise
                            inst.then_inc(sems[tok[0]], 16 if dma else 1)
                return body
            for e in self.ENG:
                getattr(block, self.ENGOBJ[e])(mk(e))
        self.streams = {e: [] for e in self.ENG}

    def tt(self, eng, out, in0, in1, op, r, w):
        return self.op(eng, 'tensor_tensor', r, w, out=out, in0=in0, in1=in1, op=op)

    def ts(self, eng, out, in0, s1, s2, op0, op1, r, w, accum=None):
        kw = dict(out=out, in0=in0, scalar1=s1, scalar2=s2, op0=op0)
        if op1 is not None:
            kw['op1'] = op1
        if accum is not None:
            kw['accum_out'] = accum
        return self.op(eng, 'tensor_scalar', r, w, **kw)

    def stt(self, eng, out, in0, scalar, in1, op0, op1, r, w, accum=None):
        kw = dict(out=out, in0=in0, scalar=scalar, in1=in1, op0=op0, op1=op1)
        if accum is not None:
            kw['accum_out'] = accum
        return self.op(eng, 'scalar_tensor_tensor', r, w, **kw)

    def copy(self, eng, out, in_, r, w):
        if eng == 'act':
            return self.op('act', 'copy', r, w, out=out, in_=in_)
        return self.op(eng, 'tensor_copy', r, w, out=out, in_=in_)

    def act(self, out, in_, func, r, w, bias=None, scale=None, accum=None):
        kw = dict(out=out, in_=in_, func=func)
        if bias is not None:
            kw['bias'] = bias
        if scale is not None:
            kw['scale'] = scale
        if accum is not None:
            kw['accum_out'] = accum
        return self.op('act', 'activation', r, w, **kw)

    def mul(self, out, in_, m, r, w):
        return self.op('act', 'mul', r, w, out=out, in_=in_, mul=m)

    def mm(self, out, lhsT, rhs, start, stop, r, w):
        return self.op('pe', 'matmul', r, w, out=out, lhsT=lhsT, rhs=rhs, start=start, stop=stop)

    def tr(self, out, in_, ident, r, w):
        return self.op('pe', 'transpose', r, w, out=out, in_=in_, identity=ident)

    def dma(self, q, out, in_, r=(), w=()):
        return self.op(q, 'dma_start', r, w, dma=True, out=out, in_=in_)

    def memset(self, eng, ap, val, w):
        return self.op(eng, 'memset', (), w, ap=ap, constant=val)


def _gammas():
    return (1.0 - 2.0 ** (-5.0 - np.arange(4, dtype=np.float32))).astype(np.float32)


def make_consts(seq):
    f32 = np.float32
    ret_f = (np.float32(10000.0) ** (-np.linspace(0.0, 1.0, 64, dtype=f32))).astype(f32)
    att_f = (np.float32(500000.0) ** (-np.arange(0, 16, 2, dtype=f32) / np.float32(16))).astype(f32)

    def rot_table(pos):
        pos = pos.astype(f32)
        a = pos[:, None] * ret_f[None, :]
        b = pos[:, None] * att_f[None, :]
        return np.concatenate([np.cos(a), np.sin(a), np.cos(b), np.sin(b)], axis=1).astype(f32)

    c = {}
    c['rotp'] = rot_table(np.arange(seq))
    c['rots'] = rot_table(1024 + np.arange(64))
    lg = np.log(_gammas()).astype(f32)

    def dec(T):
        pos = np.arange(T, dtype=f32)
        diff = pos[:, None] - pos[None, :]
        d = np.where(diff >= 0, np.exp(np.maximum(diff, 0.0)[None] * lg[:, None, None]), 0.0)
        decT = np.ascontiguousarray(d.transpose(2, 0, 1)).astype(f32)
        xi = np.exp((pos + 1.0)[None, :] * lg[:, None]).astype(f32)
        zeta = np.exp((T - 1.0 - pos)[:, None] * lg[None, :]).astype(f32)
        gT = np.exp(T * lg).astype(f32)
        return decT, xi, zeta, gT

    d128, xi128, z128, g128 = dec(128)
    d64, xi64, z64, g64 = dec(64)
    c['dec128'] = d128.reshape(128, 512)
    c['dec64'] = d64.reshape(64, 256)
    c['xi128'] = np.ascontiguousarray(np.broadcast_to(xi128.reshape(1, 512), (128, 512)))
    c['xi64'] = np.ascontiguousarray(np.broadcast_to(xi64.reshape(1, 256), (128, 256)))
    sc = np.float32(128.0 ** -0.5)
    c['zp'] = (z128 * sc).astype(f32)
    c['zs'] = (np.concatenate([z64, z64], 0) * sc).astype(f32)
    c['g128'] = g128
    c['g64'] = g64
    return c


def col_perm():
    w = [512, 512, 1024, 1024, 512, 128, 128, 512, 64, 8, 1024, 1024]
    off = np.concatenate([[0], np.cumsum(w)])
    rq, rk, rv, rg, aq, ak, av, iq, ik, iw, gr, ga = [np.arange(off[i], off[i + 1]) for i in range(12)]
    aqh = aq.reshape(8, 64)
    aq_new = np.concatenate([aqh[h] for h in (0, 4, 1, 5, 2, 6, 3, 7)])
    return np.concatenate([rq, rk, rv, rg, aq_new, iq, ak, ik, av, iw, gr, ga])


HEAD_ORDER = (0, 4, 1, 5, 2, 6, 3, 7)


def build(cfg):
    NPS, SEQ, NSS = cfg['NPS'], cfg['SEQ'], cfg['NSS']
    dbg = cfg.get('debug', False)
    phases = cfg.get('phases', 4)
    JT = SEQ // 128
    NTP = NPS * JT
    NTS = NSS // 2
    NT = NTP + NTS
    NTOK = NT * 128
    g128 = [float(v) for v in _gammas() ** 128]
    g64 = [float(v) for v in _gammas() ** 64]

    nc = bass.Bass("TRN2", target_bir_lowering=False)

    def din(name, shape, dt=F32):
        return nc.dram_tensor(name, list(shape), dt, kind="ExternalInput").ap()

    def dout(name, shape, dt=F32):
        return nc.dram_tensor(name, list(shape), dt, kind="ExternalOutput").ap()

    def dscr(name, shape, dt):
        if dbg:
            return nc.dram_tensor(name, list(shape), dt, kind="ExternalOutput").ap()
        return nc.dram_tensor(name, list(shape), dt).ap()

    xp = din("xp", [NPS, SEQ, D])
    xs = din("xs", [NSS, 64, D])
    state_ret = din("state_ret", [NSS, 4, 128, 256])
    cache_k = din("cache_k", [NSS, 1024, 128])
    cache_v = din("cache_v", [NSS, 1024, 128])
    cache_ik = din("cache_ik", [NSS, 1024, 64])
    w_in = din("w_in", [D, PROJ])
    w_ret_o = din("w_ret_o", [D, D])
    w_dsa_o = din("w_dsa_o", [512, D])
    w_o = din("w_o", [D, D])
    ln1_g = din("ln1_g", [1, D]); ln1_b = din("ln1_b", [1, D])
    ln2_g = din("ln2_g", [1, D]); ln2_b = din("ln2_b", [1, D])
    router_w = din("router_w", [D, N_EXP]); router_b = din("router_b", [1, N_EXP])
    NE = N_EXP if phases >= 3 else 1
    w_up = din("w_up", [NE, D, 2 * D_FF]); b_up = din("b_up", [NE, 2 * D_FF])
    w_down = din("w_down", [NE, D_FF, D]); b_down = din("b_down", [NE, D])
    c_rotp = din("c_rotp", [SEQ, 144]); c_rots = din("c_rots", [64, 144])
    c_dec128 = din("c_dec128", [128, 512]); c_dec64 = din("c_dec64", [64, 256])
    c_xi128 = din("c_xi128", [128, 512]); c_xi64 = din("c_xi64", [128, 256])
    c_zp = din("c_zp", [128, 4]); c_zs = din("c_zs", [128, 4])

    y_p = dout("y_p", [NPS, SEQ, D]); y_s = dout("y_s", [NSS, 64, D])
    rs_p = dout("rs_p", [NPS, 4, 128, 256]); rs_s = dout("rs_s", [NSS, 4, 128, 256])
    k_p = dout("k_p", [NPS, SEQ, 128]); v_p = dout("v_p", [NPS, SEQ, 128]); ik_p = dout("ik_p", [NPS, SEQ, 64])
    k_s = dout("k_s", [NSS, 64, 128]); v_s = dout("v_s", [NSS, 64, 128]); ik_s = dout("ik_s", [NSS, 64, 64])

    s_rqT = dscr("s_rqT", [NT, 128, 512], BF16)
    s_rkT = dscr("s_rkT", [NT, 128, 512], BF16)
    s_rkz = dscr("s_rkz", [NT, 128, 512], BF16)
    s_rv = dscr("s_rv", [NT, 128, 1024], BF16)
    s_srg = dscr("s_srg", [NT, 128, 1024], BF16)
    s_aqT = dscr("s_aqT", [NT, 128, 512], BF16)
    s_iqT = dscr("s_iqT", [NT, 128, 512], BF16)
    s_iw = dscr("s_iw", [NT, 128, 8], F32)
    s_sgr = dscr("s_sgr", [NT, 128, 1024], F32)
    s_sga = dscr("s_sga", [NT, 128, 1024], F32)
    s_kT = dscr("s_kT", [NT, 128, 128], BF16)
    s_v = dscr("s_v", [NT, 128, 130], BF16)
    s_ikT = dscr("s_ikT", [NT, 128, 128], BF16)
    C = cfg['C']
    s_h1 = dscr("s_h1", [NTOK, D], F32)
    s_h1b = dscr("s_h1b", [NTOK + 128, D], BF16)
    slots = dscr("slots", [N_EXP * C + 128, 4], F32)
    yk = dscr("yk", [NTOK * 4 + 128, D], F32)

    tiles = []
    for s in range(NPS):
        for j in range(JT):
            tiles.append(('p', s, j))
    for u in range(NTS):
        tiles.append(('s', u, 0))

    with contextlib.ExitStack() as gst:
        S = Sched(nc, gst)

        def T(st, name, shape, dt):
            return st.enter_context(nc.sbuf_tensor(name, list(shape), dt))

        def P(st, name, shape, dt):
            return st.enter_context(nc.psum_tensor(name, list(shape), dt))

        identf = T(gst, "identf", [128, 128], F32)
        identb = T(gst, "identb", [128, 128], BF16)
        S.op('pool', 'iota', (), ['identf'], out=identf[:], pattern=[[1, 128]], base=0, channel_multiplier=-1,
             allow_small_or_imprecise_dtypes=True)
        S.ts('dve', identb[:], identf[:], 0.0, None, ALU.is_equal, None, ['identf'], ['identb'])
        S.ts('dve', identf[:], identf[:], 0.0, None, ALU.is_equal, None, ['identf', 'identb'], ['identf'])

        G = locals()
        if phases >= 1:
            phase1(nc, S, T, P, cfg, tiles, G)
        if phases >= 2:
            phase2(nc, S, T, P, cfg, tiles, G)
            phase2b(nc, S, T, P, cfg, tiles, G)
        if phases >= 3:
            phase3(nc, S, T, P, cfg, tiles, G)
        if phases >= 4:
            phase4(nc, S, T, P, cfg, tiles, G)

        S.barrier()
        S.flush()
    return nc


def phase1(nc, S, T, P, cfg, tiles, G):
    NT = len(tiles)
    identf, identb = G['identf'], G['identb']
    xp, xs, w_in = G['xp'], G['xs'], G['w_in']
    with contextlib.ExitStack() as st:
        wsb = T(st, "wsb", [128, 8, PROJ], BF16)
        xf = [T(st, "xf%d" % i, [128, D], F32) for i in range(2)]
        rot = [T(st, "rot%d" % i, [128, 144], F32) for i in range(2)]
        zt = [T(st, "zt%d" % i, [128, 4], F32) for i in range(2)]
        xT = T(st, "xT", [128, 8, 128], BF16)
        zc = [T(st, "zc%d" % i, [128, 512], F32) for i in range(2)]
        tmpa = [T(st, "tmpa%d" % i, [128, 4, 256], F32) for i in range(2)]
        rotd = [T(st, "rotd%d" % i, [128, 512], F32) for i in range(2)]
        qb = [T(st, "qb%d" % i, [128, 512], BF16) for i in range(2)]
        kzb = T(st, "kzb", [128, 512], BF16)
        trs = [T(st, "trs%d" % i, [128, 512], BF16) for i in range(2)]
        rvb = [T(st, "rvb%d" % i, [128, 512], BF16) for i in range(2)]
        sgf = [T(st, "sgf%d" % i, [128, 512], F32) for i in range(2)]
        z8 = T(st, "z8", [128, 328], F32)
        t8 = T(st, "t8", [128, 4, 24], F32)
        kb8 = T(st, "kb8", [128, 256], BF16)
        kTs = T(st, "kTs", [128, 256], BF16)
        vaug = T(st, "vaug", [128, 2, 65], BF16)
        iws = T(st, "iws", [128, 8], F32)
        psT = [P(st, "psT%d" % i, [128, 4, 128], F32) for i in range(2)]
        psz = [P(st, "psz%d" % i, [128, 512], F32) for i in range(4)]
        pstr = [P(st, "pstr%d" % i, [128, 4, 128], BF16) for i in range(2)]

        phase0_init(nc, S, T, P, cfg, G, st)
        wv = w_in.rearrange("(k p) n -> p k n", p=128)
        wbounds = [0, 1024, 2048, 3072, 4096, 4424, 5448, PROJ]
        for gi_ in range(7):
            S.dma('pool', wsb[:, :, wbounds[gi_]:wbounds[gi_ + 1]], wv[:, :, wbounds[gi_]:wbounds[gi_ + 1]], (), [('wsb', gi_)])

        def wgrp(coff):
            for gi_ in range(7):
                if wbounds[gi_] <= coff < wbounds[gi_ + 1]:
                    return gi_
        S.dma('sp', zt[0][:], G['c_zp'], (), ['zt0'])
        S.dma('sp', zt[1][:], G['c_zs'], (), ['zt1'])
        S.memset('pool', vaug[:], 1.0, ['vaug'])

        cnt = {'z': 0, 'tr': 0, 'q': 0, 'g': 0}

        def load_tile(ti):
            kind, a, j = tiles[ti]
            b = ti % 2
            if kind == 'p':
                S.dma('sp', xf[b][:], xp[a, j * 128:(j + 1) * 128, :], (), ['xf%d' % b])
                S.dma('sp', rot[b][:], G['c_rotp'][j * 128:(j + 1) * 128, :], (), ['rot%d' % b])
            else:
                for hh in range(2):
                    S.dma('sp', xf[b][hh * 64:(hh + 1) * 64, :], xs[2 * a + hh], (), ['xf%d' % b])
                    S.dma('sp', rot[b][hh * 64:(hh + 1) * 64, :], G['c_rots'], (), ['rot%d' % b])

        def transposes4(src_bf, dst_ap, srcname, dstname):
            i = cnt['tr'] % 2
            cnt['tr'] += 1
            for c in range(4):
                S.tr(pstr[i][:, c, :], src_bf[:, c * 128:(c + 1) * 128], identb[:], [srcname, 'identb'], ['pstr%d' % i])
            S.copy('act', dst_ap, pstr[i][:], ['pstr%d' % i], [dstname])

        def rotary_full(eng, src, dst, cosb, sinb, tmp, srcname, dstname, tmpname, rotname):
            sv = src.rearrange("p (h t i) -> p h t i", h=4, t=2)
            dv = dst.rearrange("p (h t i) -> p h t i", h=4, t=2)
            x1, x2 = sv[:, :, 0, :], sv[:, :, 1, :]
            ta, tb, tc_, td = tmp[:, :, 0:64], tmp[:, :, 64:128], tmp[:, :, 128:192], tmp[:, :, 192:256]
            S.tt(eng, ta, x1, cosb, ALU.mult, [srcname, rotname], [tmpname + 'a'])
            S.tt(eng, tb, x2, sinb, ALU.mult, [srcname, rotname], [tmpname + 'b'])
            S.tt(eng, tc_, x1, sinb, ALU.mult, [srcname, rotname], [tmpname + 'c'])
            S.tt(eng, td, x2, cosb, ALU.mult, [srcname, rotname], [tmpname + 'd'])
            S.tt(eng, dv[:, :, 0, :], ta, tb, ALU.subtract, [tmpname + 'a', tmpname + 'b'], [dstname + 'lo'])
            S.tt(eng, dv[:, :, 1, :], tc_, td, ALU.add, [tmpname + 'c', tmpname + 'd'], [dstname + 'hi'])

        def rotary_part(eng, buf, nh, cosa, sina, tmp, bufname, tmpname, rotname):
            bv = buf.rearrange("p (h i) -> p h i", h=nh)
            x1, x2 = bv[:, :, 0:8], bv[:, :, 8:16]
            cb = cosa.unsqueeze(1).broadcast_to([128, nh, 8])
            sb = sina.unsqueeze(1).broadcast_to([128, nh, 8])
            v3 = lambda t: t.rearrange("p (h i) -> p h i", h=nh)
            ta, tb, tc_, td = [v3(tmp[:, q, 0:nh * 8]) for q in range(4)]
            S.tt(eng, ta, x1, cb, ALU.mult, [bufname, rotname], [tmpname + 'a'])
            S.tt(eng, tb, x2, sb, ALU.mult, [bufname, rotname], [tmpname + 'b'])
            S.tt(eng, tc_, x1, sb, ALU.mult, [bufname, rotname], [tmpname + 'c'])
            S.tt(eng, td, x2, cb, ALU.mult, [bufname, rotname], [tmpname + 'd'])
            S.tt(eng, x1, ta, tb, ALU.subtract, [tmpname + 'a', tmpname + 'b', tmpname + 'c'], [bufname])
            S.tt(eng, x2, tc_, td, ALU.add, [tmpname + 'c', tmpname + 'd'], [bufname])

        order = ['rq', 'rk', 'rv0', 'rv1', 'rg0', 'rg1', 'aq', 'iq', 'b8', 'gr0', 'gr1', 'ga0', 'ga1']
        pending = []
        load_tile(0)
        for ti in range(NT):
            while pending:
                pending.pop(0)[1]()
            kind, a, j = tiles[ti]
            b = ti % 2
            if ti + 1 < NT:
                load_tile(ti + 1)
            ykz = G['_ykz']
            for _ in range(-(-len(ykz) // max(1, NT - ti))):
                if ykz:
                    n_, ap_, zt__ = ykz.pop(0)
                    S.dma('sp', ap_, zt__[:], ['zero_t'], [('yk_init', n_)])
            xfn, rotn = 'xf%d' % b, 'rot%d' % b
            ztn = 'zt0' if kind == 'p' else 'zt1'
            ztt = zt[0] if kind == 'p' else zt[1]
            for hf in range(2):
                for c in range(4):
                    k = hf * 4 + c
                    S.tr(psT[hf][:, c, :], xf[b][:, k * 128:(k + 1) * 128], identf[:], [xfn, 'identf'], ['psT%d' % hf])
                S.copy('act', xT[:, hf * 4:(hf + 1) * 4, :], psT[hf][:], ['psT%d' % hf], [('xT', hf)])
            cosb = rot[b][:, 0:64].unsqueeze(1).broadcast_to([128, 4, 64])
            sinb = rot[b][:, 64:128].unsqueeze(1).broadcast_to([128, 4, 64])
            cosa = rot[b][:, 128:136]
            sina = rot[b][:, 136:144]
            for blk in range(13):
                name = order[blk]
                wdt = 328 if name == 'b8' else 512
                if blk < 8:
                    coff = blk * 512
                elif name == 'b8':
                    coff = 8 * 512
                else:
                    coff = 8 * 512 + 328 + (blk - 9) * 512
                pz = cnt['z'] % 4
                cnt['z'] += 1
                pzn = 'psz%d' % pz
                if pending and pending[0][0] <= blk:
                    pending.pop(0)[1]()
                for k in range(8):
                    S.mm(psz[pz][:, 0:wdt], xT[:, k, :], wsb[:, k, coff:coff + wdt], k == 0, k == 7,
                         [('xT', k // 4), ('wsb', wgrp(coff))], [pzn])
                if name in ('rq', 'rk'):
                    i = cnt['q'] % 2
                    cnt['q'] += 1
                    S.copy('act', zc[i][:], psz[pz][:], [pzn], ['zc%d' % i])
                    eng = 'dve' if name == 'rq' else 'pool'
                    rotary_full(eng, zc[i][:], rotd[i][:], cosb, sinb, tmpa[i], 'zc%d' % i, 'rotd%d' % i, 'tmpa%d' % i, rotn)
                    rdn = ['rotd%dlo' % i, 'rotd%dhi' % i]
                    trv = trs[i][:].rearrange("p (c t) -> p c t", c=4)
                    if name == 'rq':
                        S.copy('act', qb[i][:], rotd[i][:], rdn, ['qb%d' % i])
                        def _f(i=i, trv=trv, ti=ti):
                            transposes4(qb[i], trv, 'qb%d' % i, 'trs%d' % i)
                            S.dma('sp', G['s_rqT'][ti], trs[i][:], ['trs%d' % i])
                        pending.append((blk + 2, _f))
                    else:
                        S.mul(qb[i][:], rotd[i][:], float(128.0 ** -0.5), rdn, ['qb%d' % i])
                        def _f(i=i, trv=trv, ti=ti):
                            transposes4(qb[i], trv, 'qb%d' % i, 'trs%d' % i)
                            S.dma('sp', G['s_rkT'][ti], trs[i][:], ['trs%d' % i])
                        pending.append((blk + 2, _f))
                        S.tt('dve', kzb[:].rearrange("p (h d) -> p h d", h=4),
                             rotd[i][:].rearrange("p (h d) -> p h d", h=4),
                             ztt[:].unsqueeze(2).broadcast_to([128, 4, 128]), ALU.mult, rdn + [ztn], ['kzb'])
                        S.dma('sp', G['s_rkz'][ti], kzb[:], ['kzb'])
                elif name in ('rv0', 'rv1', 'rg0', 'rg1'):
                    i = cnt['g'] % 2
                    cnt['g'] += 1
                    hh = int(name[-1])
                    if name[:2] == 'rv':
                        S.copy('act', rvb[i][:], psz[pz][:], [pzn], ['rvb%d' % i])
                        dst = G['s_rv']
                    else:
                        S.act(rvb[i][:], psz[pz][:], AF.Silu, [pzn], ['rvb%d' % i])
                        dst = G['s_srg']
                    S.dma('sp', dst[ti, :, hh * 512:(hh + 1) * 512], rvb[i][:], ['rvb%d' % i])
                elif name in ('aq', 'iq'):
                    i = cnt['q'] % 2
                    cnt['q'] += 1
                    S.copy('act', zc[i][:], psz[pz][:], [pzn], ['zc%d' % i])
                    rotary_part('pool', zc[i][:], 8, cosa, sina, tmpa[i], 'zc%d' % i, 'tmpa%d' % i, rotn)
                    S.mul(qb[i][:], zc[i][:], 0.125, ['zc%d' % i], ['qb%d' % i])
                    dst = G['s_aqT'] if name == 'aq' else G['s_iqT']
                    def _f(i=i, ti=ti, dst=dst):
                        transposes4(qb[i], trs[i][:].rearrange("p (c t) -> p c t", c=4), 'qb%d' % i, 'trs%d' % i)
                        S.dma('sp', dst[ti], trs[i][:], ['trs%d' % i])
                    pending.append((blk + 2, _f))
                elif name in ('gr0', 'gr1', 'ga0', 'ga1'):
                    i = cnt['g'] % 2
                    cnt['g'] += 1
                    hh = int(name[-1])
                    dst = G['s_sgr'] if name[:2] == 'gr' else G['s_sga']
                    S.act(sgf[i][:], psz[pz][:], AF.Sigmoid, [pzn], ['sgf%d' % i])
                    S.dma('sp', dst[ti, :, hh * 512:(hh + 1) * 512], sgf[i][:], ['sgf%d' % i])
                else:
                    S.copy('act', z8[:], psz[pz][:, 0:328], [pzn], ['z8'])
                    rotary_part('dve', z8[:, 0:192], 3, cosa, sina, t8, 'z8', 't8', rotn)
                    if kind == 'p':
                        r0 = j * 128
                        S.dma('sp', G['k_p'][a, r0:r0 + 128, :], z8[:, 0:128], ['z8'])
                        S.dma('sp', G['ik_p'][a, r0:r0 + 128, :], z8[:, 128:192], ['z8'])
                        S.dma('sp', G['v_p'][a, r0:r0 + 128, :], z8[:, 192:320], ['z8'])
                    else:
                        for hh in range(2):
                            sl = slice(hh * 64, (hh + 1) * 64)
                            S.dma('sp', G['k_s'][2 * a + hh], z8[sl, 0:128], ['z8'])
                            S.dma('sp', G['ik_s'][2 * a + hh], z8[sl, 128:192], ['z8'])
                            S.dma('sp', G['v_s'][2 * a + hh], z8[sl, 192:320], ['z8'])
                    S.copy('act', kb8[:, 0:128], z8[:, 0:128], ['z8'], ['kb8a'])
                    S.copy('act', kb8[:, 128:192], z8[:, 128:192], ['z8'], ['kb8b'])
                    S.copy('act', kb8[:, 192:256], z8[:, 128:192], ['z8'], ['kb8c'])
                    def _f(ti=ti):
                        i = cnt['tr'] % 2
                        cnt['tr'] += 1
                        for c in range(2):
                            S.tr(pstr[i][:, c, :], kb8[:, c * 128:(c + 1) * 128], identb[:],
                                 ['kb8a', 'kb8b', 'kb8c', 'identb'], ['pstr%d' % i])
                        S.copy('act', kTs[:].rearrange("p (c t) -> p c t", c=2), pstr[i][:, 0:2, :], ['pstr%d' % i], ['kTs'])
                        S.dma('sp', G['s_kT'][ti], kTs[:, 0:128], ['kTs'])
                        S.dma('sp', G['s_ikT'][ti], kTs[:, 128:256], ['kTs'])
                    pending.append((blk + 2, _f))
                    S.copy('act', vaug[:, :, 0:64], z8[:, 192:320].rearrange("p (g d) -> p g d", g=2), ['z8', 'vaug'], ['vaug'])
                    S.dma('sp', G['s_v'][ti], vaug[:].rearrange("p g d -> p (g d)"), ['vaug'])
                    S.mul(iws[:], z8[:, 320:328], float(8.0 ** -0.5), ['z8'], ['iws'])
                    S.dma('sp', G['s_iw'][ti], iws[:], ['iws'])
        while pending:
            pending.pop(0)[1]()
        S.barrier()
        S.flush()


def phase2(nc, S, T, P, cfg, tiles, G):
    NPS, SEQ, NSS = cfg['NPS'], cfg['SEQ'], cfg['NSS']
    C = cfg['C']
    NIT = cfg.get('NIT', 18)
    JT = SEQ // 128
    NTP = NPS * JT
    LK = max(SEQ, 1152)
    NKB = LK // 128
    identf, identb = G['identf'], G['identb']
    gam = _gammas().astype(np.float64)
    g128 = [float(v) for v in gam ** 128]
    g64 = [float(v) for v in gam ** 64]
    IOA = bass.IndirectOffsetOnAxis
    with contextlib.ExitStack() as st:
        wro = T(st, "wro", [128, 8, D], BF16)
        wdo = T(st, "wdo", [128, 4, D], BF16)
        wo = T(st, "wo", [128, 8, D], BF16)
        rw = T(st, "rw", [128, 8, N_EXP], F32)
        rb = T(st, "rb", [128, N_EXP], F32)
        l1g = T(st, "l1g", [128, D], F32)
        l1b = T(st, "l1b", [128, D], F32)
        dec128 = T(st, "dec128", [128, 512], F32)
        dec64 = T(st, "dec64", [64, 256], F32)
        xi128 = T(st, "xi128", [128, 512], F32)
        xi64 = T(st, "xi64", [128, 256], F32)
        pow2 = T(st, "pow2", [128, NIT], F32)
        trib = T(st, "trib", [128, 128], BF16)
        onesb = T(st, "onesb", [128, 128], BF16)
        iotae = T(st, "iotae", [128, N_EXP], F32)
        pidx = T(st, "pidx", [128, 1], F32)
        kconst = T(st, "kconst", [128, 4], F32)
        tmpc = T(st, "tmpc", [128, 128], F32)
        carry = T(st, "carry", [128, N_EXP], F32)
        S.dma('pool', wro[:], G['w_ret_o'].rearrange("(k p) n -> p k n", p=128), (), ['wro'])
        S.dma('pool', wdo[:], G['w_dsa_o'].rearrange("(k p) n -> p k n", p=128), (), ['wdo'])
        S.dma('pool', wo[:], G['w_o'].rearrange("(k p) n -> p k n", p=128), (), ['wo'])
        S.dma('sp', rw[:], G['router_w'].rearrange("(k p) n -> p k n", p=128), (), ['rw'])
        S.dma('sp', rb[:], G['router_b'].partition_broadcast(128)[:, 0, :], (), ['rb'])
        S.dma('sp', l1g[:], G['ln1_g'].partition_broadcast(128)[:, 0, :], (), ['l1g'])
        S.dma('sp', l1b[:], G['ln1_b'].partition_broadcast(128)[:, 0, :], (), ['l1b'])
        S.dma('sp', dec128[:], G['c_dec128'], (), ['dec128'])
        S.dma('sp', dec64[:], G['c_dec64'], (), ['dec64'])
        S.dma('sp', xi128[:], G['c_xi128'], (), ['xi128'])
        S.dma('sp', xi64[:], G['c_xi64'], (), ['xi64'])
        for i in range(NIT):
            S.memset('pool', pow2[:, i:i + 1], float(2.0 ** -(i + 1)), ['pow2'])
        S.op('pool', 'iota', (), ['tmpc'], out=tmpc[:], pattern=[[1, 128]], base=0, channel_multiplier=-1,
             allow_small_or_imprecise_dtypes=True)
        S.ts('dve', trib[:], tmpc[:], 0.0, None, ALU.is_gt, None, ['tmpc'], ['trib'])
        S.memset('pool', onesb[:], 1.0, ['onesb'])
        S.op('pool', 'iota', (), ['iotae'], out=iotae[:], pattern=[[1, N_EXP]], base=0, channel_multiplier=0,
             allow_small_or_imprecise_dtypes=True)
        S.op('pool', 'iota', (), ['pidx'], out=pidx[:], pattern=[[0, 1]], base=0, channel_multiplier=1,
             allow_small_or_imprecise_dtypes=True)
        S.op('pool', 'iota', (), ['kconst'], out=kconst[:], pattern=[[1, 4]], base=0, channel_multiplier=0,
             allow_small_or_imprecise_dtypes=True)
        S.memset('pool', carry[:], 0.0, ['carry'])

        S32 = T(st, "S32", [128, 4, 256], F32)
        Sb = T(st, "Sb", [128, 4, 256], BF16)
        KT = T(st, "KT", [128, LK], BF16)
        VA = T(st, "VA", [128, NKB, 2, 65], BF16)
        IKT = T(st, "IKT", [128, LK], BF16)
        kc = T(st, "kc", [128, 8, 128], BF16)
        ikc = T(st, "ikc", [128, 8, 2, 64], BF16)
        S.memset('pool', VA[:], 1.0, ['VA'])
        S.memset('pool', KT[:], 0.0, ['KT'])
        rqT_t = T(st, "rqT_t", [128, 512], BF16)
        rkT_t = T(st, "rkT_t", [128, 512], BF16)
        rkz_t = T(st, "rkz_t", [128, 512], BF16)
        rv_t = T(st, "rv_t", [128, 1024], BF16)
        srg_t = T(st, "srg_t", [128, 1024], BF16)
        aqT_t = T(st, "aqT_t", [128, 512], BF16)
        iqT_tt = [T(st, "iqT_t%d" % i, [128, 512], BF16) for i in range(2)]
        iw_tt = [T(st, "iw_t%d" % i, [128, 8], F32) for i in range(2)]
        sgr_t = T(st, "sgr_t", [128, 1024], F32)
        sga_t = T(st, "sga_t", [128, 1024], F32)
        x_t = T(st, "x_t", [128, D], F32)
        scTd = T(st, "scTd", [128, 512], BF16)
        rqxi = T(st, "rqxi", [128, 512], BF16)
        o_sb = T(st, "o_sb", [128, 1024], F32)
        junkf = T(st, "junkf", [128, 1024], F32)
        stt_ = T(st, "stt_", [128, 32], F32)
        retb = T(st, "retb", [128, 1024], BF16)
        retT = T(st, "retT", [128, 8, 128], BF16)
        idx = T(st, "idx", [128, LK], F32)
        junkb = T(st, "junkb", [128, LK], BF16)
        rl = [T(st, "rl%d" % i, [128, 512], F32) for i in range(2)]
        maskb = T(st, "maskb", [128, LK], BF16)
        biasTT = [T(st, "biasT%d" % i, [128, NKB, 128], BF16) for i in range(2)]
        PT = [T(st, "PT%d" % i, [128, 512], BF16) for i in range(4)]
        bis = T(st, "bis", [128, 8 + 3 * NIT], F32)
        rec = T(st, "rec", [128, 8], F32)
        oT_sb = T(st, "oT_sb", [65, 1024], F32)
        odsa = T(st, "odsa", [128, 512], BF16)
        odT = T(st, "odT", [128, 4, 128], BF16)
        m1 = T(st, "m1", [128, D], F32)
        m2 = T(st, "m2", [128, D], F32)
        mgb = T(st, "mgb", [128, D], BF16)
        mgT = T(st, "mgT", [128, 8, 128], BF16)
        pre = T(st, "pre", [128, D], F32)
        h1 = T(st, "h1", [128, D], F32)
        h1b = T(st, "h1b", [128, D], BF16)
        h1T = T(st, "h1T", [128, 8, 128], F32)
        lg = T(st, "lg", [128, N_EXP], F32)
        mx8 = T(st, "mx8", [128, 8], F32)
        ix8 = T(st, "ix8", [128, 8], U32)
        rt = T(st, "rt", [128, 64], F32)
        mask32 = T(st, "mask32", [128, N_EXP], BF16)
        rk32 = T(st, "rk32", [128, N_EXP], F32)
        junk32 = T(st, "junk32", [128, N_EXP], F32)
        sloti = T(st, "sloti", [128, 4], I32)
        pl = T(st, "pl", [128, 4, 4], F32)
        bigA = P(st, "bigA", [128, 1024], F32)
        bigB = P(st, "bigB", [128, 1024], F32)
        b1 = [P(st, "b1_%d" % i, [128, 512], F32) for i in range(2)]
        bh = [P(st, "bh_%d" % i, [128, 8, 128], BF16) for i in range(2)]
        rr = {'b1': 0, 'bh': 0, 'rl': 0, 'pt': 0, 'ab': 0}

        def nb1():
            i = rr['b1'] % 2; rr['b1'] += 1
            return b1[i], 'b1_%d' % i

        def nbh():
            i = rr['bh'] % 2; rr['bh'] += 1
            return bh[i], 'bh_%d' % i

        stop = cfg.get('p2stop', 99)
        ulim = cfg.get('p2units', 10 ** 9)
        ucount = [0]

        def unit(ti, c0, nq, kind, sq, j, first, last, tokbase, RX, RY, par):
            S = RY
            iqT_t = iqT_tt[par]; iw_t = iw_tt[par]; biasT = biasTT[par]
            iqn, iwn, bTn = 'iqT_t%d' % par, 'iw_t%d' % par, 'biasT%d' % par
            L = 128 * (j + 1) if kind == 'p' else 1088
            v4 = lambda buf: buf[:, 0:4 * nq].rearrange("p (c t) -> p c t", c=4)
            sc4 = lambda ap: ap.rearrange("p (c t) -> p c t", c=4)
            def ldT(dst, src, name):
                S.dma('sp', v4(dst), src[ti].rearrange("p (c t) -> p c t", c=4)[:, :, c0:c0 + nq], (), [name])
            ldT(rqT_t, G['s_rqT'], 'rqT_t'); ldT(rkT_t, G['s_rkT'], 'rkT_t')
            ldT(aqT_t, G['s_aqT'], 'aqT_t')
            S = RX
            ldT(iqT_t, G['s_iqT'], iqn)
            S.dma('sp', iw_t[0:nq, :], G['s_iw'][ti, c0:c0 + nq, :], (), [iwn])
            S = RY
            rows = slice(c0, c0 + nq)
            S.dma('sp', rkz_t[0:nq, :], G['s_rkz'][ti, rows, :], (), ['rkz_t'])
            S.dma('sp', rv_t[0:nq, :], G['s_rv'][ti, rows, :], (), ['rv_t'])
            S.dma('sp', srg_t[0:nq, :], G['s_srg'][ti, rows, :], (), ['srg_t'])
            S.dma('sp', sgr_t[0:nq, :], G['s_sgr'][ti, rows, :], (), ['sgr_t'])
            S.dma('sp', sga_t[0:nq, :], G['s_sga'][ti, rows, :], (), ['sga_t'])
            if kind == 'p':
                S.dma('sp', x_t[0:nq, :], G['xp'][sq, j * 128:(j + 1) * 128, :], (), ['x_t'])
            else:
                S.dma('sp', x_t[0:nq, :], G['xs'][sq], (), ['x_t'])
            if first:
                if kind == 'p':
                    S.memset('pool', S32[:], 0.0, ['S32'])
                    S.memset('pool', Sb[:], 0.0, ['Sb'])
                else:
                    S.dma('sp', S32[:], G['state_ret'][sq].rearrange("h p e -> p h e"), (), ['S32'])
                    S.copy('pool', Sb[:], S32[:], ['S32'], ['Sb'])
                    S.dma('pool', kc[:], G['cache_k'][sq].rearrange("(b p) f -> p b f", p=128), (), ['kc'])
                    ckv = G['cache_ik'][sq].rearrange("(b p) f -> p b f", p=128)
                    RX.dma('pool', ikc[:, :, 0, :], ckv, (), ['ikc'])
                    RX.dma('pool', ikc[:, :, 1, :], ckv, (), ['ikc'])
                    cvv = G['cache_v'][sq].rearrange("(b p) (g d) -> p b g d", p=128, g=2)
                    for g in range(2):
                        S.dma('pool', VA[:, 0:8, g, 0:64], cvv[:, :, g, :], (), ['VA'])
                    for (src, sname, dst, dname) in ((kc, 'kc', KT, 'KT'), (ikc, 'ikc', IKT, 'IKT')):
                        bb, bn = nbh()
                        RR = RY if sname == 'kc' else RX
                        for c in range(8):
                            sv = src[:, c, :] if sname == 'kc' else src[:, c, :, :].rearrange("p a d -> p (a d)")
                            RR.tr(bb[:, c, :], sv, identb[:], [sname, 'identb'], [bn])
                        RR.copy('act', dst[:, 0:1024].rearrange("p (c t) -> p c t", c=8), bb[:], [bn], [dname])
            if kind == 'p':
                kcol, kblk, kn_new = j * 128, j, 128
            else:
                kcol, kblk, kn_new = 1024, 8, 64
            S.dma('sp', KT[:, kcol:kcol + nq], G['s_kT'][ti][:, c0:c0 + nq], (), ['KT'])
            RX.dma('sp', IKT[:, kcol:kcol + nq], G['s_ikT'][ti][:, c0:c0 + nq], (), ['IKT'])
            S.dma('sp', VA[0:nq, kblk, :, :], G['s_v'][ti, rows, :].rearrange("p (g d) -> p g d", g=2), (), ['VA'])

            bk, bkn = bigB, 'bigB'
            for h in range(4):
                S.mm(bk[0:nq, h * nq:(h + 1) * nq], v4(rkT_t)[:, h, :], v4(rqT_t)[:, h, :], True, True,
                     ['rkT_t', 'rqT_t'], [bkn])
            dec = dec128 if nq == 128 else dec64
            decn = 'dec128' if nq == 128 else 'dec64'
            xi = xi128 if nq == 128 else xi64
            xin = 'xi128' if nq == 128 else 'xi64'
            S.tt('dve', scTd[0:nq, 0:4 * nq], bk[0:nq, 0:4 * nq], dec[0:nq, 0:4 * nq], ALU.mult, [bkn, decn], ['scTd'])
            S.tt('pool', rqxi[:, 0:4 * nq], rqT_t[:, 0:4 * nq], xi[:, 0:4 * nq], ALU.mult, ['rqT_t', xin], ['rqxi'])
            for h in range(4):
                osl = bigA[0:nq, h * 256:(h + 1) * 256]
                S.mm(osl, scTd[0:nq, h * nq:(h + 1) * nq], rv_t[0:nq, h * 256:(h + 1) * 256], True, False,
                     ['scTd', 'rv_t'], ['bigA_lo', 'bigA_hi'])
                S.mm(osl, v4(rqxi)[:, h, :], Sb[:, h, :], False, True, ['rqxi', 'Sb'], ['bigA_lo', 'bigA_hi'])
            gT = g128 if nq == 128 else g64
            for h in range(4):
                S.mm(bigB[:, h * 256:(h + 1) * 256], rkz_t[0:nq, h * 128:(h + 1) * 128], rv_t[0:nq, h * 256:(h + 1) * 256],
                     True, True, ['rkz_t', 'rv_t'], ['bigB'])
            for h in range(4):
                S.stt('dve', S32[:, h, :], S32[:, h, :], gT[h], bigB[:, h * 256:(h + 1) * 256], ALU.mult, ALU.add,
                      ['S32', 'bigB'], ['S32'])
            S.copy('pool', Sb[:], S32[:], ['S32'], ['Sb'])
            if last:
                dst = G['rs_p'][sq] if kind == 'p' else G['rs_s'][sq]
                S.dma('sp', dst.rearrange("h p e -> p h e"), S32[:], ['S32'])
            rtail = []

            def _gn1():
                for h in range(4):
                    hs = slice(h * 256, (h + 1) * 256)
                    S.act(o_sb[0:nq, hs], bigA[0:nq, hs], AF.Identity, ['bigA_lo', 'bigA_hi'], ['o_sb'], accum=stt_[0:nq, h:h + 1])
            rtail.append(_gn1)

            def _gn2():
                for h in range(4):
                    hs = slice(h * 256, (h + 1) * 256)
                    S.act(junkf[0:nq, hs], o_sb[0:nq, hs], AF.Square, ['o_sb'], ['junkf', 'stt_'], accum=stt_[0:nq, 4 + h:5 + h])
            rtail.append(_gn2)

            def _gn3():
                S.ts('dve', stt_[0:nq, 8:12], stt_[0:nq, 0:4], 1.0 / 256, None, ALU.mult, None, ['o_sb', 'stt_'], ['stt_'])
                S.tt('dve', stt_[0:nq, 12:16], stt_[0:nq, 8:12], stt_[0:nq, 8:12], ALU.mult, ['stt_'], ['stt_'])
                S.stt('dve', stt_[0:nq, 16:20], stt_[0:nq, 4:8], 1.0 / 256, stt_[0:nq, 12:16], ALU.mult, ALU.subtract, ['stt_'], ['stt_'])
                S.ts('dve', stt_[0:nq, 16:20], stt_[0:nq, 16:20], 1e-6, None, ALU.add, None, ['stt_'], ['stt_'])
                S.act(stt_[0:nq, 16:20], stt_[0:nq, 16:20], AF.Sqrt, ['stt_'], ['stt_'])
            rtail.append(_gn3)

            def _gn4():
                S.op('dve', 'reciprocal', ['stt_'], ['stt_'], out=stt_[0:nq, 16:20], in_=stt_[0:nq, 16:20])
                S.stt('dve', stt_[0:nq, 20:24], stt_[0:nq, 8:12], -1.0, stt_[0:nq, 16:20], ALU.mult, ALU.mult, ['stt_'], ['stt_'])
            rtail.append(_gn4)

            def _gn5():
                for h in range(4):
                    hs = slice(h * 256, (h + 1) * 256)
                    S.act(o_sb[0:nq, hs], o_sb[0:nq, hs], AF.Identity, ['o_sb', 'stt_', 'junkf'], ['o_sb'],
                          bias=stt_[0:nq, 20 + h:21 + h], scale=stt_[0:nq, 16 + h:17 + h])
                S.tt('pool', retb[0:nq, :], o_sb[0:nq, :], srg_t[0:nq, :], ALU.mult, ['o_sb', 'srg_t'], ['retb'])
            rtail.append(_gn5)

            def _gn6():
                bb, bn = nbh()
                for c in range(8):
                    S.tr(bb[:, c, 0:nq], retb[0:nq, c * 128:(c + 1) * 128], identb[0:nq, 0:nq], ['retb', 'identb'], [bn])
                S.copy('act', retT[:, :, 0:nq], bb[:, :, 0:nq], [bn], ['retT'])
            rtail.append(_gn6)

            def _gn7():
                for cb in range(2):
                    for k in range(8):
                        S.mm(bigA[0:nq, cb * 512:(cb + 1) * 512], retT[:, k, 0:nq], wro[:, k, cb * 512:(cb + 1) * 512],
                             k == 0, k == 7, ['retT', 'wro'], ['bigA_lo', 'bigA_hi'])
            rtail.append(_gn7)

            def _gn8():
                S.tt('dve', m1[0:nq, :], bigA[0:nq, :], sgr_t[0:nq, :], ALU.mult, ['bigA_lo', 'bigA_hi', 'sgr_t'], ['m1'])
            rtail.append(_gn8)
            while rtail:
                rtail.pop(0)()
            S = RX
            nb5 = (L + 511) // 512
            for kb5 in range(nb5):
                off = kb5 * 512
                w = min(512, L - off)
                for h in range(8):
                    c, hf = h // 2, h % 2
                    ps_, psn = nb1()
                    S.mm(ps_[0:nq, 0:w], v4(iqT_t)[hf * 64:(hf + 1) * 64, c, :], IKT[hf * 64:(hf + 1) * 64, off:off + w],
                         True, True, [iqn, 'IKT'], [psn])
                    i = rr['rl'] % 2; rr['rl'] += 1
                    S.act(rl[i][0:nq, 0:w], ps_[0:nq, 0:w], AF.Relu, [psn], ['rl%d' % i])
                    if h == 0:
                        S.ts('dve', idx[0:nq, off:off + w], rl[i][0:nq, 0:w], iw_t[0:nq, 0:1], None, ALU.mult, None,
                             ['rl%d' % i, iwn], ['idx'])
                    else:
                        S.stt('dve', idx[0:nq, off:off + w], rl[i][0:nq, 0:w], iw_t[0:nq, h:h + 1], idx[0:nq, off:off + w],
                              ALU.mult, ALU.add, ['rl%d' % i, iwn, 'idx'], ['idx'])
            S.op('dve', 'tensor_reduce', ['idx'], ['bis'], out=bis[0:nq, 0:1], in_=idx[0:nq, 0:L], axis=AX.X, op=ALU.max,
                 apply_absolute_value=True)
            if kind == 'p':
                S.memset('pool', idx[0:64, L - 64:L], NEG, ['idx'])
            S.ts('dve', bis[0:nq, 5:6], bis[0:nq, 0:1], 1.001, 1e-6, ALU.mult, ALU.add, ['bis'], ['bis'])
            S.ts('dve', bis[0:nq, 8:8 + NIT], pow2[0:nq, :], bis[0:nq, 5:6], None, ALU.mult, None, ['bis', 'pow2'], ['bis'])
            S.ts('dve', bis[0:nq, 8 + NIT:8 + 2 * NIT], bis[0:nq, 8:8 + NIT], 2.0, None, ALU.mult, None, ['bis'], ['bis'])
            S.ts('dve', bis[0:nq, 8 + 2 * NIT:8 + 3 * NIT], bis[0:nq, 8:8 + NIT], -1.0, None, ALU.mult, None, ['bis'], ['bis'])
            S.memset('dve', bis[0:nq, 2:3], 0.0, ['bis'])
            for it in range(NIT):
                S.ts('dve', junkb[0:nq, 0:L], idx[0:nq, 0:L], bis[0:nq, 2:3], 0.0, ALU.is_ge, ALU.add, ['idx', 'bis'],
                     ['junkb', 'bis'], accum=bis[0:nq, 3:4])
                S.ts('dve', bis[0:nq, 4:5], bis[0:nq, 3:4], 255.5, bis[0:nq, 8 + NIT + it:9 + NIT + it], ALU.is_ge, ALU.mult,
                     ['bis'], ['bis'])
                S.stt('dve', bis[0:nq, 2:3], bis[0:nq, 4:5], bis[0:nq, 8 + 2 * NIT + it:9 + 2 * NIT + it], bis[0:nq, 2:3],
                      ALU.add, ALU.add, ['bis'], ['bis'])
            S.ts('dve', bis[0:nq, 1:2], bis[0:nq, 2:3], bis[0:nq, 8 + NIT - 1:8 + NIT], None, ALU.subtract, None, ['bis'], ['bis'])
            S.ts('dve', maskb[0:nq, 0:L], idx[0:nq, 0:L], bis[0:nq, 1:2], None, ALU.is_ge, None, ['idx', 'bis'], ['maskb'])
            nkb = (L + 127) // 128
            kb = 0
            while kb < nkb:
                grp = []
                while kb < nkb and len(grp) < 8:
                    kn = min(128, L - kb * 128)
                    if grp and kn != 128:
                        break
                    grp.append((kb, kn)); kb += 1
                    if kn != 128:
                        break
                bb, bn = nbh()
                for gi, (kbi, kn) in enumerate(grp):
                    S.tr(bb[0:kn, gi, 0:nq], maskb[0:nq, kbi * 128:kbi * 128 + kn], identb[0:nq, 0:nq], ['maskb', 'identb'], [bn])
                kn = grp[0][1]
                S.copy('dve', biasT[0:kn, grp[0][0]:grp[0][0] + len(grp), 0:nq], bb[0:kn, 0:len(grp), 0:nq], [bn], [bTn])
            if L % 128:
                S.memset('pool', biasT[64:128, nkb - 1, 0:nq], 0.0, [bTn])
            S = RY
            abanks = [(bigA[:, 0:512], 'bigA_lo'), (bigA[:, 512:1024], 'bigA_hi')]
            kn = 128
            steps = [(kbi, g) for kbi in range(nkb) for g in range(2)]
            prev = None

            def _pv(kbi, g, i):
                if not cfg.get('noPV'):
                    S.mm(bigB[0:65, g * 512:g * 512 + 4 * nq], VA[0:kn, kbi, g, :], PT[i][0:kn, 0:4 * nq],
                         kbi == 0, kbi == nkb - 1, ['PT%d' % i, 'VA'], ['bigB'])
            for (kbi, g) in steps:
                ps_, psn = abanks[rr['ab'] % 2]; rr['ab'] += 1
                gs = slice(g * 64, (g + 1) * 64)
                S.mm(sc4(ps_[0:kn, 0:4 * nq]), KT[gs, kbi * 128:kbi * 128 + kn], v4(aqT_t)[gs, :, :], True, True,
                     ['KT', 'aqT_t'], [psn])
                i = rr['pt'] % 4; rr['pt'] += 1
                S.act(PT[i][0:kn, 0:4 * nq], ps_[0:kn, 0:4 * nq], AF.Exp, [psn], ['PT%d' % i])
                S.tt('dve', sc4(PT[i][0:kn, 0:4 * nq]), sc4(PT[i][0:kn, 0:4 * nq]),
                     biasT[0:kn, kbi, 0:nq].unsqueeze(1).broadcast_to([kn, 4, nq]), ALU.mult, ['PT%d' % i, bTn], ['PT%d' % i])
                if prev is not None:
                    _pv(*prev)
                prev = (kbi, g, i)
            _pv(*prev)
            for g in range(2):
                S.copy('act', oT_sb[0:65, g * 512:g * 512 + 4 * nq], bigB[0:65, g * 512:g * 512 + 4 * nq], ['bigB'], ['oT_sb'])
            for g in range(2):
                for r in range(4):
                    S.tr(bigA[0:nq, g * 512 + r * 65:g * 512 + (r + 1) * 65], oT_sb[0:65, g * 512 + r * nq:g * 512 + (r + 1) * nq],
                         identf[0:65, 0:65], ['oT_sb', 'identf'], ['bigA_lo', 'bigA_hi'])
            for g in range(2):
                pg = bigA[0:nq, g * 512:g * 512 + 260].rearrange("p (r d) -> p r d", r=4)
                S.op('dve', 'reciprocal', ['bigA_lo', 'bigA_hi'], ['rec'], out=rec[0:nq, g * 4:(g + 1) * 4].unsqueeze(2), in_=pg[:, :, 64:65])
                S.tt('dve', odsa[0:nq, g * 256:(g + 1) * 256].rearrange("p (r d) -> p r d", r=4), pg[:, :, 0:64],
                     rec[0:nq, g * 4:(g + 1) * 4].unsqueeze(2).broadcast_to([nq, 4, 64]), ALU.mult, ['bigA_lo', 'bigA_hi', 'rec'], ['odsa'])
            bb, bn = nbh()
            for c in range(4):
                S.tr(bb[:, c, 0:nq], odsa[0:nq, c * 128:(c + 1) * 128], identb[0:nq, 0:nq], ['odsa', 'identb'], [bn])
            S.copy('act', odT[:, :, 0:nq], bb[:, 0:4, 0:nq], [bn], ['odT'])
            for cb in range(2):
                for k in range(4):
                    S.mm(bigB[0:nq, cb * 512:(cb + 1) * 512], odT[:, k, 0:nq], wdo[:, k, cb * 512:(cb + 1) * 512],
                         k == 0, k == 3, ['odT', 'wdo'], ['bigB'])
            S.tt('dve', m2[0:nq, :], bigB[0:nq, :], sga_t[0:nq, :], ALU.mult, ['bigB', 'sga_t'], ['m2'])
            S.tt('pool', mgb[0:nq, :], m1[0:nq, :], m2[0:nq, :], ALU.add, ['m1', 'm2'], ['mgb'])
            bb, bn = nbh()
            for c in range(8):
                S.tr(bb[:, c, 0:nq], mgb[0:nq, c * 128:(c + 1) * 128], identb[0:nq, 0:nq], ['mgb', 'identb'], [bn])
            S.copy('act', mgT[:, :, 0:nq], bb[:, :, 0:nq], [bn], ['mgT'])
            for cb in range(2):
                for k in range(8):
                    S.mm(bigA[0:nq, cb * 512:(cb + 1) * 512], mgT[:, k, 0:nq], wo[:, k, cb * 512:(cb + 1) * 512],
                         k == 0, k == 7, ['mgT', 'wo'], ['bigA_lo', 'bigA_hi'])
            S.stt('dve', pre[0:nq, :], x_t[0:nq, :], float(ALPHA), bigA[0:nq, :], ALU.mult, ALU.add, ['x_t', 'bigA_lo', 'bigA_hi'], ['pre'])
            ln_apply(S, nq, pre, 'pre', junkf, 'junkf', stt_, 'stt_', l1g, 'l1g', l1b, 'l1b', h1, 'h1')
            S.dma('sp', G['s_h1'][tokbase:tokbase + nq, :], h1[0:nq, :], ['h1'])
            S.copy('act', h1b[0:nq, :], h1[0:nq, :], ['h1'], ['h1b'])
            S.dma('sp', G['s_h1b'][tokbase:tokbase + nq, :], h1b[0:nq, :], ['h1b'])

        class Rec:
            def __init__(self, real):
                self.real = real
                self.q = []

            def __getattr__(self, name):
                f = getattr(self.real, name)

                def w(*a, **k):
                    self.q.append((f, a, k))
                return w

        ulist = []
        for s_ in range(NPS):
            for j in range(JT):
                ulist.append((s_ * JT + j, 0, 128, 'p', s_, j, j == 0, j == JT - 1, s_ * SEQ + j * 128))
        for sq in range(NSS):
            ulist.append((NTP + sq // 2, (sq % 2) * 64, 64, 's', sq, 0, True, True, NPS * SEQ + sq * 64))
        recs = []
        for ui, uargs in enumerate(ulist):
            RX, RY = Rec(S), Rec(S)
            unit(*uargs, RX, RY, ui % 2)
            recs.append((RX.q, RY.q))

        def run(q):
            for (f, a_, k_) in q:
                f(*a_, **k_)

        run(recs[0][0])
        for ui in range(len(recs)):
            qy = recs[ui][1]
            qx = recs[ui + 1][0] if ui + 1 < len(recs) else []
            ny, nx = len(qy), len(qx)
            ix = 0
            for iy in range(ny):
                run(qy[iy:iy + 1])
                tgt = 0 if (cfg.get('nointerleave') or ulist[ui][3] == 's') else (iy + 1) * nx // ny
                while ix < tgt:
                    run(qx[ix:ix + 1]); ix += 1
            run(qx[ix:])
            S.flush()
        S.barrier()
        S.flush()


def phase2b(nc, S, T, P, cfg, tiles, G):
    C = cfg['C']
    NT = len(tiles)
    identf = G['identf']
    identb = G['identb']
    IOA = bass.IndirectOffsetOnAxis
    nq = 128
    with contextlib.ExitStack() as st:
        rw = T(st, "rw2", [128, 8, N_EXP], F32)
        rb = T(st, "rb2", [128, N_EXP], F32)
        trib = T(st, "trib2", [128, 128], BF16)
        onesb = T(st, "onesb2", [128, 128], BF16)
        iotae = T(st, "iotae2", [128, N_EXP], F32)
        pidx = T(st, "pidx2", [128, 1], F32)
        kconst = T(st, "kconst2", [128, 4], F32)
        tmpc = T(st, "tmpc2", [128, 128], F32)
        carry = T(st, "carry2", [128, N_EXP], F32)
        h1 = [T(st, "h1r%d" % i, [128, D], F32) for i in range(2)]
        hi_ = [T(st, "hir%d" % i, [128, D], BF16) for i in range(2)]
        lo_ = [T(st, "lor%d" % i, [128, D], BF16) for i in range(2)]
        hiT = [T(st, "hiTr%d" % i, [128, 8, 128], BF16) for i in range(2)]
        loT = [T(st, "loTr%d" % i, [128, 8, 128], BF16) for i in range(2)]
        rwh = T(st, "rwh", [128, 8, N_EXP], BF16)
        rwl = T(st, "rwl", [128, 8, N_EXP], BF16)
        lg = [T(st, "lgr%d" % i, [128, N_EXP], F32) for i in range(2)]
        mx8 = [T(st, "mx8r%d" % i, [128, 8], F32) for i in range(2)]
        ix8 = [T(st, "ix8r%d" % i, [128, 8], U32) for i in range(2)]
        rt = [T(st, "rtr%d" % i, [128, 64], F32) for i in range(2)]
        mask32 = [T(st, "mask32r%d" % i, [128, N_EXP], BF16) for i in range(2)]
        rk32 = [T(st, "rk32r%d" % i, [128, N_EXP], F32) for i in range(2)]
        junk32 = T(st, "junk32r", [128, N_EXP], F32)
        sloti = [T(st, "slotir%d" % i, [128, 4], I32) for i in range(2)]
        pl = [T(st, "plr%d" % i, [128, 4, 4], F32) for i in range(2)]
        bhr = [[P(st, "rbh%d_%d" % (i, q), [128, 8, 128], BF16) for q in range(2)] for i in range(2)]
        b1 = [P(st, "rb1_%d" % i, [128, 512], F32) for i in range(2)]
        S.dma('sp', rw[:], G['router_w'].rearrange("(k p) n -> p k n", p=128), (), ['rw'])
        S.dma('sp', rb[:], G['router_b'].partition_broadcast(128)[:, 0, :], (), ['rb'])
        S.copy('act', rwh[:], rw[:], ['rw'], ['rwh'])
        S.tt('dve', rwl[:], rw[:], rwh[:], ALU.subtract, ['rw', 'rwh'], ['rwl'])
        S.op('pool', 'iota', (), ['tmpc'], out=tmpc[:], pattern=[[1, 128]], base=0, channel_multiplier=-1,
             allow_small_or_imprecise_dtypes=True)
        S.ts('dve', trib[:], tmpc[:], 0.0, None, ALU.is_gt, None, ['tmpc'], ['trib'])
        S.memset('pool', onesb[:], 1.0, ['onesb'])
        S.op('pool', 'iota', (), ['iotae'], out=iotae[:], pattern=[[1, N_EXP]], base=0, channel_multiplier=0,
             allow_small_or_imprecise_dtypes=True)
        S.op('pool', 'iota', (), ['pidx'], out=pidx[:], pattern=[[0, 1]], base=0, channel_multiplier=1,
             allow_small_or_imprecise_dtypes=True)
        S.op('pool', 'iota', (), ['kconst'], out=kconst[:], pattern=[[1, 4]], base=0, channel_multiplier=0,
             allow_small_or_imprecise_dtypes=True)
        S.memset('pool', carry[:], 0.0, ['carry'])

        def load(ti):
            S.dma('sp', h1[ti % 2][:], G['s_h1'][ti * 128:(ti + 1) * 128, :], (), [('h1', ti % 2)])

        def tile_thunks(ti):
            b = ti % 2
            tokbase = ti * 128
            H1, LG, MX, IX, RT, M32, RK, SL, PL = h1[b], lg[b], mx8[b], ix8[b], rt[b], mask32[b], rk32[b], sloti[b], pl[b]
            HI, LO, HIT, LOT = hi_[b], lo_[b], hiT[b], loT[b]
            n = lambda x: (x, b)
            ps_, psn = b1[b], n('b1')
            R = n('rt')
            Q = []
            def _t0():
                S.copy('act', HI[:], H1[:], [n('h1')], [n('hi')])
                S.tt('dve', LO[:], H1[:], HI[:], ALU.subtract, [n('h1'), n('hi')], [n('lo')])
                for (src, sn_, dst, dn_, bank, bkn) in ((HI, n('hi'), HIT, n('hiT'), bhr[b][0], n('bhr0')),
                                                      (LO, n('lo'), LOT, n('loT'), bhr[b][1], n('bhr1'))):
                    for k in range(8):
                        S.tr(bank[:, k, :], src[:, k * 128:(k + 1) * 128], identb[:], [sn_, 'identb'], [bkn])
                    S.copy('act', dst[:], bank[:], [bkn], [dn_])
            Q.append(_t0)
            def _t1():
                terms = [(HIT, n('hiT'), rwh, 'rwh'), (HIT, n('hiT'), rwl, 'rwl'), (LOT, n('loT'), rwh, 'rwh')]
                cnt_ = 0
                for (xt_, xn_, w_, wn_) in terms:
                    for k in range(8):
                        S.mm(ps_[:, 0:N_EXP], xt_[:, k, :], w_[:, k, :], cnt_ == 0, cnt_ == 23, [xn_, wn_], [psn])
                        cnt_ += 1
            Q.append(_t1)
            def _t2():
                S.tt('dve', LG[:], ps_[:, 0:N_EXP], rb[:], ALU.add, [psn, 'rb'], [n('lg')])
            Q.append(_t2)
            def _t3():
                S.op('dve', 'max', [n('lg')], [n('mx8')], out=MX[:], in_=LG[:])
            Q.append(_t3)
            def _t4():
                S.op('dve', 'max_index', [n('lg'), n('mx8')], [n('ix8')], out=IX[:], in_max=MX[:], in_values=LG[:])
            Q.append(_t4)
            def _t5():
                S.ts('dve', RT[:, 0:1], MX[:, 0:1], -1.0, None, ALU.mult, None, [n('mx8')], [R])
            Q.append(_t5)
            def _t6():
                S.act(RT[:, 1:5], MX[:, 0:4], AF.Exp, [n('mx8'), R], [R], bias=RT[:, 0:1], scale=1.0, accum=RT[:, 5:6])
            Q.append(_t6)
            def _t7():
                S.op('dve', 'reciprocal', [R], [R], out=RT[:, 6:7], in_=RT[:, 5:6])
            Q.append(_t7)
            def _t8():
                S.ts('dve', RT[:, 8:12], RT[:, 1:5], RT[:, 6:7], None, ALU.mult, None, [R], [R])
            Q.append(_t8)
            def _t9():
                S.ts('dve', M32[:], LG[:], MX[:, 3:4], None, ALU.is_ge, None, [n('lg'), n('mx8')], [n('mask32')])
            Q.append(_t9)
            def _t10():
                S.mm(ps_[:, 64:64 + N_EXP], trib[:], M32[:], True, True, ['trib', n('mask32'), n('lg')], [psn])
            Q.append(_t10)
            def _t11():
                S.mm(ps_[:, 128:128 + N_EXP], onesb[:], M32[:], True, True, ['onesb', n('mask32')], [psn])
            Q.append(_t11)
            def _t12():
                S.tt('dve', RK[:], ps_[:, 64:64 + N_EXP], carry[:], ALU.add, [psn, 'carry'], [n('rk32')])
                S.tt('dve', carry[:], carry[:], ps_[:, 128:128 + N_EXP], ALU.add, [psn, 'carry', n('rk32')], ['carry'])
            Q.append(_t12)
            def _t13():
                S.copy('dve', RT[:, 12:16], IX[:, 0:4], [n('ix8'), R], [R])
            Q.append(_t13)
            def _t14():
                for k in range(4):
                    S.stt('dve', junk32[:], iotae[:], RT[:, 12 + k:13 + k], RK[:], ALU.is_equal, ALU.mult,
                          ['iotae', R, n('rk32')], ['junk32', R], accum=RT[:, 16 + k:17 + k])
            Q.append(_t14)
            def _t15():
                S.stt('dve', RT[:, 20:24], RT[:, 12:16], float(C), RT[:, 16:20], ALU.mult, ALU.add, [R], [R])
            Q.append(_t15)
            def _t16():
                S.ts('dve', RT[:, 24:28], RT[:, 16:20], float(C) - 0.5, None, ALU.is_ge, None, [R], [R])
            Q.append(_t16)
            def _t17():
                S.ts('dve', RT[:, 32:36], RT[:, 20:24], -1.0, float(N_EXP * C), ALU.mult, ALU.add, [R], [R])
            Q.append(_t17)
            def _t18():
                S.tt('dve', RT[:, 32:36], RT[:, 32:36], RT[:, 24:28], ALU.mult, [R], [R])
            Q.append(_t18)
            def _t19():
                S.tt('dve', RT[:, 20:24], RT[:, 20:24], RT[:, 32:36], ALU.add, [R], [R])
            Q.append(_t19)
            def _t20():
                S.copy('dve', SL[:], RT[:, 20:24], [R], [n('sloti')])
            Q.append(_t20)
            def _t21():
                S.ts('dve', RT[:, 28:29], pidx[:], float(tokbase), None, ALU.add, None, ['pidx', R], [R])
            Q.append(_t21)
            def _t22():
                S.copy('dve', PL[:, :, 0], RT[:, 28:29].broadcast_to([nq, 4]), [R], [n('pl')])
            Q.append(_t22)
            def _t23():
                S.stt('dve', PL[:, :, 1], RT[:, 28:29].broadcast_to([nq, 4]), 4.0, kconst[:], ALU.mult, ALU.add,
                      [R, 'kconst', n('pl')], [n('pl')])
            Q.append(_t23)
            def _t24():
                S.copy('dve', PL[:, :, 2], RT[:, 8:12], [R, n('pl')], [n('pl')])
            Q.append(_t24)
            def _t25():
                S.memset('pool', PL[:, :, 3], 0.0, [n('pl')])
            Q.append(_t25)
            def _t26():
                for k in range(4):
                    S.op('pool', 'indirect_dma_start', [n('pl'), n('sloti')], [('slots', ti, k)], dma=True,
                         out=G['slots'][:, :], out_offset=IOA(ap=SL[:, k:k + 1], axis=0),
                         in_=PL[:, k, :], in_offset=None)
            Q.append(_t26)
            return Q

        for p0 in range(0, NT, 2):
            pair = [t for t in (p0, p0 + 1) if t < NT]
            for t in pair:
                load(t)
            qs = [tile_thunks(t) for t in pair]
            for k in range(max(len(q) for q in qs)):
                for q in qs:
                    if k < len(q):
                        q[k]()
            S.flush()
        S.barrier()
        S.flush()


def ln_apply(S, nq, src, srcn, junk, junkn, st_, stn, g, gn, b, bn, dst, dstn):
    S.act(junk[0:nq, :], src[0:nq, :], AF.Identity, [srcn], [junkn, stn], accum=st_[0:nq, 24:25])
    S.act(junk[0:nq, :], src[0:nq, :], AF.Square, [srcn, junkn], [junkn, stn], accum=st_[0:nq, 25:26])
    S.ts('dve', st_[0:nq, 26:27], st_[0:nq, 24:25], 1.0 / D, None, ALU.mult, None, [stn, junkn], [stn])
    S.tt('dve', st_[0:nq, 27:28], st_[0:nq, 26:27], st_[0:nq, 26:27], ALU.mult, [stn], [stn])
    S.stt('dve', st_[0:nq, 28:29], st_[0:nq, 25:26], 1.0 / D, st_[0:nq, 27:28], ALU.mult, ALU.subtract, [stn], [stn])
    S.ts('dve', st_[0:nq, 28:29], st_[0:nq, 28:29], 1e-5, None, ALU.add, None, [stn], [stn])
    S.act(st_[0:nq, 28:29], st_[0:nq, 28:29], AF.Sqrt, [stn], [stn])
    S.op('dve', 'reciprocal', [stn], [stn], out=st_[0:nq, 28:29], in_=st_[0:nq, 28:29])
    S.stt('dve', st_[0:nq, 29:30], st_[0:nq, 26:27], -1.0, st_[0:nq, 28:29], ALU.mult, ALU.mult, [stn], [stn])
    S.act(junk[0:nq, :], src[0:nq, :], AF.Identity, [srcn, stn, junkn], [junkn], bias=st_[0:nq, 29:30], scale=st_[0:nq, 28:29])
    S.tt('dve', junk[0:nq, :], junk[0:nq, :], g[0:nq, :], ALU.mult, [junkn, gn], [junkn])
    S.tt('pool', dst[0:nq, :], junk[0:nq, :], b[0:nq, :], ALU.add, [junkn, bn], [dstn])


def phase0_init(nc, S, T, P, cfg, G, st):
    C = cfg['C']
    NTOK = G['NTOK']
    nsl = N_EXP * C // 128
    zt_ = T(st, "zero_t", [128, 2048], F32)
    zb_ = T(st, "zero_b", [128, D], BF16)
    sinit = T(st, "sinit", [128, nsl, 4], F32)
    S.memset('pool', zt_[:], 0.0, ['zero_t'])
    S.memset('pool', zb_[:], 0.0, ['zero_b'])
    S.memset('pool', sinit[:], 0.0, ['sinit'])
    S.memset('pool', sinit[:, :, 0], float(NTOK), ['sinit'])
    S.memset('pool', sinit[:, :, 1], float(NTOK * 4), ['sinit'])
    S.dma('act', G['slots'][0:N_EXP * C, :].rearrange("(n p) f -> p n f", p=128), sinit[:], ['sinit'], ['slots_init'])
    S.dma('act', G['s_h1b'][NTOK:NTOK + 128, :], zb_[:], ['zero_b'], ['h1b_init'])
    ykv = G['yk'][0:NTOK * 4, :].rearrange("(n p two) f -> n p (two f)", p=128, two=2)
    G['_ykz'] = [(n, ykv[n], zt_) for n in range(ykv.shape[0])]


def phase3(nc, S, T, P, cfg, tiles, G):
    C = cfg['C']
    NTOK = G['NTOK']
    NB = C // 128
    NCH = (C + 511) // 512
    CW = C // NCH
    identf, identb = G['identf'], G['identb']
    IOA = bass.IndirectOffsetOnAxis
    YSPLIT = cfg.get('YSPLIT', 2)
    with contextlib.ExitStack() as st:
        wu = [T(st, "wu%d" % i, [128, 8, 2 * D_FF], BF16) for i in range(2)]
        wd = [T(st, "wd%d" % i, [128, 8, D], BF16) for i in range(2)]
        bd = [T(st, "bd%d" % i, [1, D], F32) for i in range(2)]
        buT = T(st, "buT", [128, N_EXP * 16], F32)
        bul = T(st, "bul", [128, 4, 128], F32)
        onesf = T(st, "onesf", [1, 128], F32)
        slot_t = [T(st, "slot_t%d" % i, [128, NB, 4], F32) for i in range(4)]
        srci = [T(st, "srci%d" % i, [128, NB], I32) for i in range(4)]
        dsti = [T(st, "dsti%d" % i, [128, 2, NB], I32) for i in range(4)]
        dstf = [T(st, "dstf%d" % i, [128, 2, NB], F32) for i in range(2)]
        xg = [T(st, "xg%d" % i, [128, D], BF16) for i in range(NB)]
        xgT = [T(st, "xgT%d" % i, [128, 8, C], BF16) for i in range(2)]
        NGL = 3
        g_ = [T(st, "g_%d" % i, [128, CW], F32) for i in range(NGL)]
        s_ = [T(st, "s_%d" % i, [128, CW], F32) for i in range(NGL)]
        l_ = [T(st, "l_%d" % i, [128, CW], F32) for i in range(NGL)]
        aT = T(st, "aT", [128, 8, C], BF16)
        yo = [T(st, "yo%d" % i, [128, D], F32) for i in range(3)]
        bigA = P(st, "p3A", [128, 1024], F32)
        bigB = P(st, "p3B", [128, 1024], F32)
        b1 = [P(st, "p3b1_%d" % i, [128, 512], F32) for i in range(2)]
        bh = [P(st, "p3bh_%d" % i, [128, 8, 128], BF16) for i in range(2)]
        banks = [(b1[0][:, :], 'p3b1_0'), (b1[1][:, :], 'p3b1_1'), (bigA[:, 0:512], 'p3A0'), (bigA[:, 512:1024], 'p3A1')]
        rr = {'bk': 0, 'bh': 0, 'xg': 0, 'gl': 0, 'yo': 0, 'ds': 0}

        S.memset('pool', onesf[:], 1.0, ['onesf'])
        buv = G['b_up'].rearrange("e (c p) -> (e c) p", p=128)
        S.dma('sp', bul[:], buv.rearrange("(t r) p -> r t p", r=128), (), ['bul'])
        for t in range(4):
            S.tr(bigB[:, t * 128:(t + 1) * 128], bul[:, t, :], identf[:], ['bul', 'identf'], ['p3B'])
        S.copy('act', buT[:], bigB[:, 0:512], ['p3B'], ['buT'])

        def load_weights(e):
            b = e % 2
            wv = G['w_up'][e].rearrange("(k p) n -> p k n", p=128)
            for hk in range(2):
                S.dma('pool', wu[b][:, hk * 4:(hk + 1) * 4, :], wv[:, hk * 4:(hk + 1) * 4, :], (), [('wu', b, hk)])
            S.dma('pool', wd[b][:], G['w_down'][e].rearrange("(k p) n -> p k n", p=128), (), [('wd', b)])
            S.dma('sp', bd[b][:], G['b_down'][e:e + 1, :], (), [('bd', b)])

        def load_slots(e):
            t4 = e % 4
            S.dma('sp', slot_t[t4][:], G['slots'][e * C:(e + 1) * C, :].rearrange("(n p) f -> p n f", p=128), (), [('slot_t', t4)])
            S.copy('dve', srci[t4][:], slot_t[t4][:, :, 0], [('slot_t', t4)], [('srci', t4)])
            S.ts('dve', dstf[t4 % 2][:, 0, :], slot_t[t4][:, :, 1], 2.0, None, ALU.mult, None, [('slot_t', t4)], [('dstf', t4 % 2)])
            S.ts('dve', dstf[t4 % 2][:, 1, :], slot_t[t4][:, :, 1], 2.0, 1.0, ALU.mult, ALU.add, [('slot_t', t4)], [('dstf', t4 % 2)])
            S.copy('dve', dsti[t4][:], dstf[t4 % 2][:], [('dstf', t4 % 2)], [('dsti', t4)])

        def gathers(e):
            t4 = e % 4
            for blk in range(NB):
                S.op('pool', 'indirect_dma_start', [('srci', t4)], [('xg', blk)], dma=True,
                     out=xg[blk][:, :], out_offset=None, in_=G['s_h1b'][:, :],
                     in_offset=IOA(ap=srci[t4][:, blk:blk + 1], axis=0))

        def xposes(e):
            xb = e % 2
            for blk in range(NB):
                hi = rr['bh'] % 2; rr['bh'] += 1
                for k in range(8):
                    S.tr(bh[hi][:, k, :], xg[blk][:, k * 128:(k + 1) * 128], identb[:], [('xg', blk), 'identb'], ['p3bh_%d' % hi])
                S.copy('act', xgT[xb][:, :, blk * 128:(blk + 1) * 128], bh[hi][:], ['p3bh_%d' % hi], [('xgT', xb, blk)])

        load_weights(0)
        load_slots(0)
        if N_EXP > 1:
            load_slots(1)
        gathers(0)
        xposes(0)
        for e in range(N_EXP):
            b = e % 2
            t4 = e % 4
            xb = e % 2
            if e + 1 < N_EXP:
                load_weights(e + 1)
            if e + 2 < N_EXP:
                load_slots(e + 2)
            if e + 1 < N_EXP:
                gathers(e + 1)
            xgr = [('xgT', xb, blk) for blk in range(NB)]
            for fc in range(8):
                for ch in range(NCH):
                    cs = slice(ch * CW, (ch + 1) * CW)
                    pg, pgn = banks[rr['bk'] % 4]; rr['bk'] += 1
                    plb, pln = banks[rr['bk'] % 4]; rr['bk'] += 1
                    for k in range(8):
                        S.mm(pg[:, 0:CW], wu[b][:, k, fc * 128:(fc + 1) * 128], xgT[xb][:, k, cs], k == 0, k == 7,
                             [('wu', b, k // 4)] + xgr, [pgn])
                    for k in range(8):
                        S.mm(plb[:, 0:CW], wu[b][:, k, D_FF + fc * 128:D_FF + (fc + 1) * 128], xgT[xb][:, k, cs], k == 0, k == 7,
                             [('wu', b, k // 4)] + xgr, [pln])
                    i = rr['gl'] % NGL; rr['gl'] += 1
                    gn_, sn_, ln_ = 'g_%d' % i, 's_%d' % i, 'l_%d' % i
                    S.ts('dve', g_[i][:], pg[:, 0:CW], buT[:, e * 16 + fc:e * 16 + fc + 1], 7.0, ALU.add, ALU.min,
                         [pgn, 'buT'], [gn_])
                    S.act(s_[i][:], g_[i][:], AF.Sigmoid, [gn_], [sn_], scale=1.702)
                    S.act(l_[i][:], plb[:, 0:CW], AF.Identity, [pln, 'buT'], [ln_],
                          bias=buT[:, e * 16 + 8 + fc:e * 16 + 8 + fc + 1], scale=1.0)
                    S.ts('dve', l_[i][:], l_[i][:], -7.0, 7.0, ALU.max, ALU.min, [ln_], [ln_])
                    S.tt('pool', g_[i][:], g_[i][:], s_[i][:], ALU.mult, [gn_, sn_], [gn_])
                    S.stt('dve', aT[:, fc, cs], l_[i][:], 1.0, g_[i][:], ALU.add, ALU.mult, [ln_, gn_], [('aT', fc, ch)])
            if e + 1 < N_EXP:
                xposes(e + 1)
            atr = [('aT', fc, ch) for fc in range(8) for ch in range(NCH)]
            ykh = G['yk'].rearrange("r (two f) -> (r two) f", two=2)
            dsets = [((bigB[:, 0:512], 'p3B'), (bigB[:, 512:1024], 'p3B_hi')),
                     ((bigA[:, 0:512], 'p3A0'), (bigA[:, 512:1024], 'p3A1')),
                     ((b1[0][:, :], 'p3b1_0'), (b1[1][:, :], 'p3b1_1'))]
            for blk in range(NB):
                dset = dsets[rr['ds'] % 3]; rr['ds'] += 1
                i = rr['yo'] % 3; rr['yo'] += 1
                for cb in range(2):
                    osl, osn = dset[cb]
                    for k in range(8):
                        S.mm(osl, aT[:, k, blk * 128:(blk + 1) * 128], wd[b][:, k, cb * 512:(cb + 1) * 512], k == 0, False,
                             atr + [('wd', b)], [osn])
                    S.mm(osl, onesf[0:1, :], bd[b][0:1, cb * 512:(cb + 1) * 512], False, True, ['onesf', ('bd', b)], [osn])
                for cb in range(2):
                    osl, osn = dset[cb]
                    S.act(yo[i][:, cb * 512:(cb + 1) * 512], osl, AF.Copy, [osn, ('slot_t', t4)], [('yo', i, cb)],
                          scale=slot_t[t4][:, blk, 2:3])
                for hc in range(2):
                    S.op('pool', 'indirect_dma_start', [('yo', i, hc), ('dsti', t4)], [('yk', e, blk, hc)], dma=True,
                         out=ykh[:, :], out_offset=IOA(ap=dsti[t4][:, hc, blk:blk + 1], axis=0),
                         in_=yo[i][:, hc * 512:(hc + 1) * 512], in_offset=None)
            S.flush()
        S.barrier()
        S.flush()


def phase4(nc, S, T, P, cfg, tiles, G):
    NPS, SEQ, NSS = cfg['NPS'], cfg['SEQ'], cfg['NSS']
    NT = len(tiles)
    with contextlib.ExitStack() as st:
        l2g = T(st, "l2g", [128, D], F32)
        l2b = T(st, "l2b", [128, D], F32)
        ykt = [T(st, "ykt%d" % i, [128, 4, D], F32) for i in range(3)]
        h1t = [T(st, "h1t%d" % i, [128, D], F32) for i in range(3)]
        sa_l = [T(st, "sa%d" % i, [128, D], F32) for i in range(2)]
        sb_l = [T(st, "sb_%d" % i, [128, D], F32) for i in range(2)]
        pre2_l = [T(st, "pre2%d" % i, [128, D], F32) for i in range(2)]
        junk4_l = [T(st, "junk4%d" % i, [128, D], F32) for i in range(2)]
        st4_l = [T(st, "st4%d" % i, [128, 32], F32) for i in range(2)]
        yout = [T(st, "yout%d" % i, [128, D], F32) for i in range(2)]
        S.dma('sp', l2g[:], G['ln2_g'].partition_broadcast(128)[:, 0, :], (), ['l2g'])
        S.dma('sp', l2b[:], G['ln2_b'].partition_broadcast(128)[:, 0, :], (), ['l2b'])
        ykv = G['yk'][0:G['NTOK'] * 4, :].rearrange("(t k) f -> t k f", k=4)

        def load(ti):
            b = ti % 3
            S.dma('sp', ykt[b][:, 0:2, :], ykv[ti * 128:(ti + 1) * 128, 0:2, :], (), [('ykt', b, 0)])
            S.dma('act', ykt[b][:, 2:4, :], ykv[ti * 128:(ti + 1) * 128, 2:4, :], (), [('ykt', b, 1)])
            S.dma('sp', h1t[b][:], G['s_h1'][ti * 128:(ti + 1) * 128, :], (), ['h1t%d' % b])

        load(0)
        if NT > 1:
            load(1)
        for ti in range(NT):
            kind, a, j = tiles[ti]
            b = ti % 3
            if ti + 2 < NT:
                load(ti + 2)
            yb = ti % 2
            sa, sb_, pre2, junk4, st4 = sa_l[yb], sb_l[yb], pre2_l[yb], junk4_l[yb], st4_l[yb]
            san, sbn, pn, jn, stn4 = 'sa%d' % yb, 'sb_%d' % yb, 'pre2%d' % yb, 'junk4%d' % yb, 'st4%d' % yb
            S.tt('dve', sa[:], ykt[b][:, 0, :], ykt[b][:, 1, :], ALU.add, [('ykt', b, 0)], [san])
            S.tt('dve', sb_[:], ykt[b][:, 2, :], ykt[b][:, 3, :], ALU.add, [('ykt', b, 1)], [sbn])
            S.tt('dve', sa[:], sa[:], sb_[:], ALU.add, [san, sbn], [san])
            S.stt('dve', pre2[:], h1t[b][:], float(ALPHA), sa[:], ALU.mult, ALU.add, ['h1t%d' % b, san], [pn])
            ln_apply(S, 128, pre2, pn, junk4, jn, st4, stn4, l2g, 'l2g', l2b, 'l2b', yout[yb], 'yout%d' % yb)
            if kind == 'p':
                S.dma('act', G['y_p'][a, j * 128:(j + 1) * 128, :], yout[yb][:], ['yout%d' % yb])
            else:
                for hh in range(2):
                    S.dma('act', G['y_s'][2 * a + hh], yout[yb][hh * 64:(hh + 1) * 64, :], ['yout%d' % yb])
        S.barrier()
        S.flush()


def prep_inputs(inputs, cfg, ncores):
    NPS, SEQ, NSS = cfg['NPS'], cfg['SEQ'], cfg['NSS']
    f = lambda a: np.ascontiguousarray(np.asarray(a, dtype=np.float32))
    consts = make_consts(SEQ)
    cp = col_perm()
    w_in = f(inputs['w_in'][0][:, cp])
    hperm = np.concatenate([np.arange(h * 64, (h + 1) * 64) for h in HEAD_ORDER])
    w_dsa_o = f(inputs['w_dsa_o'][0])
    ff = np.concatenate([np.arange(0, 2 * D_FF, 2), np.arange(1, 2 * D_FF, 2)])
    w_up = f(inputs['w_up'][0][:, :, ff])
    b_up = f(inputs['b_up'][0][:, ff])
    shared = dict(
        w_in=w_in, w_ret_o=f(inputs['w_ret_o'][0]), w_dsa_o=w_dsa_o, w_o=f(inputs['w_o'][0]),
        ln1_g=f(inputs['ln1_g']), ln1_b=f(inputs['ln1_b']), ln2_g=f(inputs['ln2_g']), ln2_b=f(inputs['ln2_b']),
        router_w=f(inputs['router_w'][0]), router_b=f(inputs['router_b']),
        w_up=w_up, b_up=b_up, w_down=f(inputs['w_down'][0]), b_down=f(inputs['b_down'][0]),
        c_rotp=consts['rotp'], c_rots=consts['rots'], c_dec128=consts['dec128'], c_dec64=consts['dec64'],
        c_xi128=consts['xi128'], c_xi64=consts['xi64'], c_zp=consts['zp'], c_zs=consts['zs'],
    )
    if cfg.get('phases', 4) < 3:
        for k in ('w_up', 'b_up', 'w_down', 'b_down'):
            shared[k] = np.ascontiguousarray(shared[k][0:1])
    maps = []
    for c in range(ncores):
        m = dict(shared)
        m['xp'] = f(inputs['x_prompt'][c * NPS:(c + 1) * NPS])
        m['xs'] = f(inputs['x_sample'][c * NSS:(c + 1) * NSS])
        m['state_ret'] = f(inputs['state_ret'][0][c * NSS:(c + 1) * NSS])
        m['cache_k'] = f(inputs['cache_k'][0][c * NSS:(c + 1) * NSS]).reshape(NSS, 1024, 128)
        m['cache_v'] = f(inputs['cache_v'][0][c * NSS:(c + 1) * NSS]).reshape(NSS, 1024, 128)
        m['cache_ik'] = f(inputs['cache_idx_k'][0][c * NSS:(c + 1) * NSS])
        maps.append(m)
    return maps


def assemble(results, cfg, ncores):
    NPS, SEQ, NSS = cfg['NPS'], cfg['SEQ'], cfg['NSS']
    cat = lambda k: np.concatenate([np.asarray(r[k]) for r in results], axis=0)
    y_p = cat('y_p'); y_s = cat('y_s')
    rs_p = cat('rs_p')[None]; rs_s = cat('rs_s')[None]
    k_p = cat('k_p').reshape(ncores * NPS, SEQ, 2, 64)[None]
    v_p = cat('v_p').reshape(ncores * NPS, SEQ, 2, 64)[None]
    ik_p = cat('ik_p')[None]
    k_s = cat('k_s').reshape(ncores * NSS, 64, 2, 64)[None]
    v_s = cat('v_s').reshape(ncores * NSS, 64, 2, 64)[None]
    ik_s = cat('ik_s')[None]
    return (y_p, y_s, rs_p, k_p, v_p, ik_p, rs_s, k_s, v_s, ik_s)


def run(inputs, cfg, ncores):
    nc = build(cfg)
    maps = prep_inputs(inputs, cfg, ncores)
    res = run_bass_kernel_spmd(nc, maps, core_ids=list(range(ncores)))
    return res


def kernel(**inputs):
    cfg = dict(NPS=2, SEQ=2048, NSS=4, C=768)
    res = run(inputs, cfg, 8)
    outs = assemble(res.results, cfg, 8)
    return tuple(np.ascontiguousarray(o.astype(np.float32)) for o in outs)
```

```python
import contextlib
import numpy as np
import concourse.bass as bass
import concourse.mybir as mybir
from concourse.bass_utils import run_bass_kernel_spmd

F32 = mybir.dt.float32
BF16 = mybir.dt.bfloat16
I32 = mybir.dt.int32
U32 = mybir.dt.uint32
ALU = mybir.AluOpType
AF = mybir.ActivationFunctionType
AX = mybir.AxisListType

D = 1024
NDS = 32
NEG = -1.0e30
N_EXP = 32
D_FF = 1024
ALPHA = 2.0 ** 0.25
PROJ = 6472


class Sched:
    ENG = ('pe', 'dve', 'act', 'pool', 'sp')
    DMAQ = ('sp', 'act', 'pool')
    ENGOBJ = {'pe': 'tensor', 'dve': 'vector', 'act': 'scalar', 'pool': 'gpsimd', 'sp': 'sync'}

    def __init__(self, nc, stack):
        self.nc = nc
        self.streams = {e: [] for e in self.ENG}
        self.cnt = {e: 0 for e in self.ENG}
        self.dcnt = {q: [0] * NDS for q in self.DMAQ}
        self.drr = {q: 0 for q in self.DMAQ}
        self.pw = {}
        self.last_w = {}
        self.readers = {}
        self.nops = 0
        self.sems = {}
        for e in self.ENG:
            self.sems[('c', e)] = stack.enter_context(nc.semaphore('c_' + e))
        for q in self.DMAQ:
            for j in range(NDS):
                self.sems[('d', q, j)] = stack.enter_context(nc.semaphore('d_%s_%d' % (q, j)))
        self.fence = {e: stack.enter_context(nc.sbuf_tensor('fence_' + e, [128, 2], F32)) for e in ('dve', 'act', 'pool')}

    def op(self, eng, meth, reads=(), writes=(), dma=False, after=(), **kw):
        fn = (meth, kw)
        deps = set(t for t in after if t is not None)

        for r in reads:
            t = self.last_w.get(r)
            if t is not None:
                deps.add(t)
        for w in writes:
            t = self.last_w.get(w)
            if t is not None:
                deps.add(t)
            for t in self.readers.get(w, ()):
                deps.add(t)
        if dma:
            j = self.drr[eng]
            self.drr[eng] = (j + 1) % NDS
            self.dcnt[eng][j] += 1
            tok = (('d', eng, j), 16 * self.dcnt[eng][j])
        else:
            self.cnt[eng] += 1
            tok = (('c', eng), self.cnt[eng])
        need = {}
        for (sk, v) in deps:
            if sk == ('c', 'pe') and eng == 'pe' and not dma:
                continue
            need[sk] = max(need.get(sk, 0), v)
        waits = []
        for sk, v in need.items():
            if self.pw.get((eng, sk), 0) < v:
                waits.append((sk, v))
                self.pw[(eng, sk)] = v
        for r in reads:
            self.readers.setdefault(r, []).append(tok)
        for w in writes:
            self.last_w[w] = tok
            self.readers[w] = []
        self.streams[eng].append((waits, fn, tok, dma))
        self.nops += 1
        if kw.get('accum_out') is not None and not kw.get('_nofence'):
            if eng == 'act':
                return self.op('act', 'memzero', (), writes, ap=self.fence['act'][:, 0:1])
            return self.op(eng, 'memset', (), writes, ap=self.fence[eng][:, 0:1], constant=0.0)
        return tok

    def barrier(self):
        cur = []
        for e in self.ENG:
            if self.cnt[e]:
                cur.append((('c', e), self.cnt[e]))
        for q in self.DMAQ:
            for j in range(NDS):
                if self.dcnt[q][j]:
                    cur.append((('d', q, j), 16 * self.dcnt[q][j]))
        for e in self.ENG:
            waits = []
            for sk, v in cur:
                if sk == ('c', e):
                    continue
                if self.pw.get((e, sk), 0) < v:
                    waits.append((sk, v))
                    self.pw[(e, sk)] = v
            self.streams[e].append((waits, None, None, False))
        self.last_w = {}
        self.readers = {}

    def flush(self):
        nc = self.nc
        sems = self.sems
        with nc.Block() as block:
            def mk(ename):
                stream = self.streams[ename]

                def body(eng):
                    for (waits, fn, tok, dma) in stream:
                        for (sk, v) in waits:
                            eng.wait_ge(sems[sk], v)
                        if fn is not None:
                            try:
                                inst = getattr(eng, fn[0])(**fn[1])
                            except Exception:
                                print("FAILED OP", ename, fn[0], {k: (getattr(v, 'shape', v), getattr(v, 'name', '')) for k, v in fn[1].items()})
                                raise
                            inst.then_inc(sems[tok[0]], 16 if dma else 1)
                return body
            for e in self.ENG:
                getattr(block, self.ENGOBJ[e])(mk(e))
        self.streams = {e: [] for e in self.ENG}

    def tt(self, eng, out, in0, in1, op, r, w):
        return self.op(eng, 'tensor_tensor', r, w, out=out, in0=in0, in1=in1, op=op)

    def ts(self, eng, out, in0, s1, s2, op0, op1, r, w, accum=None):
        kw = dict(out=out, in0=in0, scalar1=s1, scalar2=s2, op0=op0)
        if op1 is not None:
            kw['op1'] = op1
        if accum is not None:
            kw['accum_out'] = accum
        return self.op(eng, 'tensor_scalar', r, w, **kw)

    def stt(self, eng, out, in0, scalar, in1, op0, op1, r, w, accum=None):
        kw = dict(out=out, in0=in0, scalar=scalar, in1=in1, op0=op0, op1=op1)
        if accum is not None:
            kw['accum_out'] = accum
        return self.op(eng, 'scalar_tensor_tensor', r, w, **kw)

    def copy(self, eng, out, in_, r, w):
        if eng == 'act':
            return self.op('act', 'copy', r, w, out=out, in_=in_)
        return self.op(eng, 'tensor_copy', r, w, out=out, in_=in_)

    def act(self, out, in_, func, r, w, bias=None, scale=None, accum=None):
        kw = dict(out=out, in_=in_, func=func)
        if bias is not None:
            kw['bias'] = bias
        if scale is not None:
            kw['scale'] = scale
        if accum is not None:
            kw['accum_out'] = accum
        return self.op('act', 'activation', r, w, **kw)

    def mul(self, out, in_, m, r, w):
        return self.op('act', 'mul', r, w, out=out, in_=in_, mul=m)

    def mm(self, out, lhsT, rhs, start, stop, r, w):
        return self.op('pe', 'matmul', r, w, out=out, lhsT=lhsT, rhs=rhs, start=start, stop=stop)

    def tr(self, out, in_, ident, r, w):
        return self.op('pe', 'transpose', r, w, out=out, in_=in_, identity=ident)

    def dma(self, q, out, in_, r=(), w=()):
        return self.op(q, 'dma_start', r, w, dma=True, out=out, in_=in_)

    def memset(self, eng, ap, val, w):
        return self.op(eng, 'memset', (), w, ap=ap, constant=val)


def _gammas():
    return (1.0 - 2.0 ** (-5.0 - np.arange(4, dtype=np.float32))).astype(np.float32)


def make_consts(seq):
    f32 = np.float32
    ret_f = (np.float32(10000.0) ** (-np.linspace(0.0, 1.0, 64, dtype=f32))).astype(f32)
    att_f = (np.float32(500000.0) ** (-np.arange(0, 16, 2, dtype=f32) / np.float32(16))).astype(f32)

    def rot_table(pos):
        pos = pos.astype(f32)
        a = pos[:, None] * ret_f[None, :]
        b = pos[:, None] * att_f[None, :]
        return np.concatenate([np.cos(a), np.sin(a), np.cos(b), np.sin(b)], axis=1).astype(f32)

    c = {}
    c['rotp'] = rot_table(np.arange(seq))
    c['rots'] = rot_table(1024 + np.arange(64))
    lg = np.log(_gammas()).astype(f32)

    def dec(T):
        pos = np.arange(T, dtype=f32)
        diff = pos[:, None] - pos[None, :]
        d = np.where(diff >= 0, np.exp(np.maximum(diff, 0.0)[None] * lg[:, None, None]), 0.0)
        decT = np.ascontiguousarray(d.transpose(2, 0, 1)).astype(f32)
        xi = np.exp((pos + 1.0)[None, :] * lg[:, None]).astype(f32)
        zeta = np.exp((T - 1.0 - pos)[:, None] * lg[None, :]).astype(f32)
        gT = np.exp(T * lg).astype(f32)
        return decT, xi, zeta, gT

    d128, xi128, z128, g128 = dec(128)
    d64, xi64, z64, g64 = dec(64)
    c['dec128'] = d128.reshape(128, 512)
    c['dec64'] = d64.reshape(64, 256)
    c['xi128'] = np.ascontiguousarray(np.broadcast_to(xi128.reshape(1, 512), (128, 512)))
    c['xi64'] = np.ascontiguousarray(np.broadcast_to(xi64.reshape(1, 256), (128, 256)))
    sc = np.float32(128.0 ** -0.5)
    c['zp'] = (z128 * sc).astype(f32)
    c['zs'] = (np.concatenate([z64, z64], 0) * sc).astype(f32)
    c['g128'] = g128
    c['g64'] = g64
    return c


def col_perm():
    w = [512, 512, 1024, 1024, 512, 128, 128, 512, 64, 8, 1024, 1024]
    off = np.concatenate([[0], np.cumsum(w)])
    rq, rk, rv, rg, aq, ak, av, iq, ik, iw, gr, ga = [np.arange(off[i], off[i + 1]) for i in range(12)]
    aqh = aq.reshape(8, 64)
    aq_new = np.concatenate([aqh[h] for h in (0, 4, 1, 5, 2, 6, 3, 7)])
    return np.concatenate([rq, rk, rv, rg, aq_new, iq, ak, ik, av, iw, gr, ga])


HEAD_ORDER = (0, 4, 1, 5, 2, 6, 3, 7)


def build(cfg):
    NPS, SEQ, NSS = cfg['NPS'], cfg['SEQ'], cfg['NSS']
    dbg = cfg.get('debug', False)
    phases = cfg.get('phases', 4)
    JT = SEQ // 128
    NTP = NPS * JT
    NTS = NSS // 2
    NT = NTP + NTS
    NTOK = NT * 128
    g128 = [float(v) for v in _gammas() ** 128]
    g64 = [float(v) for v in _gammas() ** 64]

    nc = bass.Bass("TRN2", target_bir_lowering=False)

    def din(name, shape, dt=F32):
        return nc.dram_tensor(name, list(shape), dt, kind="ExternalInput").ap()

    def dout(name, shape, dt=F32):
        return nc.dram_tensor(name, list(shape), dt, kind="ExternalOutput").ap()

    def dscr(name, shape, dt):
        if dbg:
            return nc.dram_tensor(name, list(shape), dt, kind="ExternalOutput").ap()
        return nc.dram_tensor(name, list(shape), dt).ap()

    xp = din("xp", [NPS, SEQ, D])
    xs = din("xs", [NSS, 64, D])
    state_ret = din("state_ret", [NSS, 4, 128, 256])
    cache_k = din("cache_k", [NSS, 1024, 128])
    cache_v = din("cache_v", [NSS, 1024, 128])
    cache_ik = din("cache_ik", [NSS, 1024, 64])
    w_in = din("w_in", [D, PROJ])
    w_ret_o = din("w_ret_o", [D, D])
    w_dsa_o = din("w_dsa_o", [512, D])
    w_o = din("w_o", [D, D])
    ln1_g = din("ln1_g", [1, D]); ln1_b = din("ln1_b", [1, D])
    ln2_g = din("ln2_g", [1, D]); ln2_b = din("ln2_b", [1, D])
    router_w = din("router_w", [D, N_EXP]); router_b = din("router_b", [1, N_EXP])
    NE = N_EXP if phases >= 3 else 1
    w_up = din("w_up", [NE, D, 2 * D_FF]); b_up = din("b_up", [NE, 2 * D_FF])
    w_down = din("w_down", [NE, D_FF, D]); b_down = din("b_down", [NE, D])
    c_rotp = din("c_rotp", [SEQ, 144]); c_rots = din("c_rots", [64, 144])
    c_dec128 = din("c_dec128", [128, 512]); c_dec64 = din("c_dec64", [64, 256])
    c_xi128 = din("c_xi128", [128, 512]); c_xi64 = din("c_xi64", [128, 256])
    c_zp = din("c_zp", [128, 4]); c_zs = din("c_zs", [128, 4])

    y_p = dout("y_p", [NPS, SEQ, D]); y_s = dout("y_s", [NSS, 64, D])
    rs_p = dout("rs_p", [NPS, 4, 128, 256]); rs_s = dout("rs_s", [NSS, 4, 128, 256])
    k_p = dout("k_p", [NPS, SEQ, 128]); v_p = dout("v_p", [NPS, SEQ, 128]); ik_p = dout("ik_p", [NPS, SEQ, 64])
    k_s = dout("k_s", [NSS, 64, 128]); v_s = dout("v_s", [NSS, 64, 128]); ik_s = dout("ik_s", [NSS, 64, 64])

    s_rqT = dscr("s_rqT", [NT, 128, 512], BF16)
    s_rkT = dscr("s_rkT", [NT, 128, 512], BF16)
    s_rkz = dscr("s_rkz", [NT, 128, 512], BF16)
    s_rv = dscr("s_rv", [NT, 128, 1024], BF16)
    s_srg = dscr("s_srg", [NT, 128, 1024], BF16)
    s_aqT = dscr("s_aqT", [NT, 128, 512], BF16)
    s_iqT = dscr("s_iqT", [NT, 128, 512], BF16)
    s_iw = dscr("s_iw", [NT, 128, 8], F32)
    s_sgr = dscr("s_sgr", [NT, 128, 1024], F32)
    s_sga = dscr("s_sga", [NT, 128, 1024], F32)
    s_kT = dscr("s_kT", [NT, 128, 128], BF16)
    s_v = dscr("s_v", [NT, 128, 130], BF16)
    s_ikT = dscr("s_ikT", [NT, 128, 128], BF16)
    C = cfg['C']
    s_h1 = dscr("s_h1", [NTOK, D], F32)
    s_h1b = dscr("s_h1b", [NTOK + 128, D], BF16)
    slots = dscr("slots", [N_EXP * C + 128, 4], F32)
    yk = dscr("yk", [NTOK * 4 + 128, D], F32)

    tiles = []
    for s in range(NPS):
        for j in range(JT):
            tiles.append(('p', s, j))
    for u in range(NTS):
        tiles.append(('s', u, 0))

    with contextlib.ExitStack() as gst:
        S = Sched(nc, gst)

        def T(st, name, shape, dt):
            return st.enter_context(nc.sbuf_tensor(name, list(shape), dt))

        def P(st, name, shape, dt):
            return st.enter_context(nc.psum_tensor(name, list(shape), dt))

        identf = T(gst, "identf", [128, 128], F32)
        identb = T(gst, "identb", [128, 128], BF16)
        S.op('pool', 'iota', (), ['identf'], out=identf[:], pattern=[[1, 128]], base=0, channel_multiplier=-1,
             allow_small_or_imprecise_dtypes=True)
        S.ts('dve', identb[:], identf[:], 0.0, None, ALU.is_equal, None, ['identf'], ['identb'])
        S.ts('dve', identf[:], identf[:], 0.0, None, ALU.is_equal, None, ['identf', 'identb'], ['identf'])

        G = locals()
        if phases >= 1:
            phase1(nc, S, T, P, cfg, tiles, G)
        if phases >= 2:
            phase2(nc, S, T, P, cfg, tiles, G)
            phase2b(nc, S, T, P, cfg, tiles, G)
        if phases >= 3:
            phase3(nc, S, T, P, cfg, tiles, G)
        if phases >= 4:
            phase4(nc, S, T, P, cfg, tiles, G)

        S.barrier()
        S.flush()
    return nc


def phase1(nc, S, T, P, cfg, tiles, G):
    NT = len(tiles)
    identf, identb = G['identf'], G['identb']
    xp, xs, w_in = G['xp'], G['xs'], G['w_in']
    with contextlib.ExitStack() as st:
        wsb = T(st, "wsb", [128, 8, PROJ], BF16)
        xf = [T(st, "xf%d" % i, [128, D], F32) for i in range(2)]
        rot = [T(st, "rot%d" % i, [128, 144], F32) for i in range(2)]
        zt = [T(st, "zt%d" % i, [128, 4], F32) for i in range(2)]
        xT = T(st, "xT", [128, 8, 128], BF16)
        zc = [T(st, "zc%d" % i, [128, 512], F32) for i in range(2)]
        tmpa = [T(st, "tmpa%d" % i, [128, 4, 256], F32) for i in range(2)]
        rotd = [T(st, "rotd%d" % i, [128, 512], F32) for i in range(2)]
        qb = [T(st, "qb%d" % i, [128, 512], BF16) for i in range(2)]
        kzb = T(st, "kzb", [128, 512], BF16)
        trs = [T(st, "trs%d" % i, [128, 512], BF16) for i in range(2)]
        rvb = [T(st, "rvb%d" % i, [128, 512], BF16) for i in range(2)]
        sgf = [T(st, "sgf%d" % i, [128, 512], F32) for i in range(2)]
        z8 = T(st, "z8", [128, 328], F32)
        t8 = T(st, "t8", [128, 4, 24], F32)
        kb8 = T(st, "kb8", [128, 256], BF16)
        kTs = T(st, "kTs", [128, 256], BF16)
        vaug = T(st, "vaug", [128, 2, 65], BF16)
        iws = T(st, "iws", [128, 8], F32)
        psT = [P(st, "psT%d" % i, [128, 4, 128], F32) for i in range(2)]
        psz = [P(st, "psz%d" % i, [128, 512], F32) for i in range(4)]
        pstr = [P(st, "pstr%d" % i, [128, 4, 128], BF16) for i in range(2)]

        phase0_init(nc, S, T, P, cfg, G, st)
        wv = w_in.rearrange("(k p) n -> p k n", p=128)
        wbounds = [0, 1024, 2048, 3072, 4096, 4424, 5448, PROJ]
        for gi_ in range(7):
            S.dma('pool', wsb[:, :, wbounds[gi_]:wbounds[gi_ + 1]], wv[:, :, wbounds[gi_]:wbounds[gi_ + 1]], (), [('wsb', gi_)])

        def wgrp(coff):
            for gi_ in range(7):
                if wbounds[gi_] <= coff < wbounds[gi_ + 1]:
                    return gi_
        S.dma('sp', zt[0][:], G['c_zp'], (), ['zt0'])
        S.dma('sp', zt[1][:], G['c_zs'], (), ['zt1'])
        S.memset('pool', vaug[:], 1.0, ['vaug'])

        cnt = {'z': 0, 'tr': 0, 'q': 0, 'g': 0}

        def load_tile(ti):
            kind, a, j = tiles[ti]
            b = ti % 2
            if kind == 'p':
                S.dma('sp', xf[b][:], xp[a, j * 128:(j + 1) * 128, :], (), ['xf%d' % b])
                S.dma('sp', rot[b][:], G['c_rotp'][j * 128:(j + 1) * 128, :], (), ['rot%d' % b])
            else:
                for hh in range(2):
                    S.dma('sp', xf[b][hh * 64:(hh + 1) * 64, :], xs[2 * a + hh], (), ['xf%d' % b])
                    S.dma('sp', rot[b][hh * 64:(hh + 1) * 64, :], G['c_rots'], (), ['rot%d' % b])

        def transposes4(src_bf, dst_ap, srcname, dstname):
            i = cnt['tr'] % 2
            cnt['tr'] += 1
            for c in range(4):
                S.tr(pstr[i][:, c, :], src_bf[:, c * 128:(c + 1) * 128], identb[:], [srcname, 'identb'], ['pstr%d' % i])
            S.copy('act', dst_ap, pstr[i][:], ['pstr%d' % i], [dstname])

        def rotary_full(eng, src, dst, cosb, sinb, tmp, srcname, dstname, tmpname, rotname):
            sv = src.rearrange("p (h t i) -> p h t i", h=4, t=2)
            dv = dst.rearrange("p (h t i) -> p h t i", h=4, t=2)
            x1, x2 = sv[:, :, 0, :], sv[:, :, 1, :]
            ta, tb, tc_, td = tmp[:, :, 0:64], tmp[:, :, 64:128], tmp[:, :, 128:192], tmp[:, :, 192:256]
            S.tt(eng, ta, x1, cosb, ALU.mult, [srcname, rotname], [tmpname + 'a'])
            S.tt(eng, tb, x2, sinb, ALU.mult, [srcname, rotname], [tmpname + 'b'])
            S.tt(eng, tc_, x1, sinb, ALU.mult, [srcname, rotname], [tmpname + 'c'])
            S.tt(eng, td, x2, cosb, ALU.mult, [srcname, rotname], [tmpname + 'd'])
            S.tt(eng, dv[:, :, 0, :], ta, tb, ALU.subtract, [tmpname + 'a', tmpname + 'b'], [dstname + 'lo'])
            S.tt(eng, dv[:, :, 1, :], tc_, td, ALU.add, [tmpname + 'c', tmpname + 'd'], [dstname + 'hi'])

        def rotary_part(eng, buf, nh, cosa, sina, tmp, bufname, tmpname, rotname):
            bv = buf.rearrange("p (h i) -> p h i", h=nh)
            x1, x2 = bv[:, :, 0:8], bv[:, :, 8:16]
            cb = cosa.unsqueeze(1).broadcast_to([128, nh, 8])
            sb = sina.unsqueeze(1).broadcast_to([128, nh, 8])
            v3 = lambda t: t.rearrange("p (h i) -> p h i", h=nh)
            ta, tb, tc_, td = [v3(tmp[:, q, 0:nh * 8]) for q in range(4)]
            S.tt(eng, ta, x1, cb, ALU.mult, [bufname, rotname], [tmpname + 'a'])
            S.tt(eng, tb, x2, sb, ALU.mult, [bufname, rotname], [tmpname + 'b'])
            S.tt(eng, tc_, x1, sb, ALU.mult, [bufname, rotname], [tmpname + 'c'])
            S.tt(eng, td, x2, cb, ALU.mult, [bufname, rotname], [tmpname + 'd'])
            S.tt(eng, x1, ta, tb, ALU.subtract, [tmpname + 'a', tmpname + 'b', tmpname + 'c'], [bufname])
            S.tt(eng, x2, tc_, td, ALU.add, [tmpname + 'c', tmpname + 'd'], [bufname])

        order = ['rq', 'rk', 'rv0', 'rv1', 'rg0', 'rg1', 'aq', 'iq', 'b8', 'gr0', 'gr1', 'ga0', 'ga1']
        pending = []
        load_tile(0)
        for ti in range(NT):
            while pending:
                pending.pop(0)[1]()
            kind, a, j = tiles[ti]
            b = ti % 2
            if ti + 1 < NT:
                load_tile(ti + 1)
            ykz = G['_ykz']
            for _ in range(-(-len(ykz) // max(1, NT - ti))):
                if ykz:
                    n_, ap_, zt__ = ykz.pop(0)
                    S.dma('sp', ap_, zt__[:], ['zero_t'], [('yk_init', n_)])
            xfn, rotn = 'xf%d' % b, 'rot%d' % b
            ztn = 'zt0' if kind == 'p' else 'zt1'
            ztt = zt[0] if kind == 'p' else zt[1]
            for hf in range(2):
                for c in range(4):
                    k = hf * 4 + c
                    S.tr(psT[hf][:, c, :], xf[b][:, k * 128:(k + 1) * 128], identf[:], [xfn, 'identf'], ['psT%d' % hf])
                S.copy('act', xT[:, hf * 4:(hf + 1) * 4, :], psT[hf][:], ['psT%d' % hf], [('xT', hf)])
            cosb = rot[b][:, 0:64].unsqueeze(1).broadcast_to([128, 4, 64])
            sinb = rot[b][:, 64:128].unsqueeze(1).broadcast_to([128, 4, 64])
            cosa = rot[b][:, 128:136]
            sina = rot[b][:, 136:144]
            for blk in range(13):
                name = order[blk]
                wdt = 328 if name == 'b8' else 512
                if blk < 8:
                    coff = blk * 512
                elif name == 'b8':
                    coff = 8 * 512
                else:
                    coff = 8 * 512 + 328 + (blk - 9) * 512
                pz = cnt['z'] % 4
                cnt['z'] += 1
                pzn = 'psz%d' % pz
                if pending and pending[0][0] <= blk:
                    pending.pop(0)[1]()
                for k in range(8):
                    S.mm(psz[pz][:, 0:wdt], xT[:, k, :], wsb[:, k, coff:coff + wdt], k == 0, k == 7,
                         [('xT', k // 4), ('wsb', wgrp(coff))], [pzn])
                if name in ('rq', 'rk'):
                    i = cnt['q'] % 2
                    cnt['q'] += 1
                    S.copy('act', zc[i][:], psz[pz][:], [pzn], ['zc%d' % i])
                    eng = 'dve' if name == 'rq' else 'pool'
                    rotary_full(eng, zc[i][:], rotd[i][:], cosb, sinb, tmpa[i], 'zc%d' % i, 'rotd%d' % i, 'tmpa%d' % i, rotn)
                    rdn = ['rotd%dlo' % i, 'rotd%dhi' % i]
                    trv = trs[i][:].rearrange("p (c t) -> p c t", c=4)
                    if name == 'rq':
                        S.copy('act', qb[i][:], rotd[i][:], rdn, ['qb%d' % i])
                        def _f(i=i, trv=trv, ti=ti):
                            transposes4(qb[i], trv, 'qb%d' % i, 'trs%d' % i)
                            S.dma('sp', G['s_rqT'][ti], trs[i][:], ['trs%d' % i])
                        pending.append((blk + 2, _f))
                    else:
                        S.mul(qb[i][:], rotd[i][:], float(128.0 ** -0.5), rdn, ['qb%d' % i])
                        def _f(i=i, trv=trv, ti=ti):
                            transposes4(qb[i], trv, 'qb%d' % i, 'trs%d' % i)
                            S.dma('sp', G['s_rkT'][ti], trs[i][:], ['trs%d' % i])
                        pending.append((blk + 2, _f))
                        S.tt('dve', kzb[:].rearrange("p (h d) -> p h d", h=4),
                             rotd[i][:].rearrange("p (h d) -> p h d", h=4),
                             ztt[:].unsqueeze(2).broadcast_to([128, 4, 128]), ALU.mult, rdn + [ztn], ['kzb'])
                        S.dma('sp', G['s_rkz'][ti], kzb[:], ['kzb'])
                elif name in ('rv0', 'rv1', 'rg0', 'rg1'):
                    i = cnt['g'] % 2
                    cnt['g'] += 1
                    hh = int(name[-1])
                    if name[:2] == 'rv':
                        S.copy('act', rvb[i][:], psz[pz][:], [pzn], ['rvb%d' % i])
                        dst = G['s_rv']
                    else:
                        S.act(rvb[i][:], psz[pz][:], AF.Silu, [pzn], ['rvb%d' % i])
                        dst = G['s_srg']
                    S.dma('sp', dst[ti, :, hh * 512:(hh + 1) * 512], rvb[i][:], ['rvb%d' % i])
                elif name in ('aq', 'iq'):
                    i = cnt['q'] % 2
                    cnt['q'] += 1
                    S.copy('act', zc[i][:], psz[pz][:], [pzn], ['zc%d' % i])
                    rotary_part('pool', zc[i][:], 8, cosa, sina, tmpa[i], 'zc%d' % i, 'tmpa%d' % i, rotn)
                    S.mul(qb[i][:], zc[i][:], 0.125, ['zc%d' % i], ['qb%d' % i])
                    dst = G['s_aqT'] if name == 'aq' else G['s_iqT']
                    def _f(i=i, ti=ti, dst=dst):
                        transposes4(qb[i], trs[i][:].rearrange("p (c t) -> p c t", c=4), 'qb%d' % i, 'trs%d' % i)
                        S.dma('sp', dst[ti], trs[i][:], ['trs%d' % i])
                    pending.append((blk + 2, _f))
                elif name in ('gr0', 'gr1', 'ga0', 'ga1'):
                    i = cnt['g'] % 2
                    cnt['g'] += 1
                    hh = int(name[-1])
                    dst = G['s_sgr'] if name[:2] == 'gr' else G['s_sga']
                    S.act(sgf[i][:], psz[pz][:], AF.Sigmoid, [pzn], ['sgf%d' % i])
                    S.dma('sp', dst[ti, :, hh * 512:(hh + 1) * 512], sgf[i][:], ['sgf%d' % i])
                else:
                    S.copy('act', z8[:], psz[pz][:, 0:328], [pzn], ['z8'])
                    rotary_part('dve', z8[:, 0:192], 3, cosa, sina, t8, 'z8', 't8', rotn)
                    if kind == 'p':
                        r0 = j * 128
                        S.dma('sp', G['k_p'][a, r0:r0 + 128, :], z8[:, 0:128], ['z8'])
                        S.dma('sp', G['ik_p'][a, r0:r0 + 128, :], z8[:, 128:192], ['z8'])
                        S.dma('sp', G['v_p'][a, r0:r0 + 128, :], z8[:, 192:320], ['z8'])
                    else:
                        for hh in range(2):
                            sl = slice(hh * 64, (hh + 1) * 64)
                            S.dma('sp', G['k_s'][2 * a + hh], z8[sl, 0:128], ['z8'])
                            S.dma('sp', G['ik_s'][2 * a + hh], z8[sl, 128:192], ['z8'])
                            S.dma('sp', G['v_s'][2 * a + hh], z8[sl, 192:320], ['z8'])
                    S.copy('act', kb8[:, 0:128], z8[:, 0:128], ['z8'], ['kb8a'])
                    S.copy('act', kb8[:, 128:192], z8[:, 128:192], ['z8'], ['kb8b'])
                    S.copy('act', kb8[:, 192:256], z8[:, 128:192], ['z8'], ['kb8c'])
                    def _f(ti=ti):
                        i = cnt['tr'] % 2
                        cnt['tr'] += 1
                        for c in range(2):
                            S.tr(pstr[i][:, c, :], kb8[:, c * 128:(c + 1) * 128], identb[:],
                                 ['kb8a', 'kb8b', 'kb8c', 'identb'], ['pstr%d' % i])
                        S.copy('act', kTs[:].rearrange("p (c t) -> p c t", c=2), pstr[i][:, 0:2, :], ['pstr%d' % i], ['kTs'])
                        S.dma('sp', G['s_kT'][ti], kTs[:, 0:128], ['kTs'])
                        S.dma('sp', G['s_ikT'][ti], kTs[:, 128:256], ['kTs'])
                    pending.append((blk + 2, _f))
                    S.copy('act', vaug[:, :, 0:64], z8[:, 192:320].rearrange("p (g d) -> p g d", g=2), ['z8', 'vaug'], ['vaug'])
                    S.dma('sp', G['s_v'][ti], vaug[:].rearrange("p g d -> p (g d)"), ['vaug'])
                    S.mul(iws[:], z8[:, 320:328], float(8.0 ** -0.5), ['z8'], ['iws'])
                    S.dma('sp', G['s_iw'][ti], iws[:], ['iws'])
        while pending:
            pending.pop(0)[1]()
        S.barrier()
        S.flush()


def phase2(nc, S, T, P, cfg, tiles, G):
    NPS, SEQ, NSS = cfg['NPS'], cfg['SEQ'], cfg['NSS']
    C = cfg['C']
    NIT = cfg.get('NIT', 18)
    JT = SEQ // 128
    NTP = NPS * JT
    LK = max(SEQ, 1152)
    NKB = LK // 128
    identf, identb = G['identf'], G['identb']
    gam = _gammas().astype(np.float64)
    g128 = [float(v) for v in gam ** 128]
    g64 = [float(v) for v in gam ** 64]
    IOA = bass.IndirectOffsetOnAxis
    with contextlib.ExitStack() as st:
        wro = T(st, "wro", [128, 8, D], BF16)
        wdo = T(st, "wdo", [128, 4, D], BF16)
        wo = T(st, "wo", [128, 8, D], BF16)
        rw = T(st, "rw", [128, 8, N_EXP], F32)
        rb = T(st, "rb", [128, N_EXP], F32)
        l1g = T(st, "l1g", [128, D], F32)
        l1b = T(st, "l1b", [128, D], F32)
        dec128 = T(st, "dec128", [128, 512], F32)
        dec64 = T(st, "dec64", [64, 256], F32)
        xi128 = T(st, "xi128", [128, 512], F32)
        xi64 = T(st, "xi64", [128, 256], F32)
        pow2 = T(st, "pow2", [128, NIT], F32)
        trib = T(st, "trib", [128, 128], BF16)
        onesb = T(st, "onesb", [128, 128], BF16)
        iotae = T(st, "iotae", [128, N_EXP], F32)
        pidx = T(st, "pidx", [128, 1], F32)
        kconst = T(st, "kconst", [128, 4], F32)
        tmpc = T(st, "tmpc", [128, 128], F32)
        carry = T(st, "carry", [128, N_EXP], F32)
        S.dma('pool', wro[:], G['w_ret_o'].rearrange("(k p) n -> p k n", p=128), (), ['wro'])
        S.dma('pool', wdo[:], G['w_dsa_o'].rearrange("(k p) n -> p k n", p=128), (), ['wdo'])
        S.dma('pool', wo[:], G['w_o'].rearrange("(k p) n -> p k n", p=128), (), ['wo'])
        S.dma('sp', rw[:], G['router_w'].rearrange("(k p) n -> p k n", p=128), (), ['rw'])
        S.dma('sp', rb[:], G['router_b'].partition_broadcast(128)[:, 0, :], (), ['rb'])
        S.dma('sp', l1g[:], G['ln1_g'].partition_broadcast(128)[:, 0, :], (), ['l1g'])
        S.dma('sp', l1b[:], G['ln1_b'].partition_broadcast(128)[:, 0, :], (), ['l1b'])
        S.dma('sp', dec128[:], G['c_dec128'], (), ['dec128'])
        S.dma('sp', dec64[:], G['c_dec64'], (), ['dec64'])
        S.dma('sp', xi128[:], G['c_xi128'], (), ['xi128'])
        S.dma('sp', xi64[:], G['c_xi64'], (), ['xi64'])
        for i in range(NIT):
            S.memset('pool', pow2[:, i:i + 1], float(2.0 ** -(i + 1)), ['pow2'])
        S.op('pool', 'iota', (), ['tmpc'], out=tmpc[:], pattern=[[1, 128]], base=0, channel_multiplier=-1,
             allow_small_or_imprecise_dtypes=True)
        S.ts('dve', trib[:], tmpc[:], 0.0, None, ALU.is_gt, None, ['tmpc'], ['trib'])
        S.memset('pool', onesb[:], 1.0, ['onesb'])
        S.op('pool', 'iota', (), ['iotae'], out=iotae[:], pattern=[[1, N_EXP]], base=0, channel_multiplier=0,
             allow_small_or_imprecise_dtypes=True)
        S.op('pool', 'iota', (), ['pidx'], out=pidx[:], pattern=[[0, 1]], base=0, channel_multiplier=1,
             allow_small_or_imprecise_dtypes=True)
        S.op('pool', 'iota', (), ['kconst'], out=kconst[:], pattern=[[1, 4]], base=0, channel_multiplier=0,
             allow_small_or_imprecise_dtypes=True)
        S.memset('pool', carry[:], 0.0, ['carry'])

        S32 = T(st, "S32", [128, 4, 256], F32)
        Sb = T(st, "Sb", [128, 4, 256], BF16)
        KT = T(st, "KT", [128, LK], BF16)
        VA = T(st, "VA", [128, NKB, 2, 65], BF16)
        IKT = T(st, "IKT", [128, LK], BF16)
        kc = T(st, "kc", [128, 8, 128], BF16)
        ikc = T(st, "ikc", [128, 8, 2, 64], BF16)
        S.memset('pool', VA[:], 1.0, ['VA'])
        S.memset('pool', KT[:], 0.0, ['KT'])
        rqT_t = T(st, "rqT_t", [128, 512], BF16)
        rkT_t = T(st, "rkT_t", [128, 512], BF16)
        rkz_t = T(st, "rkz_t", [128, 512], BF16)
        rv_t = T(st, "rv_t", [128, 1024], BF16)
        srg_t = T(st, "srg_t", [128, 1024], BF16)
        aqT_t = T(st, "aqT_t", [128, 512], BF16)
        iqT_tt = [T(st, "iqT_t%d" % i, [128, 512], BF16) for i in range(2)]
        iw_tt = [T(st, "iw_t%d" % i, [128, 8], F32) for i in range(2)]
        sgr_t = T(st, "sgr_t", [128, 1024], F32)
        sga_t = T(st, "sga_t", [128, 1024], F32)
        x_t = T(st, "x_t", [128, D], F32)
        scTd = T(st, "scTd", [128, 512], BF16)
        rqxi = T(st, "rqxi", [128, 512], BF16)
        o_sb = T(st, "o_sb", [128, 1024], F32)
        junkf = T(st, "junkf", [128, 1024], F32)
        stt_ = T(st, "stt_", [128, 32], F32)
        retb = T(st, "retb", [128, 1024], BF16)
        retT = T(st, "retT", [128, 8, 128], BF16)
        idx = T(st, "idx", [128, LK], F32)
        junkb = T(st, "junkb", [128, LK], BF16)
        rl = [T(st, "rl%d" % i, [128, 512], F32) for i in range(2)]
        maskb = T(st, "maskb", [128, LK], BF16)
        biasTT = [T(st, "biasT%d" % i, [128, NKB, 128], BF16) for i in range(2)]
        PT = [T(st, "PT%d" % i, [128, 512], BF16) for i in range(4)]
        bis = T(st, "bis", [128, 8 + 3 * NIT], F32)
        rec = T(st, "rec", [128, 8], F32)
        oT_sb = T(st, "oT_sb", [65, 1024], F32)
        odsa = T(st, "odsa", [128, 512], BF16)
        odT = T(st, "odT", [128, 4, 128], BF16)
        m1 = T(st, "m1", [128, D], F32)
        m2 = T(st, "m2", [128, D], F32)
        mgb = T(st, "mgb", [128, D], BF16)
        mgT = T(st, "mgT", [128, 8, 128], BF16)
        pre = T(st, "pre", [128, D], F32)
        h1 = T(st, "h1", [128, D], F32)
        h1b = T(st, "h1b", [128, D], BF16)
        h1T = T(st, "h1T", [128, 8, 128], F32)
        lg = T(st, "lg", [128, N_EXP], F32)
        mx8 = T(st, "mx8", [128, 8], F32)
        ix8 = T(st, "ix8", [128, 8], U32)
        rt = T(st, "rt", [128, 64], F32)
        mask32 = T(st, "mask32", [128, N_EXP], BF16)
        rk32 = T(st, "rk32", [128, N_EXP], F32)
        junk32 = T(st, "junk32", [128, N_EXP], F32)
        sloti = T(st, "sloti", [128, 4], I32)
        pl = T(st, "pl", [128, 4, 4], F32)
        bigA = P(st, "bigA", [128, 1024], F32)
        bigB = P(st, "bigB", [128, 1024], F32)
        b1 = [P(st, "b1_%d" % i, [128, 512], F32) for i in range(2)]
        bh = [P(st, "bh_%d" % i, [128, 8, 128], BF16) for i in range(2)]
        rr = {'b1': 0, 'bh': 0, 'rl': 0, 'pt': 0, 'ab': 0}

        def nb1():
            i = rr['b1'] % 2; rr['b1'] += 1
            return b1[i], 'b1_%d' % i

        def nbh():
            i = rr['bh'] % 2; rr['bh'] += 1
            return bh[i], 'bh_%d' % i

        stop = cfg.get('p2stop', 99)
        ulim = cfg.get('p2units', 10 ** 9)
        ucount = [0]

        def unit(ti, c0, nq, kind, sq, j, first, last, tokbase, RX, RY, par):
            S = RY
            iqT_t = iqT_tt[par]; iw_t = iw_tt[par]; biasT = biasTT[par]
            iqn, iwn, bTn = 'iqT_t%d' % par, 'iw_t%d' % par, 'biasT%d' % par
            L = 128 * (j + 1) if kind == 'p' else 1088
            v4 = lambda buf: buf[:, 0:4 * nq].rearrange("p (c t) -> p c t", c=4)
            sc4 = lambda ap: ap.rearrange("p (c t) -> p c t", c=4)
            def ldT(dst, src, name):
                S.dma('sp', v4(dst), src[ti].rearrange("p (c t) -> p c t", c=4)[:, :, c0:c0 + nq], (), [name])
            ldT(rqT_t, G['s_rqT'], 'rqT_t'); ldT(rkT_t, G['s_rkT'], 'rkT_t')
            ldT(aqT_t, G['s_aqT'], 'aqT_t')
            S = RX
            ldT(iqT_t, G['s_iqT'], iqn)
            S.dma('sp', iw_t[0:nq, :], G['s_iw'][ti, c0:c0 + nq, :], (), [iwn])
            S = RY
            rows = slice(c0, c0 + nq)
            S.dma('sp', rkz_t[0:nq, :], G['s_rkz'][ti, rows, :], (), ['rkz_t'])
            S.dma('sp', rv_t[0:nq, :], G['s_rv'][ti, rows, :], (), ['rv_t'])
            S.dma('sp', srg_t[0:nq, :], G['s_srg'][ti, rows, :], (), ['srg_t'])
            S.dma('sp', sgr_t[0:nq, :], G['s_sgr'][ti, rows, :], (), ['sgr_t'])
            S.dma('sp', sga_t[0:nq, :], G['s_sga'][ti, rows, :], (), ['sga_t'])
            if kind == 'p':
                S.dma('sp', x_t[0:nq, :], G['xp'][sq, j * 128:(j + 1) * 128, :], (), ['x_t'])
            else:
                S.dma('sp', x_t[0:nq, :], G['xs'][sq], (), ['x_t'])
            if first:
                if kind == 'p':
                    S.memset('pool', S32[:], 0.0, ['S32'])
                    S.memset('pool', Sb[:], 0.0, ['Sb'])
                else:
                    S.dma('sp', S32[:], G['state_ret'][sq].rearrange("h p e -> p h e"), (), ['S32'])
                    S.copy('pool', Sb[:], S32[:], ['S32'], ['Sb'])
                    S.dma('pool', kc[:], G['cache_k'][sq].rearrange("(b p) f -> p b f", p=128), (), ['kc'])
                    ckv = G['cache_ik'][sq].rearrange("(b p) f -> p b f", p=128)
                    RX.dma('pool', ikc[:, :, 0, :], ckv, (), ['ikc'])
                    RX.dma('pool', ikc[:, :, 1, :], ckv, (), ['ikc'])
                    cvv = G['cache_v'][sq].rearrange("(b p) (g d) -> p b g d", p=128, g=2)
                    for g in range(2):
                        S.dma('pool', VA[:, 0:8, g, 0:64], cvv[:, :, g, :], (), ['VA'])
                    for (src, sname, dst, dname) in ((kc, 'kc', KT, 'KT'), (ikc, 'ikc', IKT, 'IKT')):
                        bb, bn = nbh()
                        RR = RY if sname == 'kc' else RX
                        for c in range(8):
                            sv = src[:, c, :] if sname == 'kc' else src[:, c, :, :].rearrange("p a d -> p (a d)")
                            RR.tr(bb[:, c, :], sv, identb[:], [sname, 'identb'], [bn])
                        RR.copy('act', dst[:, 0:1024].rearrange("p (c t) -> p c t", c=8), bb[:], [bn], [dname])
            if kind == 'p':
                kcol, kblk, kn_new = j * 128, j, 128
            else:
                kcol, kblk, kn_new = 1024, 8, 64
            S.dma('sp', KT[:, kcol:kcol + nq], G['s_kT'][ti][:, c0:c0 + nq], (), ['KT'])
            RX.dma('sp', IKT[:, kcol:kcol + nq], G['s_ikT'][ti][:, c0:c0 + nq], (), ['IKT'])
            S.dma('sp', VA[0:nq, kblk, :, :], G['s_v'][ti, rows, :].rearrange("p (g d) -> p g d", g=2), (), ['VA'])

            bk, bkn = bigB, 'bigB'
            for h in range(4):
                S.mm(bk[0:nq, h * nq:(h + 1) * nq], v4(rkT_t)[:, h, :], v4(rqT_t)[:, h, :], True, True,
                     ['rkT_t', 'rqT_t'], [bkn])
            dec = dec128 if nq == 128 else dec64
            decn = 'dec128' if nq == 128 else 'dec64'
            xi = xi128 if nq == 128 else xi64
            xin = 'xi128' if nq == 128 else 'xi64'
            S.tt('dve', scTd[0:nq, 0:4 * nq], bk[0:nq, 0:4 * nq], dec[0:nq, 0:4 * nq], ALU.mult, [bkn, decn], ['scTd'])
            S.tt('pool', rqxi[:, 0:4 * nq], rqT_t[:, 0:4 * nq], xi[:, 0:4 * nq], ALU.mult, ['rqT_t', xin], ['rqxi'])
            for h in range(4):
                osl = bigA[0:nq, h * 256:(h + 1) * 256]
                S.mm(osl, scTd[0:nq, h * nq:(h + 1) * nq], rv_t[0:nq, h * 256:(h + 1) * 256], True, False,
                     ['scTd', 'rv_t'], ['bigA_lo', 'bigA_hi'])
                S.mm(osl, v4(rqxi)[:, h, :], Sb[:, h, :], False, True, ['rqxi', 'Sb'], ['bigA_lo', 'bigA_hi'])
            gT = g128 if nq == 128 else g64
            for h in range(4):
                S.mm(bigB[:, h * 256:(h + 1) * 256], rkz_t[0:nq, h * 128:(h + 1) * 128], rv_t[0:nq, h * 256:(h + 1) * 256],
                     True, True, ['rkz_t', 'rv_t'], ['bigB'])
            for h in range(4):
                S.stt('dve', S32[:, h, :], S32[:, h, :], gT[h], bigB[:, h * 256:(h + 1) * 256], ALU.mult, ALU.add,
                      ['S32', 'bigB'], ['S32'])
            S.copy('pool', Sb[:], S32[:], ['S32'], ['Sb'])
            if last:
                dst = G['rs_p'][sq] if kind == 'p' else G['rs_s'][sq]
                S.dma('sp', dst.rearrange("h p e -> p h e"), S32[:], ['S32'])
            rtail = []

            def _gn1():
                for h in range(4):
                    hs = slice(h * 256, (h + 1) * 256)
                    S.act(o_sb[0:nq, hs], bigA[0:nq, hs], AF.Identity, ['bigA_lo', 'bigA_hi'], ['o_sb'], accum=stt_[0:nq, h:h + 1])
            rtail.append(_gn1)

            def _gn2():
                for h in range(4):
                    hs = slice(h * 256, (h + 1) * 256)
                    S.act(junkf[0:nq, hs], o_sb[0:nq, hs], AF.Square, ['o_sb'], ['junkf', 'stt_'], accum=stt_[0:nq, 4 + h:5 + h])
            rtail.append(_gn2)

            def _gn3():
                S.ts('dve', stt_[0:nq, 8:12], stt_[0:nq, 0:4], 1.0 / 256, None, ALU.mult, None, ['o_sb', 'stt_'], ['stt_'])
                S.tt('dve', stt_[0:nq, 12:16], stt_[0:nq, 8:12], stt_[0:nq, 8:12], ALU.mult, ['stt_'], ['stt_'])
                S.stt('dve', stt_[0:nq, 16:20], stt_[0:nq, 4:8], 1.0 / 256, stt_[0:nq, 12:16], ALU.mult, ALU.subtract, ['stt_'], ['stt_'])
                S.ts('dve', stt_[0:nq, 16:20], stt_[0:nq, 16:20], 1e-6, None, ALU.add, None, ['stt_'], ['stt_'])
                S.act(stt_[0:nq, 16:20], stt_[0:nq, 16:20], AF.Sqrt, ['stt_'], ['stt_'])
            rtail.append(_gn3)

            def _gn4():
                S.op('dve', 'reciprocal', ['stt_'], ['stt_'], out=stt_[0:nq, 16:20], in_=stt_[0:nq, 16:20])
                S.stt('dve', stt_[0:nq, 20:24], stt_[0:nq, 8:12], -1.0, stt_[0:nq, 16:20], ALU.mult, ALU.mult, ['stt_'], ['stt_'])
            rtail.append(_gn4)

            def _gn5():
                for h in range(4):
                    hs = slice(h * 256, (h + 1) * 256)
                    S.act(o_sb[0:nq, hs], o_sb[0:nq, hs], AF.Identity, ['o_sb', 'stt_', 'junkf'], ['o_sb'],
                          bias=stt_[0:nq, 20 + h:21 + h], scale=stt_[0:nq, 16 + h:17 + h])
                S.tt('pool', retb[0:nq, :], o_sb[0:nq, :], srg_t[0:nq, :], ALU.mult, ['o_sb', 'srg_t'], ['retb'])
            rtail.append(_gn5)

            def _gn6():
                bb, bn = nbh()
                for c in range(8):
                    S.tr(bb[:, c, 0:nq], retb[0:nq, c * 128:(c + 1) * 128], identb[0:nq, 0:nq], ['retb', 'identb'], [bn])
                S.copy('act', retT[:, :, 0:nq], bb[:, :, 0:nq], [bn], ['retT'])
            rtail.append(_gn6)

            def _gn7():
                for cb in range(2):
                    for k in range(8):
                        S.mm(bigA[0:nq, cb * 512:(cb + 1) * 512], retT[:, k, 0:nq], wro[:, k, cb * 512:(cb + 1) * 512],
                             k == 0, k == 7, ['retT', 'wro'], ['bigA_lo', 'bigA_hi'])
            rtail.append(_gn7)

            def _gn8():
                S.tt('dve', m1[0:nq, :], bigA[0:nq, :], sgr_t[0:nq, :], ALU.mult, ['bigA_lo', 'bigA_hi', 'sgr_t'], ['m1'])
            rtail.append(_gn8)
            while rtail:
                rtail.pop(0)()
            S = RX
            nb5 = (L + 511) // 512
            for kb5 in range(nb5):
                off = kb5 * 512
                w = min(512, L - off)
                for h in range(8):
                    c, hf = h // 2, h % 2
                    ps_, psn = nb1()
                    S.mm(ps_[0:nq, 0:w], v4(iqT_t)[hf * 64:(hf + 1) * 64, c, :], IKT[hf * 64:(hf + 1) * 64, off:off + w],
                         True, True, [iqn, 'IKT'], [psn])
                    i = rr['rl'] % 2; rr['rl'] += 1
                    S.act(rl[i][0:nq, 0:w], ps_[0:nq, 0:w], AF.Relu, [psn], ['rl%d' % i])
                    if h == 0:
                        S.ts('dve', idx[0:nq, off:off + w], rl[i][0:nq, 0:w], iw_t[0:nq, 0:1], None, ALU.mult, None,
                             ['rl%d' % i, iwn], ['idx'])
                    else:
                        S.stt('dve', idx[0:nq, off:off + w], rl[i][0:nq, 0:w], iw_t[0:nq, h:h + 1], idx[0:nq, off:off + w],
                              ALU.mult, ALU.add, ['rl%d' % i, iwn, 'idx'], ['idx'])
            S.op('dve', 'tensor_reduce', ['idx'], ['bis'], out=bis[0:nq, 0:1], in_=idx[0:nq, 0:L], axis=AX.X, op=ALU.max,
                 apply_absolute_value=True)
            if kind == 'p':
                S.memset('pool', idx[0:64, L - 64:L], NEG, ['idx'])
            S.ts('dve', bis[0:nq, 5:6], bis[0:nq, 0:1], 1.001, 1e-6, ALU.mult, ALU.add, ['bis'], ['bis'])
            S.ts('dve', bis[0:nq, 8:8 + NIT], pow2[0:nq, :], bis[0:nq, 5:6], None, ALU.mult, None, ['bis', 'pow2'], ['bis'])
            S.ts('dve', bis[0:nq, 8 + NIT:8 + 2 * NIT], bis[0:nq, 8:8 + NIT], 2.0, None, ALU.mult, None, ['bis'], ['bis'])
            S.ts('dve', bis[0:nq, 8 + 2 * NIT:8 + 3 * NIT], bis[0:nq, 8:8 + NIT], -1.0, None, ALU.mult, None, ['bis'], ['bis'])
            S.memset('dve', bis[0:nq, 2:3], 0.0, ['bis'])
            for it in range(NIT):
                S.ts('dve', junkb[0:nq, 0:L], idx[0:nq, 0:L], bis[0:nq, 2:3], 0.0, ALU.is_ge, ALU.add, ['idx', 'bis'],
                     ['junkb', 'bis'], accum=bis[0:nq, 3:4])
                S.ts('dve', bis[0:nq, 4:5], bis[0:nq, 3:4], 255.5, bis[0:nq, 8 + NIT + it:9 + NIT + it], ALU.is_ge, ALU.mult,
                     ['bis'], ['bis'])
                S.stt('dve', bis[0:nq, 2:3], bis[0:nq, 4:5], bis[0:nq, 8 + 2 * NIT + it:9 + 2 * NIT + it], bis[0:nq, 2:3],
                      ALU.add, ALU.add, ['bis'], ['bis'])
            S.ts('dve', bis[0:nq, 1:2], bis[0:nq, 2:3], bis[0:nq, 8 + NIT - 1:8 + NIT], None, ALU.subtract, None, ['bis'], ['bis'])
            S.ts('dve', maskb[0:nq, 0:L], idx[0:nq, 0:L], bis[0:nq, 1:2], None, ALU.is_ge, None, ['idx', 'bis'], ['maskb'])
            nkb = (L + 127) // 128
            kb = 0
            while kb < nkb:
                grp = []
                while kb < nkb and len(grp) < 8:
                    kn = min(128, L - kb * 128)
                    if grp and kn != 128:
                        break
                    grp.append((kb, kn)); kb += 1
                    if kn != 128:
                        break
                bb, bn = nbh()
                for gi, (kbi, kn) in enumerate(grp):
                    S.tr(bb[0:kn, gi, 0:nq], maskb[0:nq, kbi * 128:kbi * 128 + kn], identb[0:nq, 0:nq], ['maskb', 'identb'], [bn])
                kn = grp[0][1]
                S.ts('dve', biasT[0:kn, grp[0][0]:grp[0][0] + len(grp), 0:nq], bb[0:kn, 0:len(grp), 0:nq], 1.0, 30000.0,
                     ALU.subtract, ALU.mult, [bn], [bTn])
            if L % 128:
                S.memset('pool', biasT[64:128, nkb - 1, 0:nq], -30000.0, [bTn])
            S = RY
            abanks = [(bigA[:, 0:512], 'bigA_lo'), (bigA[:, 512:1024], 'bigA_hi')]
            kn = 128
            steps = [(kbi, g) for kbi in range(nkb) for g in range(2)]
            prev = None

            def _pv(kbi, g, i):
                if not cfg.get('noPV'):
                    S.mm(bigB[0:65, g * 512:g * 512 + 4 * nq], VA[0:kn, kbi, g, :], PT[i][0:kn, 0:4 * nq],
                         kbi == 0, kbi == nkb - 1, ['PT%d' % i, 'VA'], ['bigB'])
            for (kbi, g) in steps:
                ps_, psn = abanks[rr['ab'] % 2]; rr['ab'] += 1
                gs = slice(g * 64, (g + 1) * 64)
                S.mm(sc4(ps_[0:kn, 0:4 * nq]), KT[gs, kbi * 128:kbi * 128 + kn], v4(aqT_t)[gs, :, :], True, False,
                     ['KT', 'aqT_t'], [psn])
                S.mm(sc4(ps_[0:kn, 0:4 * nq]), identb[0:kn, 0:kn],
                     biasT[0:kn, kbi, 0:nq].unsqueeze(1).broadcast_to([kn, 4, nq]), False, True,
                     [bTn, 'identb'], [psn])
                i = rr['pt'] % 4; rr['pt'] += 1
                S.act(PT[i][0:kn, 0:4 * nq], ps_[0:kn, 0:4 * nq], AF.Exp, [psn], ['PT%d' % i])
                if prev is not None:
                    _pv(*prev)
                prev = (kbi, g, i)
            _pv(*prev)
            for g in range(2):
                S.copy('act', oT_sb[0:65, g * 512:g * 512 + 4 * nq], bigB[0:65, g * 512:g * 512 + 4 * nq], ['bigB'], ['oT_sb'])
            for g in range(2):
                for r in range(4):
                    S.tr(bigA[0:nq, g * 512 + r * 65:g * 512 + (r + 1) * 65], oT_sb[0:65, g * 512 + r * nq:g * 512 + (r + 1) * nq],
                         identf[0:65, 0:65], ['oT_sb', 'identf'], ['bigA_lo', 'bigA_hi'])
            for g in range(2):
                pg = bigA[0:nq, g * 512:g * 512 + 260].rearrange("p (r d) -> p r d", r=4)
                S.op('dve', 'reciprocal', ['bigA_lo', 'bigA_hi'], ['rec'], out=rec[0:nq, g * 4:(g + 1) * 4].unsqueeze(2), in_=pg[:, :, 64:65])
                S.tt('dve', odsa[0:nq, g * 256:(g + 1) * 256].rearrange("p (r d) -> p r d", r=4), pg[:, :, 0:64],
                     rec[0:nq, g * 4:(g + 1) * 4].unsqueeze(2).broadcast_to([nq, 4, 64]), ALU.mult, ['bigA_lo', 'bigA_hi', 'rec'], ['odsa'])
            bb, bn = nbh()
            for c in range(4):
                S.tr(bb[:, c, 0:nq], odsa[0:nq, c * 128:(c + 1) * 128], identb[0:nq, 0:nq], ['odsa', 'identb'], [bn])
            S.copy('act', odT[:, :, 0:nq], bb[:, 0:4, 0:nq], [bn], ['odT'])
            for cb in range(2):
                for k in range(4):
                    S.mm(bigB[0:nq, cb * 512:(cb + 1) * 512], odT[:, k, 0:nq], wdo[:, k, cb * 512:(cb + 1) * 512],
                         k == 0, k == 3, ['odT', 'wdo'], ['bigB'])
            S.tt('dve', m2[0:nq, :], bigB[0:nq, :], sga_t[0:nq, :], ALU.mult, ['bigB', 'sga_t'], ['m2'])
            S.tt('pool', mgb[0:nq, :], m1[0:nq, :], m2[0:nq, :], ALU.add, ['m1', 'm2'], ['mgb'])
            bb, bn = nbh()
            for c in range(8):
                S.tr(bb[:, c, 0:nq], mgb[0:nq, c * 128:(c + 1) * 128], identb[0:nq, 0:nq], ['mgb', 'identb'], [bn])
            S.copy('act', mgT[:, :, 0:nq], bb[:, :, 0:nq], [bn], ['mgT'])
            for cb in range(2):
                for k in range(8):
                    S.mm(bigA[0:nq, cb * 512:(cb + 1) * 512], mgT[:, k, 0:nq], wo[:, k, cb * 512:(cb + 1) * 512],
                         k == 0, k == 7, ['mgT', 'wo'], ['bigA_lo', 'bigA_hi'])
            S.stt('dve', pre[0:nq, :], x_t[0:nq, :], float(ALPHA), bigA[0:nq, :], ALU.mult, ALU.add, ['x_t', 'bigA_lo', 'bigA_hi'], ['pre'])
            ln_apply(S, nq, pre, 'pre', junkf, 'junkf', stt_, 'stt_', l1g, 'l1g', l1b, 'l1b', h1, 'h1')
            S.dma('sp', G['s_h1'][tokbase:tokbase + nq, :], h1[0:nq, :], ['h1'])
            S.copy('act', h1b[0:nq, :], h1[0:nq, :], ['h1'], ['h1b'])
            S.dma('sp', G['s_h1b'][tokbase:tokbase + nq, :], h1b[0:nq, :], ['h1b'])

        class Rec:
            def __init__(self, real):
                self.real = real
                self.q = []

            def __getattr__(self, name):
                f = getattr(self.real, name)

                def w(*a, **k):
                    self.q.append((f, a, k))
                return w

        ulist = []
        for s_ in range(NPS):
            for j in range(JT):
                ulist.append((s_ * JT + j, 0, 128, 'p', s_, j, j == 0, j == JT - 1, s_ * SEQ + j * 128))
        for sq in range(NSS):
            ulist.append((NTP + sq // 2, (sq % 2) * 64, 64, 's', sq, 0, True, True, NPS * SEQ + sq * 64))
        recs = []
        for ui, uargs in enumerate(ulist):
            RX, RY = Rec(S), Rec(S)
            unit(*uargs, RX, RY, ui % 2)
            recs.append((RX.q, RY.q))

        def run(q):
            for (f, a_, k_) in q:
                f(*a_, **k_)

        run(recs[0][0])
        for ui in range(len(recs)):
            qy = recs[ui][1]
            qx = recs[ui + 1][0] if ui + 1 < len(recs) else []
            ny, nx = len(qy), len(qx)
            ix = 0
            for iy in range(ny):
                run(qy[iy:iy + 1])
                tgt = 0 if (cfg.get('nointerleave') or ulist[ui][3] == 's') else (iy + 1) * nx // ny
                while ix < tgt:
                    run(qx[ix:ix + 1]); ix += 1
            run(qx[ix:])
            S.flush()
        S.barrier()
        S.flush()


def phase2b(nc, S, T, P, cfg, tiles, G):
    C = cfg['C']
    NT = len(tiles)
    identf = G['identf']
    identb = G['identb']
    IOA = bass.IndirectOffsetOnAxis
    nq = 128
    with contextlib.ExitStack() as st:
        rw = T(st, "rw2", [128, 8, N_EXP], F32)
        rb = T(st, "rb2", [128, N_EXP], F32)
        trib = T(st, "trib2", [128, 128], BF16)
        onesb = T(st, "onesb2", [128, 128], BF16)
        iotae = T(st, "iotae2", [128, N_EXP], F32)
        pidx = T(st, "pidx2", [128, 1], F32)
        kconst = T(st, "kconst2", [128, 4], F32)
        tmpc = T(st, "tmpc2", [128, 128], F32)
        carry = T(st, "carry2", [128, N_EXP], F32)
        h1 = [T(st, "h1r%d" % i, [128, D], F32) for i in range(2)]
        hi_ = [T(st, "hir%d" % i, [128, D], BF16) for i in range(2)]
        lo_ = [T(st, "lor%d" % i, [128, D], BF16) for i in range(2)]
        hiT = [T(st, "hiTr%d" % i, [128, 8, 128], BF16) for i in range(2)]
        loT = [T(st, "loTr%d" % i, [128, 8, 128], BF16) for i in range(2)]
        rwh = T(st, "rwh", [128, 8, N_EXP], BF16)
        rwl = T(st, "rwl", [128, 8, N_EXP], BF16)
        lg = [T(st, "lgr%d" % i, [128, N_EXP], F32) for i in range(2)]
        mx8 = [T(st, "mx8r%d" % i, [128, 8], F32) for i in range(2)]
        ix8 = [T(st, "ix8r%d" % i, [128, 8], U32) for i in range(2)]
        rt = [T(st, "rtr%d" % i, [128, 64], F32) for i in range(2)]
        mask32 = [T(st, "mask32r%d" % i, [128, N_EXP], BF16) for i in range(2)]
        rk32 = [T(st, "rk32r%d" % i, [128, N_EXP], F32) for i in range(2)]
        junk32 = T(st, "junk32r", [128, N_EXP], F32)
        sloti = [T(st, "slotir%d" % i, [128, 4], I32) for i in range(2)]
        pl = [T(st, "plr%d" % i, [128, 4, 4], F32) for i in range(2)]
        bhr = [[P(st, "rbh%d_%d" % (i, q), [128, 8, 128], BF16) for q in range(2)] for i in range(2)]
        b1 = [P(st, "rb1_%d" % i, [128, 512], F32) for i in range(2)]
        S.dma('sp', rw[:], G['router_w'].rearrange("(k p) n -> p k n", p=128), (), ['rw'])
        S.dma('sp', rb[:], G['router_b'].partition_broadcast(128)[:, 0, :], (), ['rb'])
        S.copy('act', rwh[:], rw[:], ['rw'], ['rwh'])
        S.tt('dve', rwl[:], rw[:], rwh[:], ALU.subtract, ['rw', 'rwh'], ['rwl'])
        S.op('pool', 'iota', (), ['tmpc'], out=tmpc[:], pattern=[[1, 128]], base=0, channel_multiplier=-1,
             allow_small_or_imprecise_dtypes=True)
        S.ts('dve', trib[:], tmpc[:], 0.0, None, ALU.is_gt, None, ['tmpc'], ['trib'])
        S.memset('pool', onesb[:], 1.0, ['onesb'])
        S.op('pool', 'iota', (), ['iotae'], out=iotae[:], pattern=[[1, N_EXP]], base=0, channel_multiplier=0,
             allow_small_or_imprecise_dtypes=True)
        S.op('pool', 'iota', (), ['pidx'], out=pidx[:], pattern=[[0, 1]], base=0, channel_multiplier=1,
             allow_small_or_imprecise_dtypes=True)
        S.op('pool', 'iota', (), ['kconst'], out=kconst[:], pattern=[[1, 4]], base=0, channel_multiplier=0,
             allow_small_or_imprecise_dtypes=True)
        S.memset('pool', carry[:], 0.0, ['carry'])

        def load(ti):
            S.dma('sp', h1[ti % 2][:], G['s_h1'][ti * 128:(ti + 1) * 128, :], (), [('h1', ti % 2)])

        def tile_thunks(ti):
            b = ti % 2
            tokbase = ti * 128
            H1, LG, MX, IX, RT, M32, RK, SL, PL = h1[b], lg[b], mx8[b], ix8[b], rt[b], mask32[b], rk32[b], sloti[b], pl[b]
            HI, LO, HIT, LOT = hi_[b], lo_[b], hiT[b], loT[b]
            n = lambda x: (x, b)
            ps_, psn = b1[b], n('b1')
            R = n('rt')
            Q = []
            def _t0():
                S.copy('act', HI[:], H1[:], [n('h1')], [n('hi')])
                S.tt('dve', LO[:], H1[:], HI[:], ALU.subtract, [n('h1'), n('hi')], [n('lo')])
                for (src, sn_, dst, dn_, bank, bkn) in ((HI, n('hi'), HIT, n('hiT'), bhr[b][0], n('bhr0')),
                                                      (LO, n('lo'), LOT, n('loT'), bhr[b][1], n('bhr1'))):
                    for k in range(8):
                        S.tr(bank[:, k, :], src[:, k * 128:(k + 1) * 128], identb[:], [sn_, 'identb'], [bkn])
                    S.copy('act', dst[:], bank[:], [bkn], [dn_])
            Q.append(_t0)
            def _t1():
                terms = [(HIT, n('hiT'), rwh, 'rwh'), (HIT, n('hiT'), rwl, 'rwl'), (LOT, n('loT'), rwh, 'rwh')]
                cnt_ = 0
                for (xt_, xn_, w_, wn_) in terms:
                    for k in range(8):
                        S.mm(ps_[:, 0:N_EXP], xt_[:, k, :], w_[:, k, :], cnt_ == 0, cnt_ == 23, [xn_, wn_], [psn])
                        cnt_ += 1
            Q.append(_t1)
            def _t2():
                S.tt('dve', LG[:], ps_[:, 0:N_EXP], rb[:], ALU.add, [psn, 'rb'], [n('lg')])
            Q.append(_t2)
            def _t3():
                S.op('dve', 'max', [n('lg')], [n('mx8')], out=MX[:], in_=LG[:])
            Q.append(_t3)
            def _t4():
                S.op('dve', 'max_index', [n('lg'), n('mx8')], [n('ix8')], out=IX[:], in_max=MX[:], in_values=LG[:])
            Q.append(_t4)
            def _t5():
                S.ts('dve', RT[:, 0:1], MX[:, 0:1], -1.0, None, ALU.mult, None, [n('mx8')], [R])
            Q.append(_t5)
            def _t6():
                S.act(RT[:, 1:5], MX[:, 0:4], AF.Exp, [n('mx8'), R], [R], bias=RT[:, 0:1], scale=1.0, accum=RT[:, 5:6])
            Q.append(_t6)
            def _t7():
                S.op('dve', 'reciprocal', [R], [R], out=RT[:, 6:7], in_=RT[:, 5:6])
            Q.append(_t7)
            def _t8():
                S.ts('dve', RT[:, 8:12], RT[:, 1:5], RT[:, 6:7], None, ALU.mult, None, [R], [R])
            Q.append(_t8)
            def _t9():
                S.ts('dve', M32[:], LG[:], MX[:, 3:4], None, ALU.is_ge, None, [n('lg'), n('mx8')], [n('mask32')])
            Q.append(_t9)
            def _t10():
                S.mm(ps_[:, 64:64 + N_EXP], trib[:], M32[:], True, True, ['trib', n('mask32'), n('lg')], [psn])
            Q.append(_t10)
            def _t11():
                S.mm(ps_[:, 128:128 + N_EXP], onesb[:], M32[:], True, True, ['onesb', n('mask32')], [psn])
            Q.append(_t11)
            def _t12():
                S.tt('dve', RK[:], ps_[:, 64:64 + N_EXP], carry[:], ALU.add, [psn, 'carry'], [n('rk32')])
                S.tt('dve', carry[:], carry[:], ps_[:, 128:128 + N_EXP], ALU.add, [psn, 'carry', n('rk32')], ['carry'])
            Q.append(_t12)
            def _t13():
                S.copy('dve', RT[:, 12:16], IX[:, 0:4], [n('ix8'), R], [R])
            Q.append(_t13)
            def _t14():
                for k in range(4):
                    S.stt('dve', junk32[:], iotae[:], RT[:, 12 + k:13 + k], RK[:], ALU.is_equal, ALU.mult,
                          ['iotae', R, n('rk32')], ['junk32', R], accum=RT[:, 16 + k:17 + k])
            Q.append(_t14)
            def _t15():
                S.stt('dve', RT[:, 20:24], RT[:, 12:16], float(C), RT[:, 16:20], ALU.mult, ALU.add, [R], [R])
            Q.append(_t15)
            def _t16():
                S.ts('dve', RT[:, 24:28], RT[:, 16:20], float(C) - 0.5, None, ALU.is_ge, None, [R], [R])
            Q.append(_t16)
            def _t17():
                S.ts('dve', RT[:, 32:36], RT[:, 20:24], -1.0, float(N_EXP * C), ALU.mult, ALU.add, [R], [R])
            Q.append(_t17)
            def _t18():
                S.tt('dve', RT[:, 32:36], RT[:, 32:36], RT[:, 24:28], ALU.mult, [R], [R])
            Q.append(_t18)
            def _t19():
                S.tt('dve', RT[:, 20:24], RT[:, 20:24], RT[:, 32:36], ALU.add, [R], [R])
            Q.append(_t19)
            def _t20():
                S.copy('dve', SL[:], RT[:, 20:24], [R], [n('sloti')])
            Q.append(_t20)
            def _t21():
                S.ts('dve', RT[:, 28:29], pidx[:], float(tokbase), None, ALU.add, None, ['pidx', R], [R])
            Q.append(_t21)
            def _t22():
                S.copy('dve', PL[:, :, 0], RT[:, 28:29].broadcast_to([nq, 4]), [R], [n('pl')])
            Q.append(_t22)
            def _t23():
                S.stt('dve', PL[:, :, 1], RT[:, 28:29].broadcast_to([nq, 4]), 4.0, kconst[:], ALU.mult, ALU.add,
                      [R, 'kconst', n('pl')], [n('pl')])
            Q.append(_t23)
            def _t24():
                S.copy('dve', PL[:, :, 2], RT[:, 8:12], [R, n('pl')], [n('pl')])
            Q.append(_t24)
            def _t25():
                S.memset('pool', PL[:, :, 3], 0.0, [n('pl')])
            Q.append(_t25)
            def _t26():
                for k in range(4):
                    S.op('pool', 'indirect_dma_start', [n('pl'), n('sloti')], [('slots', ti, k)], dma=True,
                         out=G['slots'][:, :], out_offset=IOA(ap=SL[:, k:k + 1], axis=0),
                         in_=PL[:, k, :], in_offset=None)
            Q.append(_t26)
            return Q

        for p0 in range(0, NT, 2):
            pair = [t for t in (p0, p0 + 1) if t < NT]
            for t in pair:
                load(t)
            qs = [tile_thunks(t) for t in pair]
            for k in range(max(len(q) for q in qs)):
                for q in qs:
                    if k < len(q):
                        q[k]()
            S.flush()
        S.barrier()
        S.flush()


def ln_apply(S, nq, src, srcn, junk, junkn, st_, stn, g, gn, b, bn, dst, dstn):
    S.act(junk[0:nq, :], src[0:nq, :], AF.Identity, [srcn], [junkn, stn], accum=st_[0:nq, 24:25])
    S.act(junk[0:nq, :], src[0:nq, :], AF.Square, [srcn, junkn], [junkn, stn], accum=st_[0:nq, 25:26])
    S.ts('dve', st_[0:nq, 26:27], st_[0:nq, 24:25], 1.0 / D, None, ALU.mult, None, [stn, junkn], [stn])
    S.tt('dve', st_[0:nq, 27:28], st_[0:nq, 26:27], st_[0:nq, 26:27], ALU.mult, [stn], [stn])
    S.stt('dve', st_[0:nq, 28:29], st_[0:nq, 25:26], 1.0 / D, st_[0:nq, 27:28], ALU.mult, ALU.subtract, [stn], [stn])
    S.ts('dve', st_[0:nq, 28:29], st_[0:nq, 28:29], 1e-5, None, ALU.add, None, [stn], [stn])
    S.act(st_[0:nq, 28:29], st_[0:nq, 28:29], AF.Sqrt, [stn], [stn])
    S.op('dve', 'reciprocal', [stn], [stn], out=st_[0:nq, 28:29], in_=st_[0:nq, 28:29])
    S.stt('dve', st_[0:nq, 29:30], st_[0:nq, 26:27], -1.0, st_[0:nq, 28:29], ALU.mult, ALU.mult, [stn], [stn])
    S.act(junk[0:nq, :], src[0:nq, :], AF.Identity, [srcn, stn, junkn], [junkn], bias=st_[0:nq, 29:30], scale=st_[0:nq, 28:29])
    S.tt('dve', junk[0:nq, :], junk[0:nq, :], g[0:nq, :], ALU.mult, [junkn, gn], [junkn])
    S.tt('pool', dst[0:nq, :], junk[0:nq, :], b[0:nq, :], ALU.add, [junkn, bn], [dstn])


def phase0_init(nc, S, T, P, cfg, G, st):
    C = cfg['C']
    NTOK = G['NTOK']
    nsl = N_EXP * C // 128
    zt_ = T(st, "zero_t", [128, 2048], F32)
    zb_ = T(st, "zero_b", [128, D], BF16)
    sinit = T(st, "sinit", [128, nsl, 4], F32)
    S.memset('pool', zt_[:], 0.0, ['zero_t'])
    S.memset('pool', zb_[:], 0.0, ['zero_b'])
    S.memset('pool', sinit[:], 0.0, ['sinit'])
    S.memset('pool', sinit[:, :, 0], float(NTOK), ['sinit'])
    S.memset('pool', sinit[:, :, 1], float(NTOK * 4), ['sinit'])
    S.dma('act', G['slots'][0:N_EXP * C, :].rearrange("(n p) f -> p n f", p=128), sinit[:], ['sinit'], ['slots_init'])
    S.dma('act', G['s_h1b'][NTOK:NTOK + 128, :], zb_[:], ['zero_b'], ['h1b_init'])
    ykv = G['yk'][0:NTOK * 4, :].rearrange("(n p two) f -> n p (two f)", p=128, two=2)
    G['_ykz'] = [(n, ykv[n], zt_) for n in range(ykv.shape[0])]


def phase3(nc, S, T, P, cfg, tiles, G):
    C = cfg['C']
    NTOK = G['NTOK']
    NB = C // 128
    NCH = (C + 511) // 512
    CW = C // NCH
    identf, identb = G['identf'], G['identb']
    IOA = bass.IndirectOffsetOnAxis
    YSPLIT = cfg.get('YSPLIT', 2)
    with contextlib.ExitStack() as st:
        wu = [T(st, "wu%d" % i, [128, 8, 2 * D_FF], BF16) for i in range(2)]
        wd = [T(st, "wd%d" % i, [128, 8, D], BF16) for i in range(2)]
        bd = [T(st, "bd%d" % i, [1, D], F32) for i in range(2)]
        buT = T(st, "buT", [128, N_EXP * 16], F32)
        bul = T(st, "bul", [128, 4, 128], F32)
        onesf = T(st, "onesf", [1, 128], F32)
        slot_t = [T(st, "slot_t%d" % i, [128, NB, 4], F32) for i in range(4)]
        srci = [T(st, "srci%d" % i, [128, NB], I32) for i in range(4)]
        dsti = [T(st, "dsti%d" % i, [128, 2, NB], I32) for i in range(4)]
        dstf = [T(st, "dstf%d" % i, [128, 2, NB], F32) for i in range(2)]
        xg = [T(st, "xg%d" % i, [128, D], BF16) for i in range(NB)]
        xgT = [T(st, "xgT%d" % i, [128, 8, C], BF16) for i in range(2)]
        NGL = 4
        g_ = [T(st, "g_%d" % i, [128, CW], F32) for i in range(NGL)]
        s_ = [T(st, "s_%d" % i, [128, CW], F32) for i in range(NGL)]
        l_ = [T(st, "l_%d" % i, [128, CW], F32) for i in range(NGL)]
        aT = T(st, "aT", [128, 8, C], BF16)
        yo = [T(st, "yo%d" % i, [128, D], F32) for i in range(3)]
        bigA = P(st, "p3A", [128, 1024], F32)
        bigB = P(st, "p3B", [128, 1024], F32)
        b1 = [P(st, "p3b1_%d" % i, [128, 512], F32) for i in range(2)]
        bh = [P(st, "p3bh_%d" % i, [128, 8, 128], BF16) for i in range(2)]
        banks = [(b1[0][:, :], 'p3b1_0'), (b1[1][:, :], 'p3b1_1'), (bigA[:, 0:512], 'p3A0'), (bigA[:, 512:1024], 'p3A1')]
        rr = {'bk': 0, 'bh': 0, 'xg': 0, 'gl': 0, 'yo': 0, 'ds': 0}

        S.memset('pool', onesf[:], 1.0, ['onesf'])
        buv = G['b_up'].rearrange("e (c p) -> (e c) p", p=128)
        S.dma('sp', bul[:], buv.rearrange("(t r) p -> r t p", r=128), (), ['bul'])
        for t in range(4):
            S.tr(bigB[:, t * 128:(t + 1) * 128], bul[:, t, :], identf[:], ['bul', 'identf'], ['p3B'])
        S.copy('act', buT[:], bigB[:, 0:512], ['p3B'], ['buT'])

        def load_weights(e):
            b = e % 2
            wv = G['w_up'][e].rearrange("(k p) n -> p k n", p=128)
            for hk in range(2):
                S.dma('pool', wu[b][:, hk * 4:(hk + 1) * 4, :], wv[:, hk * 4:(hk + 1) * 4, :], (), [('wu', b, hk)])
            S.dma('pool', wd[b][:], G['w_down'][e].rearrange("(k p) n -> p k n", p=128), (), [('wd', b)])
            S.dma('sp', bd[b][:], G['b_down'][e:e + 1, :], (), [('bd', b)])

        def load_slots(e):
            t4 = e % 4
            S.dma('sp', slot_t[t4][:], G['slots'][e * C:(e + 1) * C, :].rearrange("(n p) f -> p n f", p=128), (), [('slot_t', t4)])
            S.copy('dve', srci[t4][:], slot_t[t4][:, :, 0], [('slot_t', t4)], [('srci', t4)])
            S.ts('dve', dstf[t4 % 2][:, 0, :], slot_t[t4][:, :, 1], 2.0, None, ALU.mult, None, [('slot_t', t4)], [('dstf', t4 % 2)])
            S.ts('dve', dstf[t4 % 2][:, 1, :], slot_t[t4][:, :, 1], 2.0, 1.0, ALU.mult, ALU.add, [('slot_t', t4)], [('dstf', t4 % 2)])
            S.copy('dve', dsti[t4][:], dstf[t4 % 2][:], [('dstf', t4 % 2)], [('dsti', t4)])

        def gathers(e):
            t4 = e % 4
            for blk in range(NB):
                S.op('pool', 'indirect_dma_start', [('srci', t4)], [('xg', blk)], dma=True,
                     out=xg[blk][:, :], out_offset=None, in_=G['s_h1b'][:, :],
                     in_offset=IOA(ap=srci[t4][:, blk:blk + 1], axis=0))

        def xposes(e):
            xb = e % 2
            for blk in range(NB):
                hi = rr['bh'] % 2; rr['bh'] += 1
                for k in range(8):
                    S.tr(bh[hi][:, k, :], xg[blk][:, k * 128:(k + 1) * 128], identb[:], [('xg', blk), 'identb'], ['p3bh_%d' % hi])
                S.copy('act', xgT[xb][:, :, blk * 128:(blk + 1) * 128], bh[hi][:], ['p3bh_%d' % hi], [('xgT', xb, blk)])

        load_weights(0)
        load_slots(0)
        if N_EXP > 1:
            load_slots(1)
        gathers(0)
        xposes(0)
        for e in range(N_EXP):
            b = e % 2
            t4 = e % 4
            xb = e % 2
            if e + 1 < N_EXP:
                load_weights(e + 1)
            if e + 2 < N_EXP:
                load_slots(e + 2)
            if e + 1 < N_EXP:
                gathers(e + 1)
            xgr = [('xgT', xb, blk) for blk in range(NB)]
            for fc in range(8):
                for ch in range(NCH):
                    cs = slice(ch * CW, (ch + 1) * CW)
                    pg, pgn = banks[rr['bk'] % 4]; rr['bk'] += 1
                    plb, pln = banks[rr['bk'] % 4]; rr['bk'] += 1
                    for k in range(8):
                        S.mm(pg[:, 0:CW], wu[b][:, k, fc * 128:(fc + 1) * 128], xgT[xb][:, k, cs], k == 0, k == 7,
                             [('wu', b, k // 4)] + xgr, [pgn])
                    for k in range(8):
                        S.mm(plb[:, 0:CW], wu[b][:, k, D_FF + fc * 128:D_FF + (fc + 1) * 128], xgT[xb][:, k, cs], k == 0, k == 7,
                             [('wu', b, k // 4)] + xgr, [pln])
                    i = rr['gl'] % NGL; rr['gl'] += 1
                    gn_, sn_, ln_ = 'g_%d' % i, 's_%d' % i, 'l_%d' % i
                    S.ts('dve', g_[i][:], pg[:, 0:CW], buT[:, e * 16 + fc:e * 16 + fc + 1], 7.0, ALU.add, ALU.min,
                         [pgn, 'buT'], [gn_])
                    S.act(s_[i][:], g_[i][:], AF.Sigmoid, [gn_], [sn_], scale=1.702)
                    S.act(l_[i][:], plb[:, 0:CW], AF.Identity, [pln, 'buT'], [ln_],
                          bias=buT[:, e * 16 + 8 + fc:e * 16 + 8 + fc + 1], scale=1.0)
                    S.ts('dve', l_[i][:], l_[i][:], -7.0, 7.0, ALU.max, ALU.min, [ln_], [ln_])
                    S.tt('pool', g_[i][:], g_[i][:], s_[i][:], ALU.mult, [gn_, sn_], [gn_])
                    S.stt('dve', aT[:, fc, cs], l_[i][:], 1.0, g_[i][:], ALU.add, ALU.mult, [ln_, gn_], [('aT', fc, ch)])
            if e + 1 < N_EXP:
                xposes(e + 1)
            atr = [('aT', fc, ch) for fc in range(8) for ch in range(NCH)]
            ykh = G['yk'].rearrange("r (two f) -> (r two) f", two=2)
            dsets = [((bigB[:, 0:512], 'p3B'), (bigB[:, 512:1024], 'p3B_hi')),
                     ((bigA[:, 0:512], 'p3A0'), (bigA[:, 512:1024], 'p3A1')),
                     ((b1[0][:, :], 'p3b1_0'), (b1[1][:, :], 'p3b1_1'))]
            for blk in range(NB):
                dset = dsets[rr['ds'] % 3]; rr['ds'] += 1
                i = rr['yo'] % 3; rr['yo'] += 1
                for cb in range(2):
                    osl, osn = dset[cb]
                    for k in range(8):
                        S.mm(osl, aT[:, k, blk * 128:(blk + 1) * 128], wd[b][:, k, cb * 512:(cb + 1) * 512], k == 0, False,
                             atr + [('wd', b)], [osn])
                    S.mm(osl, onesf[0:1, :], bd[b][0:1, cb * 512:(cb + 1) * 512], False, True, ['onesf', ('bd', b)], [osn])
                for cb in range(2):
                    osl, osn = dset[cb]
                    S.act(yo[i][:, cb * 512:(cb + 1) * 512], osl, AF.Copy, [osn, ('slot_t', t4)], [('yo', i, cb)],
                          scale=slot_t[t4][:, blk, 2:3])
                for hc in range(2):
                    S.op('pool', 'indirect_dma_start', [('yo', i, hc), ('dsti', t4)], [('yk', e, blk, hc)], dma=True,
                         out=ykh[:, :], out_offset=IOA(ap=dsti[t4][:, hc, blk:blk + 1], axis=0),
                         in_=yo[i][:, hc * 512:(hc + 1) * 512], in_offset=None)
            S.flush()
        S.barrier()
        S.flush()


def phase4(nc, S, T, P, cfg, tiles, G):
    NPS, SEQ, NSS = cfg['NPS'], cfg['SEQ'], cfg['NSS']
    NT = len(tiles)
    with contextlib.ExitStack() as st:
        l2g = T(st, "l2g", [128, D], F32)
        l2b = T(st, "l2b", [128, D], F32)
        ykt = [T(st, "ykt%d" % i, [128, 4, D], F32) for i in range(3)]
        h1t = [T(st, "h1t%d" % i, [128, D], F32) for i in range(3)]
        sa_l = [T(st, "sa%d" % i, [128, D], F32) for i in range(2)]
        sb_l = [T(st, "sb_%d" % i, [128, D], F32) for i in range(2)]
        pre2_l = [T(st, "pre2%d" % i, [128, D], F32) for i in range(2)]
        junk4_l = [T(st, "junk4%d" % i, [128, D], F32) for i in range(2)]
        st4_l = [T(st, "st4%d" % i, [128, 32], F32) for i in range(2)]
        yout = [T(st, "yout%d" % i, [128, D], F32) for i in range(2)]
        S.dma('sp', l2g[:], G['ln2_g'].partition_broadcast(128)[:, 0, :], (), ['l2g'])
        S.dma('sp', l2b[:], G['ln2_b'].partition_broadcast(128)[:, 0, :], (), ['l2b'])
        ykv = G['yk'][0:G['NTOK'] * 4, :].rearrange("(t k) f -> t k f", k=4)

        def load(ti):
            b = ti % 3
            S.dma('sp', ykt[b][:, 0:2, :], ykv[ti * 128:(ti + 1) * 128, 0:2, :], (), [('ykt', b, 0)])
            S.dma('act', ykt[b][:, 2:4, :], ykv[ti * 128:(ti + 1) * 128, 2:4, :], (), [('ykt', b, 1)])
            S.dma('sp', h1t[b][:], G['s_h1'][ti * 128:(ti + 1) * 128, :], (), ['h1t%d' % b])

        load(0)
        if NT > 1:
            load(1)
        for ti in range(NT):
            kind, a, j = tiles[ti]
            b = ti % 3
            if ti + 2 < NT:
                load(ti + 2)
            yb = ti % 2
            sa, sb_, pre2, junk4, st4 = sa_l[yb], sb_l[yb], pre2_l[yb], junk4_l[yb], st4_l[yb]
            san, sbn, pn, jn, stn4 = 'sa%d' % yb, 'sb_%d' % yb, 'pre2%d' % yb, 'junk4%d' % yb, 'st4%d' % yb
            S.tt('dve', sa[:], ykt[b][:, 0, :], ykt[b][:, 1, :], ALU.add, [('ykt', b, 0)], [san])
            S.tt('dve', sb_[:], ykt[b][:, 2, :], ykt[b][:, 3, :], ALU.add, [('ykt', b, 1)], [sbn])
            S.tt('dve', sa[:], sa[:], sb_[:], ALU.add, [san, sbn], [san])
            S.stt('dve', pre2[:], h1t[b][:], float(ALPHA), sa[:], ALU.mult, ALU.add, ['h1t%d' % b, san], [pn])
            ln_apply(S, 128, pre2, pn, junk4, jn, st4, stn4, l2g, 'l2g', l2b, 'l2b', yout[yb], 'yout%d' % yb)
            if kind == 'p':
                S.dma('act', G['y_p'][a, j * 128:(j + 1) * 128, :], yout[yb][:], ['yout%d' % yb])
            else:
                for hh in range(2):
                    S.dma('act', G['y_s'][2 * a + hh], yout[yb][hh * 64:(hh + 1) * 64, :], ['yout%d' % yb])
        S.barrier()
        S.flush()


def prep_inputs(inputs, cfg, ncores):
    NPS, SEQ, NSS = cfg['NPS'], cfg['SEQ'], cfg['NSS']
    f = lambda a: np.ascontiguousarray(np.asarray(a, dtype=np.float32))
    consts = make_consts(SEQ)
    cp = col_perm()
    w_in = f(inputs['w_in'][0][:, cp])
    hperm = np.concatenate([np.arange(h * 64, (h + 1) * 64) for h in HEAD_ORDER])
    w_dsa_o = f(inputs['w_dsa_o'][0])
    ff = np.concatenate([np.arange(0, 2 * D_FF, 2), np.arange(1, 2 * D_FF, 2)])
    w_up = f(inputs['w_up'][0][:, :, ff])
    b_up = f(inputs['b_up'][0][:, ff])
    shared = dict(
        w_in=w_in, w_ret_o=f(inputs['w_ret_o'][0]), w_dsa_o=w_dsa_o, w_o=f(inputs['w_o'][0]),
        ln1_g=f(inputs['ln1_g']), ln1_b=f(inputs['ln1_b']), ln2_g=f(inputs['ln2_g']), ln2_b=f(inputs['ln2_b']),
        router_w=f(inputs['router_w'][0]), router_b=f(inputs['router_b']),
        w_up=w_up, b_up=b_up, w_down=f(inputs['w_down'][0]), b_down=f(inputs['b_down'][0]),
        c_rotp=consts['rotp'], c_rots=consts['rots'], c_dec128=consts['dec128'], c_dec64=consts['dec64'],
        c_xi128=consts['xi128'], c_xi64=consts['xi64'], c_zp=consts['zp'], c_zs=consts['zs'],
    )
    if cfg.get('phases', 4) < 3:
        for k in ('w_up', 'b_up', 'w_down', 'b_down'):
            shared[k] = np.ascontiguousarray(shared[k][0:1])
    maps = []
    for c in range(ncores):
        m = dict(shared)
        m['xp'] = f(inputs['x_prompt'][c * NPS:(c + 1) * NPS])
        m['xs'] = f(inputs['x_sample'][c * NSS:(c + 1) * NSS])
        m['state_ret'] = f(inputs['state_ret'][0][c * NSS:(c + 1) * NSS])
        m['cache_k'] = f(inputs['cache_k'][0][c * NSS:(c + 1) * NSS]).reshape(NSS, 1024, 128)
        m['cache_v'] = f(inputs['cache_v'][0][c * NSS:(c + 1) * NSS]).reshape(NSS, 1024, 128)
        m['cache_ik'] = f(inputs['cache_idx_k'][0][c * NSS:(c + 1) * NSS])
        maps.append(m)
    return maps


def assemble(results, cfg, ncores):
    NPS, SEQ, NSS = cfg['NPS'], cfg['SEQ'], cfg['NSS']
    cat = lambda k: np.concatenate([np.asarray(r[k]) for r in results], axis=0)
    y_p = cat('y_p'); y_s = cat('y_s')
    rs_p = cat('rs_p')[None]; rs_s = cat('rs_s')[None]
    k_p = cat('k_p').reshape(ncores * NPS, SEQ, 2, 64)[None]
    v_p = cat('v_p').reshape(ncores * NPS, SEQ, 2, 64)[None]
    ik_p = cat('ik_p')[None]
    k_s = cat('k_s').reshape(ncores * NSS, 64, 2, 64)[None]
    v_s = cat('v_s').reshape(ncores * NSS, 64, 2, 64)[None]
    ik_s = cat('ik_s')[None]
    return (y_p, y_s, rs_p, k_p, v_p, ik_p, rs_s, k_s, v_s, ik_s)


def run(inputs, cfg, ncores):
    nc = build(cfg)
    maps = prep_inputs(inputs, cfg, ncores)
    res = run_bass_kernel_spmd(nc, maps, core_ids=list(range(ncores)))
    return res


def kernel(**inputs):
    cfg = dict(NPS=2, SEQ=2048, NSS=4, C=768)
    res = run(inputs, cfg, 8)
    outs = assemble(res.results, cfg, 8)
    return tuple(np.ascontiguousarray(o.astype(np.float32)) for o in outs)
```

```python
import contextlib
import numpy as np
import concourse.bass as bass
import concourse.mybir as mybir
from concourse.bass_utils import run_bass_kernel_spmd

F32 = mybir.dt.float32
BF16 = mybir.dt.bfloat16
I32 = mybir.dt.int32
U32 = mybir.dt.uint32
ALU = mybir.AluOpType
AF = mybir.ActivationFunctionType
AX = mybir.AxisListType

D = 1024
NDS = 32
NEG = -1.0e30
N_EXP = 32
D_FF = 1024
ALPHA = 2.0 ** 0.25
PROJ = 6472


class Sched:
    ENG = ('pe', 'dve', 'act', 'pool', 'sp')
    DMAQ = ('sp', 'act', 'pool')
    ENGOBJ = {'pe': 'tensor', 'dve': 'vector', 'act': 'scalar', 'pool': 'gpsimd', 'sp': 'sync'}

    def __init__(self, nc, stack):
        self.nc = nc
        self.streams = {e: [] for e in self.ENG}
        self.cnt = {e: 0 for e in self.ENG}
        self.dcnt = {q: [0] * NDS for q in self.DMAQ}
        self.drr = {q: 0 for q in self.DMAQ}
        self.pw = {}
        self.last_w = {}
        self.readers = {}
        self.nops = 0
        self.sems = {}
        for e in self.ENG:
            self.sems[('c', e)] = stack.enter_context(nc.semaphore('c_' + e))
        for q in self.DMAQ:
            for j in range(NDS):
                self.sems[('d', q, j)] = stack.enter_context(nc.semaphore('d_%s_%d' % (q, j)))
        self.fence = {e: stack.enter_context(nc.sbuf_tensor('fence_' + e, [128, 2], F32)) for e in ('dve', 'act', 'pool')}

    def op(self, eng, meth, reads=(), writes=(), dma=False, after=(), **kw):
        fn = (meth, kw)
        deps = set(t for t in after if t is not None)

        for r in reads:
            t = self.last_w.get(r)
            if t is not None:
                deps.add(t)
        for w in writes:
            t = self.last_w.get(w)
            if t is not None:
                deps.add(t)
            for t in self.readers.get(w, ()):
                deps.add(t)
        if dma:
            j = self.drr[eng]
            self.drr[eng] = (j + 1) % NDS
            self.dcnt[eng][j] += 1
            tok = (('d', eng, j), 16 * self.dcnt[eng][j])
        else:
            self.cnt[eng] += 1
            tok = (('c', eng), self.cnt[eng])
        need = {}
        for (sk, v) in deps:
            if sk == ('c', 'pe') and eng == 'pe' and not dma:
                continue
            need[sk] = max(need.get(sk, 0), v)
        waits = []
        for sk, v in need.items():
            if self.pw.get((eng, sk), 0) < v:
                waits.append((sk, v))
                self.pw[(eng, sk)] = v
        for r in reads:
            self.readers.setdefault(r, []).append(tok)
        for w in writes:
            self.last_w[w] = tok
            self.readers[w] = []
        self.streams[eng].append((waits, fn, tok, dma))
        self.nops += 1
        if kw.get('accum_out') is not None and not kw.get('_nofence'):
            if eng == 'act':
                return self.op('act', 'memzero', (), writes, ap=self.fence['act'][:, 0:1])
            return self.op(eng, 'memset', (), writes, ap=self.fence[eng][:, 0:1], constant=0.0)
        return tok

    def barrier(self):
        cur = []
        for e in self.ENG:
            if self.cnt[e]:
                cur.append((('c', e), self.cnt[e]))
        for q in self.DMAQ:
            for j in range(NDS):
                if self.dcnt[q][j]:
                    cur.append((('d', q, j), 16 * self.dcnt[q][j]))
        for e in self.ENG:
            waits = []
            for sk, v in cur:
                if sk == ('c', e):
                    continue
                if self.pw.get((e, sk), 0) < v:
                    waits.append((sk, v))
                    self.pw[(e, sk)] = v
            self.streams[e].append((waits, None, None, False))
        self.last_w = {}
        self.readers = {}

    def flush(self):
        nc = self.nc
        sems = self.sems
        with nc.Block() as block:
            def mk(ename):
                stream = self.streams[ename]

                def body(eng):
                    for (waits, fn, tok, dma) in stream:
                        for (sk, v) in waits:
                            eng.wait_ge(sems[sk], v)
                        if fn is not None:
                            try:
                                inst = getattr(eng, fn[0])(**fn[1])
                            except Exception:
                                print("FAILED OP", ename, fn[0], {k: (getattr(v, 'shape', v), getattr(v, 'name', '')) for k, v in fn[1].items()})
                                raise
                            inst.then_inc(sems[tok[0]], 16 if dma else 1)
                return body
            for e in self.ENG:
                getattr(block, self.ENGOBJ[e])(mk(e))
        self.streams = {e: [] for e in self.ENG}

    def tt(self, eng, out, in0, in1, op, r, w):
        return self.op(eng, 'tensor_tensor', r, w, out=out, in0=in0, in1=in1, op=op)

    def ts(self, eng, out, in0, s1, s2, op0, op1, r, w, accum=None):
        kw = dict(out=out, in0=in0, scalar1=s1, scalar2=s2, op0=op0)
        if op1 is not None:
            kw['op1'] = op1
        if accum is not None:
            kw['accum_out'] = accum
        return self.op(eng, 'tensor_scalar', r, w, **kw)

    def stt(self, eng, out, in0, scalar, in1, op0, op1, r, w, accum=None):
        kw = dict(out=out, in0=in0, scalar=scalar, in1=in1, op0=op0, op1=op1)
        if accum is not None:
            kw['accum_out'] = accum
        return self.op(eng, 'scalar_tensor_tensor', r, w, **kw)

    def copy(self, eng, out, in_, r, w):
        if eng == 'act':
            return self.op('act', 'copy', r, w, out=out, in_=in_)
        return self.op(eng, 'tensor_copy', r, w, out=out, in_=in_)

    def act(self, out, in_, func, r, w, bias=None, scale=None, accum=None):
        kw = dict(out=out, in_=in_, func=func)
        if bias is not None:
            kw['bias'] = bias
        if scale is not None:
            kw['scale'] = scale
        if accum is not None:
            kw['accum_out'] = accum
        return self.op('act', 'activation', r, w, **kw)

    def mul(self, out, in_, m, r, w):
        return self.op('act', 'mul', r, w, out=out, in_=in_, mul=m)

    def mm(self, out, lhsT, rhs, start, stop, r, w):
        return self.op('pe', 'matmul', r, w, out=out, lhsT=lhsT, rhs=rhs, start=start, stop=stop)

    def tr(self, out, in_, ident, r, w):
        return self.op('pe', 'transpose', r, w, out=out, in_=in_, identity=ident)

    def dma(self, q, out, in_, r=(), w=()):
        return self.op(q, 'dma_start', r, w, dma=True, out=out, in_=in_)

    def memset(self, eng, ap, val, w):
        return self.op(eng, 'memset', (), w, ap=ap, constant=val)


def _gammas():
    return (1.0 - 2.0 ** (-5.0 - np.arange(4, dtype=np.float32))).astype(np.float32)


def make_consts(seq):
    f32 = np.float32
    ret_f = (np.float32(10000.0) ** (-np.linspace(0.0, 1.0, 64, dtype=f32))).astype(f32)
    att_f = (np.float32(500000.0) ** (-np.arange(0, 16, 2, dtype=f32) / np.float32(16))).astype(f32)

    def rot_table(pos):
        pos = pos.astype(f32)
        a = pos[:, None] * ret_f[None, :]
        b = pos[:, None] * att_f[None, :]
        return np.concatenate([np.cos(a), np.sin(a), np.cos(b), np.sin(b)], axis=1).astype(f32)

    c = {}
    c['rotp'] = rot_table(np.arange(seq))
    c['rots'] = rot_table(1024 + np.arange(64))
    lg = np.log(_gammas()).astype(f32)

    def dec(T):
        pos = np.arange(T, dtype=f32)
        diff = pos[:, None] - pos[None, :]
        d = np.where(diff >= 0, np.exp(np.maximum(diff, 0.0)[None] * lg[:, None, None]), 0.0)
        decT = np.ascontiguousarray(d.transpose(2, 0, 1)).astype(f32)
        xi = np.exp((pos + 1.0)[None, :] * lg[:, None]).astype(f32)
        zeta = np.exp((T - 1.0 - pos)[:, None] * lg[None, :]).astype(f32)
        gT = np.exp(T * lg).astype(f32)
        return decT, xi, zeta, gT

    d128, xi128, z128, g128 = dec(128)
    d64, xi64, z64, g64 = dec(64)
    c['dec128'] = d128.reshape(128, 512)
    c['dec64'] = d64.reshape(64, 256)
    c['xi128'] = np.ascontiguousarray(np.broadcast_to(xi128.reshape(1, 512), (128, 512)))
    c['xi64'] = np.ascontiguousarray(np.broadcast_to(xi64.reshape(1, 256), (128, 256)))
    sc = np.float32(128.0 ** -0.5)
    c['zp'] = (z128 * sc).astype(f32)
    c['zs'] = (np.concatenate([z64, z64], 0) * sc).astype(f32)
    c['g128'] = g128
    c['g64'] = g64
    return c


def col_perm():
    w = [512, 512, 1024, 1024, 512, 128, 128, 512, 64, 8, 1024, 1024]
    off = np.concatenate([[0], np.cumsum(w)])
    rq, rk, rv, rg, aq, ak, av, iq, ik, iw, gr, ga = [np.arange(off[i], off[i + 1]) for i in range(12)]
    aqh = aq.reshape(8, 64)
    aq_new = np.concatenate([aqh[h] for h in (0, 4, 1, 5, 2, 6, 3, 7)])
    return np.concatenate([rq, rk, rv, rg, aq_new, iq, ak, ik, av, iw, gr, ga])


HEAD_ORDER = (0, 4, 1, 5, 2, 6, 3, 7)


def build(cfg):
    NPS, SEQ, NSS = cfg['NPS'], cfg['SEQ'], cfg['NSS']
    dbg = cfg.get('debug', False)
    phases = cfg.get('phases', 4)
    JT = SEQ // 128
    NTP = NPS * JT
    NTS = NSS // 2
    NT = NTP + NTS
    NTOK = NT * 128
    g128 = [float(v) for v in _gammas() ** 128]
    g64 = [float(v) for v in _gammas() ** 64]

    nc = bass.Bass("TRN2", target_bir_lowering=False)

    def din(name, shape, dt=F32):
        return nc.dram_tensor(name, list(shape), dt, kind="ExternalInput").ap()

    def dout(name, shape, dt=F32):
        return nc.dram_tensor(name, list(shape), dt, kind="ExternalOutput").ap()

    def dscr(name, shape, dt):
        if dbg:
            return nc.dram_tensor(name, list(shape), dt, kind="ExternalOutput").ap()
        return nc.dram_tensor(name, list(shape), dt).ap()

    xp = din("xp", [NPS, SEQ, D])
    xs = din("xs", [NSS, 64, D])
    state_ret = din("state_ret", [NSS, 4, 128, 256])
    cache_k = din("cache_k", [NSS, 1024, 128])
    cache_v = din("cache_v", [NSS, 1024, 128])
    cache_ik = din("cache_ik", [NSS, 1024, 64])
    w_in = din("w_in", [D, PROJ])
    w_ret_o = din("w_ret_o", [D, D])
    w_dsa_o = din("w_dsa_o", [512, D])
    w_o = din("w_o", [D, D])
    ln1_g = din("ln1_g", [1, D]); ln1_b = din("ln1_b", [1, D])
    ln2_g = din("ln2_g", [1, D]); ln2_b = din("ln2_b", [1, D])
    router_w = din("router_w", [D, N_EXP]); router_b = din("router_b", [1, N_EXP])
    NE = N_EXP if phases >= 3 else 1
    w_up = din("w_up", [NE, D, 2 * D_FF]); b_up = din("b_up", [NE, 2 * D_FF])
    w_down = din("w_down", [NE, D_FF, D]); b_down = din("b_down", [NE, D])
    c_rotp = din("c_rotp", [SEQ, 144]); c_rots = din("c_rots", [64, 144])
    c_dec128 = din("c_dec128", [128, 512]); c_dec64 = din("c_dec64", [64, 256])
    c_xi128 = din("c_xi128", [128, 512]); c_xi64 = din("c_xi64", [128, 256])
    c_zp = din("c_zp", [128, 4]); c_zs = din("c_zs", [128, 4])

    y_p = dout("y_p", [NPS, SEQ, D]); y_s = dout("y_s", [NSS, 64, D])
    rs_p = dout("rs_p", [NPS, 4, 128, 256]); rs_s = dout("rs_s", [NSS, 4, 128, 256])
    k_p = dout("k_p", [NPS, SEQ, 128]); v_p = dout("v_p", [NPS, SEQ, 128]); ik_p = dout("ik_p", [NPS, SEQ, 64])
    k_s = dout("k_s", [NSS, 64, 128]); v_s = dout("v_s", [NSS, 64, 128]); ik_s = dout("ik_s", [NSS, 64, 64])

    s_rqT = dscr("s_rqT", [NT, 128, 512], BF16)
    s_rkT = dscr("s_rkT", [NT, 128, 512], BF16)
    s_rkz = dscr("s_rkz", [NT, 128, 512], BF16)
    s_rv = dscr("s_rv", [NT, 128, 1024], BF16)
    s_srg = dscr("s_srg", [NT, 128, 1024], BF16)
    s_aqT = dscr("s_aqT", [NT, 128, 512], BF16)
    s_iqT = dscr("s_iqT", [NT, 128, 512], BF16)
    s_iw = dscr("s_iw", [NT, 128, 8], F32)
    s_sgr = dscr("s_sgr", [NT, 128, 1024], F32)
    s_sga = dscr("s_sga", [NT, 128, 1024], F32)
    s_kT = dscr("s_kT", [NT, 128, 128], BF16)
    s_v = dscr("s_v", [NT, 128, 130], BF16)
    s_ikT = dscr("s_ikT", [NT, 128, 128], BF16)
    C = cfg['C']
    s_h1 = dscr("s_h1", [NTOK, D], F32)
    s_h1b = dscr("s_h1b", [NTOK + 128, D], BF16)
    slots = dscr("slots", [N_EXP * C + 128, 4], F32)
    yk = dscr("yk", [NTOK * 4 + 128, D], F32)

    tiles = []
    for s in range(NPS):
        for j in range(JT):
            tiles.append(('p', s, j))
    for u in range(NTS):
        tiles.append(('s', u, 0))

    with contextlib.ExitStack() as gst:
        S = Sched(nc, gst)

        def T(st, name, shape, dt):
            return st.enter_context(nc.sbuf_tensor(name, list(shape), dt))

        def P(st, name, shape, dt):
            return st.enter_context(nc.psum_tensor(name, list(shape), dt))

        identf = T(gst, "identf", [128, 128], F32)
        identb = T(gst, "identb", [128, 128], BF16)
        S.op('pool', 'iota', (), ['identf'], out=identf[:], pattern=[[1, 128]], base=0, channel_multiplier=-1,
             allow_small_or_imprecise_dtypes=True)
        S.ts('dve', identb[:], identf[:], 0.0, None, ALU.is_equal, None, ['identf'], ['identb'])
        S.ts('dve', identf[:], identf[:], 0.0, None, ALU.is_equal, None, ['identf', 'identb'], ['identf'])

        G = locals()
        if phases >= 1:
            phase1(nc, S, T, P, cfg, tiles, G)
        if phases >= 2:
            phase2(nc, S, T, P, cfg, tiles, G)
            phase2b(nc, S, T, P, cfg, tiles, G)
        if phases >= 3:
            phase3(nc, S, T, P, cfg, tiles, G)
        if phases >= 4:
            phase4(nc, S, T, P, cfg, tiles, G)

        S.barrier()
        S.flush()
    return nc


def phase1(nc, S, T, P, cfg, tiles, G):
    NT = len(tiles)
    identf, identb = G['identf'], G['identb']
    xp, xs, w_in = G['xp'], G['xs'], G['w_in']
    with contextlib.ExitStack() as st:
        wsb = T(st, "wsb", [128, 8, PROJ], BF16)
        xf = [T(st, "xf%d" % i, [128, D], F32) for i in range(2)]
        rot = [T(st, "rot%d" % i, [128, 144], F32) for i in range(2)]
        zt = [T(st, "zt%d" % i, [128, 4], F32) for i in range(2)]
        xT = T(st, "xT", [128, 8, 128], BF16)
        zc = [T(st, "zc%d" % i, [128, 512], F32) for i in range(2)]
        tmpa = [T(st, "tmpa%d" % i, [128, 4, 256], F32) for i in range(2)]
        rotd = [T(st, "rotd%d" % i, [128, 512], F32) for i in range(2)]
        qb = [T(st, "qb%d" % i, [128, 512], BF16) for i in range(2)]
        kzb = T(st, "kzb", [128, 512], BF16)
        trs = [T(st, "trs%d" % i, [128, 512], BF16) for i in range(2)]
        rvb = [T(st, "rvb%d" % i, [128, 512], BF16) for i in range(2)]
        sgf = [T(st, "sgf%d" % i, [128, 512], F32) for i in range(2)]
        z8 = T(st, "z8", [128, 328], F32)
        t8 = T(st, "t8", [128, 4, 24], F32)
        kb8 = T(st, "kb8", [128, 256], BF16)
        kTs = T(st, "kTs", [128, 256], BF16)
        vaug = T(st, "vaug", [128, 2, 65], BF16)
        iws = T(st, "iws", [128, 8], F32)
        psT = [P(st, "psT%d" % i, [128, 4, 128], F32) for i in range(2)]
        psz = [P(st, "psz%d" % i, [128, 512], F32) for i in range(4)]
        pstr = [P(st, "pstr%d" % i, [128, 4, 128], BF16) for i in range(2)]

        phase0_init(nc, S, T, P, cfg, G, st)
        wv = w_in.rearrange("(k p) n -> p k n", p=128)
        wbounds = [0, 1024, 2048, 3072, 4096, 4424, 5448, PROJ]
        for gi_ in range(7):
            S.dma('pool', wsb[:, :, wbounds[gi_]:wbounds[gi_ + 1]], wv[:, :, wbounds[gi_]:wbounds[gi_ + 1]], (), [('wsb', gi_)])

        def wgrp(coff):
            for gi_ in range(7):
                if wbounds[gi_] <= coff < wbounds[gi_ + 1]:
                    return gi_
        S.dma('sp', zt[0][:], G['c_zp'], (), ['zt0'])
        S.dma('sp', zt[1][:], G['c_zs'], (), ['zt1'])
        S.memset('pool', vaug[:], 1.0, ['vaug'])

        cnt = {'z': 0, 'tr': 0, 'q': 0, 'g': 0}

        def load_tile(ti):
            kind, a, j = tiles[ti]
            b = ti % 2
            if kind == 'p':
                S.dma('sp', xf[b][:], xp[a, j * 128:(j + 1) * 128, :], (), ['xf%d' % b])
                S.dma('sp', rot[b][:], G['c_rotp'][j * 128:(j + 1) * 128, :], (), ['rot%d' % b])
            else:
                for hh in range(2):
                    S.dma('sp', xf[b][hh * 64:(hh + 1) * 64, :], xs[2 * a + hh], (), ['xf%d' % b])
                    S.dma('sp', rot[b][hh * 64:(hh + 1) * 64, :], G['c_rots'], (), ['rot%d' % b])

        def transposes4(src_bf, dst_ap, srcname, dstname):
            i = cnt['tr'] % 2
            cnt['tr'] += 1
            for c in range(4):
                S.tr(pstr[i][:, c, :], src_bf[:, c * 128:(c + 1) * 128], identb[:], [srcname, 'identb'], ['pstr%d' % i])
            S.copy('act', dst_ap, pstr[i][:], ['pstr%d' % i], [dstname])

        def rotary_full(eng, src, dst, cosb, sinb, tmp, srcname, dstname, tmpname, rotname):
            sv = src.rearrange("p (h t i) -> p h t i", h=4, t=2)
            dv = dst.rearrange("p (h t i) -> p h t i", h=4, t=2)
            x1, x2 = sv[:, :, 0, :], sv[:, :, 1, :]
            ta, tb, tc_, td = tmp[:, :, 0:64], tmp[:, :, 64:128], tmp[:, :, 128:192], tmp[:, :, 192:256]
            S.tt(eng, ta, x1, cosb, ALU.mult, [srcname, rotname], [tmpname + 'a'])
            S.tt(eng, tb, x2, sinb, ALU.mult, [srcname, rotname], [tmpname + 'b'])
            S.tt(eng, tc_, x1, sinb, ALU.mult, [srcname, rotname], [tmpname + 'c'])
            S.tt(eng, td, x2, cosb, ALU.mult, [srcname, rotname], [tmpname + 'd'])
            S.tt(eng, dv[:, :, 0, :], ta, tb, ALU.subtract, [tmpname + 'a', tmpname + 'b'], [dstname + 'lo'])
            S.tt(eng, dv[:, :, 1, :], tc_, td, ALU.add, [tmpname + 'c', tmpname + 'd'], [dstname + 'hi'])

        def rotary_part(eng, buf, nh, cosa, sina, tmp, bufname, tmpname, rotname):
            bv = buf.rearrange("p (h i) -> p h i", h=nh)
            x1, x2 = bv[:, :, 0:8], bv[:, :, 8:16]
            cb = cosa.unsqueeze(1).broadcast_to([128, nh, 8])
            sb = sina.unsqueeze(1).broadcast_to([128, nh, 8])
            v3 = lambda t: t.rearrange("p (h i) -> p h i", h=nh)
            ta, tb, tc_, td = [v3(tmp[:, q, 0:nh * 8]) for q in range(4)]
            S.tt(eng, ta, x1, cb, ALU.mult, [bufname, rotname], [tmpname + 'a'])
            S.tt(eng, tb, x2, sb, ALU.mult, [bufname, rotname], [tmpname + 'b'])
            S.tt(eng, tc_, x1, sb, ALU.mult, [bufname, rotname], [tmpname + 'c'])
            S.tt(eng, td, x2, cb, ALU.mult, [bufname, rotname], [tmpname + 'd'])
            S.tt(eng, x1, ta, tb, ALU.subtract, [tmpname + 'a', tmpname + 'b', tmpname + 'c'], [bufname])
            S.tt(eng, x2, tc_, td, ALU.add, [tmpname + 'c', tmpname + 'd'], [bufname])

        order = ['rq', 'rk', 'rv0', 'rv1', 'rg0', 'rg1', 'aq', 'iq', 'b8', 'gr0', 'gr1', 'ga0', 'ga1']
        pending = []
        load_tile(0)
        for ti in range(NT):
            while pending:
                pending.pop(0)[1]()
            kind, a, j = tiles[ti]
            b = ti % 2
            if ti + 1 < NT:
                load_tile(ti + 1)
            ykz = G['_ykz']
            for _ in range(-(-len(ykz) // max(1, NT - ti))):
                if ykz:
                    n_, ap_, zt__ = ykz.pop(0)
                    S.dma('sp', ap_, zt__[:], ['zero_t'], [('yk_init', n_)])
            xfn, rotn = 'xf%d' % b, 'rot%d' % b
            ztn = 'zt0' if kind == 'p' else 'zt1'
            ztt = zt[0] if kind == 'p' else zt[1]
            for hf in range(2):
                for c in range(4):
                    k = hf * 4 + c
                    S.tr(psT[hf][:, c, :], xf[b][:, k * 128:(k + 1) * 128], identf[:], [xfn, 'identf'], ['psT%d' % hf])
                S.copy('act', xT[:, hf * 4:(hf + 1) * 4, :], psT[hf][:], ['psT%d' % hf], [('xT', hf)])
            cosb = rot[b][:, 0:64].unsqueeze(1).broadcast_to([128, 4, 64])
            sinb = rot[b][:, 64:128].unsqueeze(1).broadcast_to([128, 4, 64])
            cosa = rot[b][:, 128:136]
            sina = rot[b][:, 136:144]
            for blk in range(13):
                name = order[blk]
                wdt = 328 if name == 'b8' else 512
                if blk < 8:
                    coff = blk * 512
                elif name == 'b8':
                    coff = 8 * 512
                else:
                    coff = 8 * 512 + 328 + (blk - 9) * 512
                pz = cnt['z'] % 4
                cnt['z'] += 1
                pzn = 'psz%d' % pz
                if pending and pending[0][0] <= blk:
                    pending.pop(0)[1]()
                for k in range(8):
                    S.mm(psz[pz][:, 0:wdt], xT[:, k, :], wsb[:, k, coff:coff + wdt], k == 0, k == 7,
                         [('xT', k // 4), ('wsb', wgrp(coff))], [pzn])
                if name in ('rq', 'rk'):
                    i = cnt['q'] % 2
                    cnt['q'] += 1
                    S.copy('act', zc[i][:], psz[pz][:], [pzn], ['zc%d' % i])
                    eng = 'dve' if name == 'rq' else 'pool'
                    rotary_full(eng, zc[i][:], rotd[i][:], cosb, sinb, tmpa[i], 'zc%d' % i, 'rotd%d' % i, 'tmpa%d' % i, rotn)
                    rdn = ['rotd%dlo' % i, 'rotd%dhi' % i]
                    trv = trs[i][:].rearrange("p (c t) -> p c t", c=4)
                    if name == 'rq':
                        S.copy('act', qb[i][:], rotd[i][:], rdn, ['qb%d' % i])
                        def _f(i=i, trv=trv, ti=ti):
                            transposes4(qb[i], trv, 'qb%d' % i, 'trs%d' % i)
                            S.dma('sp', G['s_rqT'][ti], trs[i][:], ['trs%d' % i])
                        pending.append((blk + 2, _f))
                    else:
                        S.mul(qb[i][:], rotd[i][:], float(128.0 ** -0.5), rdn, ['qb%d' % i])
                        def _f(i=i, trv=trv, ti=ti):
                            transposes4(qb[i], trv, 'qb%d' % i, 'trs%d' % i)
                            S.dma('sp', G['s_rkT'][ti], trs[i][:], ['trs%d' % i])
                        pending.append((blk + 2, _f))
                        S.tt('dve', kzb[:].rearrange("p (h d) -> p h d", h=4),
                             rotd[i][:].rearrange("p (h d) -> p h d", h=4),
                             ztt[:].unsqueeze(2).broadcast_to([128, 4, 128]), ALU.mult, rdn + [ztn], ['kzb'])
                        S.dma('sp', G['s_rkz'][ti], kzb[:], ['kzb'])
                elif name in ('rv0', 'rv1', 'rg0', 'rg1'):
                    i = cnt['g'] % 2
                    cnt['g'] += 1
                    hh = int(name[-1])
                    if name[:2] == 'rv':
                        S.copy('act', rvb[i][:], psz[pz][:], [pzn], ['rvb%d' % i])
                        dst = G['s_rv']
                    else:
                        S.act(rvb[i][:], psz[pz][:], AF.Silu, [pzn], ['rvb%d' % i])
                        dst = G['s_srg']
                    S.dma('sp', dst[ti, :, hh * 512:(hh + 1) * 512], rvb[i][:], ['rvb%d' % i])
                elif name in ('aq', 'iq'):
                    i = cnt['q'] % 2
                    cnt['q'] += 1
                    S.copy('act', zc[i][:], psz[pz][:], [pzn], ['zc%d' % i])
                    rotary_part('pool', zc[i][:], 8, cosa, sina, tmpa[i], 'zc%d' % i, 'tmpa%d' % i, rotn)
                    S.mul(qb[i][:], zc[i][:], 0.125, ['zc%d' % i], ['qb%d' % i])
                    dst = G['s_aqT'] if name == 'aq' else G['s_iqT']
                    def _f(i=i, ti=ti, dst=dst):
                        transposes4(qb[i], trs[i][:].rearrange("p (c t) -> p c t", c=4), 'qb%d' % i, 'trs%d' % i)
                        S.dma('sp', dst[ti], trs[i][:], ['trs%d' % i])
                    pending.append((blk + 2, _f))
                elif name in ('gr0', 'gr1', 'ga0', 'ga1'):
                    i = cnt['g'] % 2
                    cnt['g'] += 1
                    hh = int(name[-1])
                    dst = G['s_sgr'] if name[:2] == 'gr' else G['s_sga']
                    S.act(sgf[i][:], psz[pz][:], AF.Sigmoid, [pzn], ['sgf%d' % i])
                    S.dma('sp', dst[ti, :, hh * 512:(hh + 1) * 512], sgf[i][:], ['sgf%d' % i])
                else:
                    S.copy('act', z8[:], psz[pz][:, 0:328], [pzn], ['z8'])
                    rotary_part('dve', z8[:, 0:192], 3, cosa, sina, t8, 'z8', 't8', rotn)
                    if kind == 'p':
                        r0 = j * 128
                        S.dma('sp', G['k_p'][a, r0:r0 + 128, :], z8[:, 0:128], ['z8'])
                        S.dma('sp', G['ik_p'][a, r0:r0 + 128, :], z8[:, 128:192], ['z8'])
                        S.dma('sp', G['v_p'][a, r0:r0 + 128, :], z8[:, 192:320], ['z8'])
                    else:
                        for hh in range(2):
                            sl = slice(hh * 64, (hh + 1) * 64)
                            S.dma('sp', G['k_s'][2 * a + hh], z8[sl, 0:128], ['z8'])
                            S.dma('sp', G['ik_s'][2 * a + hh], z8[sl, 128:192], ['z8'])
                            S.dma('sp', G['v_s'][2 * a + hh], z8[sl, 192:320], ['z8'])
                    S.copy('act', kb8[:, 0:128], z8[:, 0:128], ['z8'], ['kb8a'])
                    S.copy('act', kb8[:, 128:192], z8[:, 128:192], ['z8'], ['kb8b'])
                    S.copy('act', kb8[:, 192:256], z8[:, 128:192], ['z8'], ['kb8c'])
                    def _f(ti=ti):
                        i = cnt['tr'] % 2
                        cnt['tr'] += 1
                        for c in range(2):
                            S.tr(pstr[i][:, c, :], kb8[:, c * 128:(c + 1) * 128], identb[:],
                                 ['kb8a', 'kb8b', 'kb8c', 'identb'], ['pstr%d' % i])
                        S.copy('act', kTs[:].rearrange("p (c t) -> p c t", c=2), pstr[i][:, 0:2, :], ['pstr%d' % i], ['kTs'])
                        S.dma('sp', G['s_kT'][ti], kTs[:, 0:128], ['kTs'])
                        S.dma('sp', G['s_ikT'][ti], kTs[:, 128:256], ['kTs'])
                    pending.append((blk + 2, _f))
                    S.copy('act', vaug[:, :, 0:64], z8[:, 192:320].rearrange("p (g d) -> p g d", g=2), ['z8', 'vaug'], ['vaug'])
                    S.dma('sp', G['s_v'][ti], vaug[:].rearrange("p g d -> p (g d)"), ['vaug'])
                    S.mul(iws[:], z8[:, 320:328], float(8.0 ** -0.5), ['z8'], ['iws'])
                    S.dma('sp', G['s_iw'][ti], iws[:], ['iws'])
        while pending:
            pending.pop(0)[1]()
        S.barrier()
        S.flush()


def phase2(nc, S, T, P, cfg, tiles, G):
    NPS, SEQ, NSS = cfg['NPS'], cfg['SEQ'], cfg['NSS']
    C = cfg['C']
    NIT = cfg.get('NIT', 18)
    JT = SEQ // 128
    NTP = NPS * JT
    LK = max(SEQ, 1152)
    NKB = LK // 128
    identf, identb = G['identf'], G['identb']
    gam = _gammas().astype(np.float64)
    g128 = [float(v) for v in gam ** 128]
    g64 = [float(v) for v in gam ** 64]
    IOA = bass.IndirectOffsetOnAxis
    with contextlib.ExitStack() as st:
        wro = T(st, "wro", [128, 8, D], BF16)
        wdo = T(st, "wdo", [128, 4, D], BF16)
        wo = T(st, "wo", [128, 8, D], BF16)
        rw = T(st, "rw", [128, 8, N_EXP], F32)
        rb = T(st, "rb", [128, N_EXP], F32)
        l1g = T(st, "l1g", [128, D], F32)
        l1b = T(st, "l1b", [128, D], F32)
        dec128 = T(st, "dec128", [128, 512], F32)
        dec64 = T(st, "dec64", [64, 256], F32)
        xi128 = T(st, "xi128", [128, 512], F32)
        xi64 = T(st, "xi64", [128, 256], F32)
        pow2 = T(st, "pow2", [128, NIT], F32)
        trib = T(st, "trib", [128, 128], BF16)
        onesb = T(st, "onesb", [128, 128], BF16)
        iotae = T(st, "iotae", [128, N_EXP], F32)
        pidx = T(st, "pidx", [128, 1], F32)
        kconst = T(st, "kconst", [128, 4], F32)
        tmpc = T(st, "tmpc", [128, 128], F32)
        carry = T(st, "carry", [128, N_EXP], F32)
        S.dma('pool', wro[:], G['w_ret_o'].rearrange("(k p) n -> p k n", p=128), (), ['wro'])
        S.dma('pool', wdo[:], G['w_dsa_o'].rearrange("(k p) n -> p k n", p=128), (), ['wdo'])
        S.dma('pool', wo[:], G['w_o'].rearrange("(k p) n -> p k n", p=128), (), ['wo'])
        S.dma('sp', rw[:], G['router_w'].rearrange("(k p) n -> p k n", p=128), (), ['rw'])
        S.dma('sp', rb[:], G['router_b'].partition_broadcast(128)[:, 0, :], (), ['rb'])
        S.dma('sp', l1g[:], G['ln1_g'].partition_broadcast(128)[:, 0, :], (), ['l1g'])
        S.dma('sp', l1b[:], G['ln1_b'].partition_broadcast(128)[:, 0, :], (), ['l1b'])
        S.dma('sp', dec128[:], G['c_dec128'], (), ['dec128'])
        S.dma('sp', dec64[:], G['c_dec64'], (), ['dec64'])
        S.dma('sp', xi128[:], G['c_xi128'], (), ['xi128'])
        S.dma('sp', xi64[:], G['c_xi64'], (), ['xi64'])
        for i in range(NIT):
            S.memset('pool', pow2[:, i:i + 1], float(2.0 ** -(i + 1)), ['pow2'])
        S.op('pool', 'iota', (), ['tmpc'], out=tmpc[:], pattern=[[1, 128]], base=0, channel_multiplier=-1,
             allow_small_or_imprecise_dtypes=True)
        S.ts('dve', trib[:], tmpc[:], 0.0, None, ALU.is_gt, None, ['tmpc'], ['trib'])
        S.memset('pool', onesb[:], 1.0, ['onesb'])
        S.op('pool', 'iota', (), ['iotae'], out=iotae[:], pattern=[[1, N_EXP]], base=0, channel_multiplier=0,
             allow_small_or_imprecise_dtypes=True)
        S.op('pool', 'iota', (), ['pidx'], out=pidx[:], pattern=[[0, 1]], base=0, channel_multiplier=1,
             allow_small_or_imprecise_dtypes=True)
        S.op('pool', 'iota', (), ['kconst'], out=kconst[:], pattern=[[1, 4]], base=0, channel_multiplier=0,
             allow_small_or_imprecise_dtypes=True)
        S.memset('pool', carry[:], 0.0, ['carry'])

        S32 = T(st, "S32", [128, 4, 256], F32)
        Sb = T(st, "Sb", [128, 4, 256], BF16)
        KT = T(st, "KT", [128, LK], BF16)
        VA = T(st, "VA", [128, NKB, 2, 65], BF16)
        IKT = T(st, "IKT", [128, LK], BF16)
        kc = T(st, "kc", [128, 8, 128], BF16)
        ikc = T(st, "ikc", [128, 8, 2, 64], BF16)
        S.memset('pool', VA[:], 1.0, ['VA'])
        S.memset('pool', KT[:], 0.0, ['KT'])
        rqT_t = T(st, "rqT_t", [128, 512], BF16)
        rkT_t = T(st, "rkT_t", [128, 512], BF16)
        rkz_t = T(st, "rkz_t", [128, 512], BF16)
        rv_t = T(st, "rv_t", [128, 1024], BF16)
        srg_t = T(st, "srg_t", [128, 1024], BF16)
        aqT_t = T(st, "aqT_t", [128, 512], BF16)
        iqT_tt = [T(st, "iqT_t%d" % i, [128, 512], BF16) for i in range(2)]
        iw_tt = [T(st, "iw_t%d" % i, [128, 8], F32) for i in range(2)]
        sgr_t = T(st, "sgr_t", [128, 1024], F32)
        sga_t = T(st, "sga_t", [128, 1024], F32)
        x_t = T(st, "x_t", [128, D], F32)
        scTd = T(st, "scTd", [128, 512], BF16)
        rqxi = T(st, "rqxi", [128, 512], BF16)
        o_sb = T(st, "o_sb", [128, 1024], F32)
        junkf = T(st, "junkf", [128, 1024], F32)
        stt_ = T(st, "stt_", [128, 32], F32)
        retb = T(st, "retb", [128, 1024], BF16)
        retT = T(st, "retT", [128, 8, 128], BF16)
        idx = T(st, "idx", [128, LK], F32)
        junkb = T(st, "junkb", [128, LK], BF16)
        rl = [T(st, "rl%d" % i, [128, 512], F32) for i in range(2)]
        maskb = T(st, "maskb", [128, LK], BF16)
        biasTT = [T(st, "biasT%d" % i, [128, NKB, 128], BF16) for i in range(2)]
        PT = [T(st, "PT%d" % i, [128, 512], BF16) for i in range(4)]
        bis = T(st, "bis", [128, 8 + 3 * NIT], F32)
        rec = T(st, "rec", [128, 8], F32)
        oT_sb = T(st, "oT_sb", [65, 1024], F32)
        odsa = T(st, "odsa", [128, 512], BF16)
        odT = T(st, "odT", [128, 4, 128], BF16)
        m1 = T(st, "m1", [128, D], F32)
        m2 = T(st, "m2", [128, D], F32)
        mgb = T(st, "mgb", [128, D], BF16)
        mgT = T(st, "mgT", [128, 8, 128], BF16)
        pre = T(st, "pre", [128, D], F32)
        h1 = T(st, "h1", [128, D], F32)
        h1b = T(st, "h1b", [128, D], BF16)
        h1T = T(st, "h1T", [128, 8, 128], F32)
        lg = T(st, "lg", [128, N_EXP], F32)
        mx8 = T(st, "mx8", [128, 8], F32)
        ix8 = T(st, "ix8", [128, 8], U32)
        rt = T(st, "rt", [128, 64], F32)
        mask32 = T(st, "mask32", [128, N_EXP], BF16)
        rk32 = T(st, "rk32", [128, N_EXP], F32)
        junk32 = T(st, "junk32", [128, N_EXP], F32)
        sloti = T(st, "sloti", [128, 4], I32)
        pl = T(st, "pl", [128, 4, 4], F32)
        bigA = P(st, "bigA", [128, 1024], F32)
        bigB = P(st, "bigB", [128, 1024], F32)
        b1 = [P(st, "b1_%d" % i, [128, 512], F32) for i in range(2)]
        bh = [P(st, "bh_%d" % i, [128, 8, 128], BF16) for i in range(2)]
        rr = {'b1': 0, 'bh': 0, 'rl': 0, 'pt': 0, 'ab': 0}

        def nb1():
            i = rr['b1'] % 2; rr['b1'] += 1
            return b1[i], 'b1_%d' % i

        def nbh():
            i = rr['bh'] % 2; rr['bh'] += 1
            return bh[i], 'bh_%d' % i

        stop = cfg.get('p2stop', 99)
        ulim = cfg.get('p2units', 10 ** 9)
        ucount = [0]

        def unit(ti, c0, nq, kind, sq, j, first, last, tokbase, RX, RY, par):
            S = RY
            iqT_t = iqT_tt[par]; iw_t = iw_tt[par]; biasT = biasTT[par]
            iqn, iwn, bTn = 'iqT_t%d' % par, 'iw_t%d' % par, 'biasT%d' % par
            L = 128 * (j + 1) if kind == 'p' else 1088
            v4 = lambda buf: buf[:, 0:4 * nq].rearrange("p (c t) -> p c t", c=4)
            sc4 = lambda ap: ap.rearrange("p (c t) -> p c t", c=4)
            def ldT(dst, src, name):
                S.dma('sp', v4(dst), src[ti].rearrange("p (c t) -> p c t", c=4)[:, :, c0:c0 + nq], (), [name])
            ldT(rqT_t, G['s_rqT'], 'rqT_t'); ldT(rkT_t, G['s_rkT'], 'rkT_t')
            ldT(aqT_t, G['s_aqT'], 'aqT_t')
            S = RX
            ldT(iqT_t, G['s_iqT'], iqn)
            S.dma('sp', iw_t[0:nq, :], G['s_iw'][ti, c0:c0 + nq, :], (), [iwn])
            S = RY
            rows = slice(c0, c0 + nq)
            S.dma('sp', rkz_t[0:nq, :], G['s_rkz'][ti, rows, :], (), ['rkz_t'])
            S.dma('sp', rv_t[0:nq, :], G['s_rv'][ti, rows, :], (), ['rv_t'])
            S.dma('sp', srg_t[0:nq, :], G['s_srg'][ti, rows, :], (), ['srg_t'])
            S.dma('sp', sgr_t[0:nq, :], G['s_sgr'][ti, rows, :], (), ['sgr_t'])
            S.dma('sp', sga_t[0:nq, :], G['s_sga'][ti, rows, :], (), ['sga_t'])
            if kind == 'p':
                S.dma('sp', x_t[0:nq, :], G['xp'][sq, j * 128:(j + 1) * 128, :], (), ['x_t'])
            else:
                S.dma('sp', x_t[0:nq, :], G['xs'][sq], (), ['x_t'])
            if first:
                if kind == 'p':
                    S.memset('pool', S32[:], 0.0, ['S32'])
                    S.memset('pool', Sb[:], 0.0, ['Sb'])
                else:
                    S.dma('sp', S32[:], G['state_ret'][sq].rearrange("h p e -> p h e"), (), ['S32'])
                    S.copy('pool', Sb[:], S32[:], ['S32'], ['Sb'])
                    S.dma('pool', kc[:], G['cache_k'][sq].rearrange("(b p) f -> p b f", p=128), (), ['kc'])
                    ckv = G['cache_ik'][sq].rearrange("(b p) f -> p b f", p=128)
                    RX.dma('pool', ikc[:, :, 0, :], ckv, (), ['ikc'])
                    RX.dma('pool', ikc[:, :, 1, :], ckv, (), ['ikc'])
                    cvv = G['cache_v'][sq].rearrange("(b p) (g d) -> p b g d", p=128, g=2)
                    for g in range(2):
                        S.dma('pool', VA[:, 0:8, g, 0:64], cvv[:, :, g, :], (), ['VA'])
                    for (src, sname, dst, dname) in ((kc, 'kc', KT, 'KT'), (ikc, 'ikc', IKT, 'IKT')):
                        bb, bn = nbh()
                        RR = RY if sname == 'kc' else RX
                        for c in range(8):
                            sv = src[:, c, :] if sname == 'kc' else src[:, c, :, :].rearrange("p a d -> p (a d)")
                            RR.tr(bb[:, c, :], sv, identb[:], [sname, 'identb'], [bn])
                        RR.copy('act', dst[:, 0:1024].rearrange("p (c t) -> p c t", c=8), bb[:], [bn], [dname])
            if kind == 'p':
                kcol, kblk, kn_new = j * 128, j, 128
            else:
                kcol, kblk, kn_new = 1024, 8, 64
            S.dma('sp', KT[:, kcol:kcol + nq], G['s_kT'][ti][:, c0:c0 + nq], (), ['KT'])
            RX.dma('sp', IKT[:, kcol:kcol + nq], G['s_ikT'][ti][:, c0:c0 + nq], (), ['IKT'])
            S.dma('sp', VA[0:nq, kblk, :, :], G['s_v'][ti, rows, :].rearrange("p (g d) -> p g d", g=2), (), ['VA'])

            bk, bkn = bigB, 'bigB'
            for h in range(4):
                S.mm(bk[0:nq, h * nq:(h + 1) * nq], v4(rkT_t)[:, h, :], v4(rqT_t)[:, h, :], True, True,
                     ['rkT_t', 'rqT_t'], [bkn])
            dec = dec128 if nq == 128 else dec64
            decn = 'dec128' if nq == 128 else 'dec64'
            xi = xi128 if nq == 128 else xi64
            xin = 'xi128' if nq == 128 else 'xi64'
            S.tt('dve', scTd[0:nq, 0:4 * nq], bk[0:nq, 0:4 * nq], dec[0:nq, 0:4 * nq], ALU.mult, [bkn, decn], ['scTd'])
            S.tt('pool', rqxi[:, 0:4 * nq], rqT_t[:, 0:4 * nq], xi[:, 0:4 * nq], ALU.mult, ['rqT_t', xin], ['rqxi'])
            for h in range(4):
                osl = bigA[0:nq, h * 256:(h + 1) * 256]
                S.mm(osl, scTd[0:nq, h * nq:(h + 1) * nq], rv_t[0:nq, h * 256:(h + 1) * 256], True, False,
                     ['scTd', 'rv_t'], ['bigA_lo', 'bigA_hi'])
                S.mm(osl, v4(rqxi)[:, h, :], Sb[:, h, :], False, True, ['rqxi', 'Sb'], ['bigA_lo', 'bigA_hi'])
            gT = g128 if nq == 128 else g64
            for h in range(4):
                S.mm(bigB[:, h * 256:(h + 1) * 256], rkz_t[0:nq, h * 128:(h + 1) * 128], rv_t[0:nq, h * 256:(h + 1) * 256],
                     True, True, ['rkz_t', 'rv_t'], ['bigB'])
            for h in range(4):
                S.stt('dve', S32[:, h, :], S32[:, h, :], gT[h], bigB[:, h * 256:(h + 1) * 256], ALU.mult, ALU.add,
                      ['S32', 'bigB'], ['S32'])
            S.copy('pool', Sb[:], S32[:], ['S32'], ['Sb'])
            if last:
                dst = G['rs_p'][sq] if kind == 'p' else G['rs_s'][sq]
                S.dma('sp', dst.rearrange("h p e -> p h e"), S32[:], ['S32'])
            rtail = []

            def _gn1():
                for h in range(4):
                    hs = slice(h * 256, (h + 1) * 256)
                    S.act(o_sb[0:nq, hs], bigA[0:nq, hs], AF.Identity, ['bigA_lo', 'bigA_hi'], ['o_sb'], accum=stt_[0:nq, h:h + 1])
            rtail.append(_gn1)

            def _gn2():
                for h in range(4):
                    hs = slice(h * 256, (h + 1) * 256)
                    S.act(junkf[0:nq, hs], o_sb[0:nq, hs], AF.Square, ['o_sb'], ['junkf', 'stt_'], accum=stt_[0:nq, 4 + h:5 + h])
            rtail.append(_gn2)

            def _gn3():
                S.ts('dve', stt_[0:nq, 8:12], stt_[0:nq, 0:4], 1.0 / 256, None, ALU.mult, None, ['o_sb', 'stt_'], ['stt_'])
                S.tt('dve', stt_[0:nq, 12:16], stt_[0:nq, 8:12], stt_[0:nq, 8:12], ALU.mult, ['stt_'], ['stt_'])
                S.stt('dve', stt_[0:nq, 16:20], stt_[0:nq, 4:8], 1.0 / 256, stt_[0:nq, 12:16], ALU.mult, ALU.subtract, ['stt_'], ['stt_'])
                S.ts('dve', stt_[0:nq, 16:20], stt_[0:nq, 16:20], 1e-6, None, ALU.add, None, ['stt_'], ['stt_'])
                S.act(stt_[0:nq, 16:20], stt_[0:nq, 16:20], AF.Sqrt, ['stt_'], ['stt_'])
            rtail.append(_gn3)

            def _gn4():
                S.op('dve', 'reciprocal', ['stt_'], ['stt_'], out=stt_[0:nq, 16:20], in_=stt_[0:nq, 16:20])
                S.stt('dve', stt_[0:nq, 20:24], stt_[0:nq, 8:12], -1.0, stt_[0:nq, 16:20], ALU.mult, ALU.mult, ['stt_'], ['stt_'])
            rtail.append(_gn4)

            def _gn5():
                for h in range(4):
                    hs = slice(h * 256, (h + 1) * 256)
                    S.act(o_sb[0:nq, hs], o_sb[0:nq, hs], AF.Identity, ['o_sb', 'stt_', 'junkf'], ['o_sb'],
                          bias=stt_[0:nq, 20 + h:21 + h], scale=stt_[0:nq, 16 + h:17 + h])
                S.tt('pool', retb[0:nq, :], o_sb[0:nq, :], srg_t[0:nq, :], ALU.mult, ['o_sb', 'srg_t'], ['retb'])
            rtail.append(_gn5)

            def _gn6():
                bb, bn = nbh()
                for c in range(8):
                    S.tr(bb[:, c, 0:nq], retb[0:nq, c * 128:(c + 1) * 128], identb[0:nq, 0:nq], ['retb', 'identb'], [bn])
                S.copy('act', retT[:, :, 0:nq], bb[:, :, 0:nq], [bn], ['retT'])
            rtail.append(_gn6)

            def _gn7():
                for cb in range(2):
                    for k in range(8):
                        S.mm(bigA[0:nq, cb * 512:(cb + 1) * 512], retT[:, k, 0:nq], wro[:, k, cb * 512:(cb + 1) * 512],
                             k == 0, k == 7, ['retT', 'wro'], ['bigA_lo', 'bigA_hi'])
            rtail.append(_gn7)

            def _gn8():
                S.tt('dve', m1[0:nq, :], bigA[0:nq, :], sgr_t[0:nq, :], ALU.mult, ['bigA_lo', 'bigA_hi', 'sgr_t'], ['m1'])
            rtail.append(_gn8)
            while rtail:
                rtail.pop(0)()
            S = RX
            nb5 = (L + 511) // 512
            for kb5 in range(nb5):
                off = kb5 * 512
                w = min(512, L - off)
                for h in range(8):
                    c, hf = h // 2, h % 2
                    ps_, psn = nb1()
                    S.mm(ps_[0:nq, 0:w], v4(iqT_t)[hf * 64:(hf + 1) * 64, c, :], IKT[hf * 64:(hf + 1) * 64, off:off + w],
                         True, True, [iqn, 'IKT'], [psn])
                    i = rr['rl'] % 2; rr['rl'] += 1
                    S.act(rl[i][0:nq, 0:w], ps_[0:nq, 0:w], AF.Relu, [psn], ['rl%d' % i])
                    if h == 0:
                        S.ts('dve', idx[0:nq, off:off + w], rl[i][0:nq, 0:w], iw_t[0:nq, 0:1], None, ALU.mult, None,
                             ['rl%d' % i, iwn], ['idx'])
                    else:
                        S.stt('dve', idx[0:nq, off:off + w], rl[i][0:nq, 0:w], iw_t[0:nq, h:h + 1], idx[0:nq, off:off + w],
                              ALU.mult, ALU.add, ['rl%d' % i, iwn, 'idx'], ['idx'])
            S.op('dve', 'tensor_reduce', ['idx'], ['bis'], out=bis[0:nq, 0:1], in_=idx[0:nq, 0:L], axis=AX.X, op=ALU.max,
                 apply_absolute_value=True)
            if kind == 'p':
                S.memset('pool', idx[0:64, L - 64:L], NEG, ['idx'])
            S.ts('dve', bis[0:nq, 5:6], bis[0:nq, 0:1], 1.001, 1e-6, ALU.mult, ALU.add, ['bis'], ['bis'])
            S.ts('dve', bis[0:nq, 8:8 + NIT], pow2[0:nq, :], bis[0:nq, 5:6], None, ALU.mult, None, ['bis', 'pow2'], ['bis'])
            S.ts('dve', bis[0:nq, 8 + NIT:8 + 2 * NIT], bis[0:nq, 8:8 + NIT], 2.0, None, ALU.mult, None, ['bis'], ['bis'])
            S.ts('dve', bis[0:nq, 8 + 2 * NIT:8 + 3 * NIT], bis[0:nq, 8:8 + NIT], -1.0, None, ALU.mult, None, ['bis'], ['bis'])
            S.memset('dve', bis[0:nq, 2:3], 0.0, ['bis'])
            for it in range(NIT):
                S.ts('dve', junkb[0:nq, 0:L], idx[0:nq, 0:L], bis[0:nq, 2:3], 0.0, ALU.is_ge, ALU.add, ['idx', 'bis'],
                     ['junkb', 'bis'], accum=bis[0:nq, 3:4])
                S.ts('dve', bis[0:nq, 4:5], bis[0:nq, 3:4], 255.5, bis[0:nq, 8 + NIT + it:9 + NIT + it], ALU.is_ge, ALU.mult,
                     ['bis'], ['bis'])
                S.stt('dve', bis[0:nq, 2:3], bis[0:nq, 4:5], bis[0:nq, 8 + 2 * NIT + it:9 + 2 * NIT + it], bis[0:nq, 2:3],
                      ALU.add, ALU.add, ['bis'], ['bis'])
            S.ts('dve', bis[0:nq, 1:2], bis[0:nq, 2:3], bis[0:nq, 8 + NIT - 1:8 + NIT], None, ALU.subtract, None, ['bis'], ['bis'])
            S.ts('dve', maskb[0:nq, 0:L], idx[0:nq, 0:L], bis[0:nq, 1:2], None, ALU.is_ge, None, ['idx', 'bis'], ['maskb'])
            nkb = (L + 127) // 128
            kb = 0
            while kb < nkb:
                grp = []
                while kb < nkb and len(grp) < 8:
                    kn = min(128, L - kb * 128)
                    if grp and kn != 128:
                        break
                    grp.append((kb, kn)); kb += 1
                    if kn != 128:
                        break
                bb, bn = nbh()
                for gi, (kbi, kn) in enumerate(grp):
                    S.tr(bb[0:kn, gi, 0:nq], maskb[0:nq, kbi * 128:kbi * 128 + kn], identb[0:nq, 0:nq], ['maskb', 'identb'], [bn])
                kn = grp[0][1]
                S.ts('dve', biasT[0:kn, grp[0][0]:grp[0][0] + len(grp), 0:nq], bb[0:kn, 0:len(grp), 0:nq], 1.0, 30000.0,
                     ALU.subtract, ALU.mult, [bn], [bTn])
            if L % 128:
                S.memset('pool', biasT[64:128, nkb - 1, 0:nq], -30000.0, [bTn])
            S = RY
            abanks = [(bigA[:, 0:512], 'bigA_lo'), (bigA[:, 512:1024], 'bigA_hi')]
            kn = 128
            steps = [(kbi, g) for kbi in range(nkb) for g in range(2)]
            prev = None

            def _pv(kbi, g, i):
                if not cfg.get('noPV'):
                    S.mm(bigB[0:65, g * 512:g * 512 + 4 * nq], VA[0:kn, kbi, g, :], PT[i][0:kn, 0:4 * nq],
                         kbi == 0, kbi == nkb - 1, ['PT%d' % i, 'VA'], ['bigB'])
            for (kbi, g) in steps:
                ps_, psn = abanks[rr['ab'] % 2]; rr['ab'] += 1
                gs = slice(g * 64, (g + 1) * 64)
                S.mm(sc4(ps_[0:kn, 0:4 * nq]), KT[gs, kbi * 128:kbi * 128 + kn], v4(aqT_t)[gs, :, :], True, False,
                     ['KT', 'aqT_t'], [psn])
                S.mm(sc4(ps_[0:kn, 0:4 * nq]), identb[0:kn, 0:kn],
                     biasT[0:kn, kbi, 0:nq].unsqueeze(1).broadcast_to([kn, 4, nq]), False, True,
                     [bTn, 'identb'], [psn])
                i = rr['pt'] % 4; rr['pt'] += 1
                S.act(PT[i][0:kn, 0:4 * nq], ps_[0:kn, 0:4 * nq], AF.Exp, [psn], ['PT%d' % i])
                if prev is not None:
                    _pv(*prev)
                prev = (kbi, g, i)
            _pv(*prev)
            for g in range(2):
                S.copy('act', oT_sb[0:65, g * 512:g * 512 + 4 * nq], bigB[0:65, g * 512:g * 512 + 4 * nq], ['bigB'], ['oT_sb'])
            for g in range(2):
                for r in range(4):
                    S.tr(bigA[0:nq, g * 512 + r * 65:g * 512 + (r + 1) * 65], oT_sb[0:65, g * 512 + r * nq:g * 512 + (r + 1) * nq],
                         identf[0:65, 0:65], ['oT_sb', 'identf'], ['bigA_lo', 'bigA_hi'])
            for g in range(2):
                pg = bigA[0:nq, g * 512:g * 512 + 260].rearrange("p (r d) -> p r d", r=4)
                S.op('dve', 'reciprocal', ['bigA_lo', 'bigA_hi'], ['rec'], out=rec[0:nq, g * 4:(g + 1) * 4].unsqueeze(2), in_=pg[:, :, 64:65])
                S.tt('dve', odsa[0:nq, g * 256:(g + 1) * 256].rearrange("p (r d) -> p r d", r=4), pg[:, :, 0:64],
                     rec[0:nq, g * 4:(g + 1) * 4].unsqueeze(2).broadcast_to([nq, 4, 64]), ALU.mult, ['bigA_lo', 'bigA_hi', 'rec'], ['odsa'])
            bb, bn = nbh()
            for c in range(4):
                S.tr(bb[:, c, 0:nq], odsa[0:nq, c * 128:(c + 1) * 128], identb[0:nq, 0:nq], ['odsa', 'identb'], [bn])
            S.copy('act', odT[:, :, 0:nq], bb[:, 0:4, 0:nq], [bn], ['odT'])
            for cb in range(2):
                for k in range(4):
                    S.mm(bigB[0:nq, cb * 512:(cb + 1) * 512], odT[:, k, 0:nq], wdo[:, k, cb * 512:(cb + 1) * 512],
                         k == 0, k == 3, ['odT', 'wdo'], ['bigB'])
            S.tt('dve', m2[0:nq, :], bigB[0:nq, :], sga_t[0:nq, :], ALU.mult, ['bigB', 'sga_t'], ['m2'])
            S.tt('pool', mgb[0:nq, :], m1[0:nq, :], m2[0:nq, :], ALU.add, ['m1', 'm2'], ['mgb'])
            bb, bn = nbh()
            for c in range(8):
                S.tr(bb[:, c, 0:nq], mgb[0:nq, c * 128:(c + 1) * 128], identb[0:nq, 0:nq], ['mgb', 'identb'], [bn])
            S.copy('act', mgT[:, :, 0:nq], bb[:, :, 0:nq], [bn], ['mgT'])
            for cb in range(2):
                for k in range(8):
                    S.mm(bigA[0:nq, cb * 512:(cb + 1) * 512], mgT[:, k, 0:nq], wo[:, k, cb * 512:(cb + 1) * 512],
                         k == 0, k == 7, ['mgT', 'wo'], ['bigA_lo', 'bigA_hi'])
            S.stt('dve', pre[0:nq, :], x_t[0:nq, :], float(ALPHA), bigA[0:nq, :], ALU.mult, ALU.add, ['x_t', 'bigA_lo', 'bigA_hi'], ['pre'])
            ln_apply(S, nq, pre, 'pre', junkf, 'junkf', stt_, 'stt_', l1g, 'l1g', l1b, 'l1b', h1, 'h1')
            S.dma('sp', G['s_h1'][tokbase:tokbase + nq, :], h1[0:nq, :], ['h1'])
            S.copy('act', h1b[0:nq, :], h1[0:nq, :], ['h1'], ['h1b'])
            S.dma('sp', G['s_h1b'][tokbase:tokbase + nq, :], h1b[0:nq, :], ['h1b'])

        class Rec:
            def __init__(self, real):
                self.real = real
                self.q = []

            def __getattr__(self, name):
                f = getattr(self.real, name)

                def w(*a, **k):
                    self.q.append((f, a, k))
                return w

        ulist = []
        for s_ in range(NPS):
            for j in range(JT):
                ulist.append((s_ * JT + j, 0, 128, 'p', s_, j, j == 0, j == JT - 1, s_ * SEQ + j * 128))
        for sq in range(NSS):
            ulist.append((NTP + sq // 2, (sq % 2) * 64, 64, 's', sq, 0, True, True, NPS * SEQ + sq * 64))
        recs = []
        for ui, uargs in enumerate(ulist):
            RX, RY = Rec(S), Rec(S)
            unit(*uargs, RX, RY, ui % 2)
            recs.append((RX.q, RY.q))

        def run(q):
            for (f, a_, k_) in q:
                f(*a_, **k_)

        run(recs[0][0])
        for ui in range(len(recs)):
            qy = recs[ui][1]
            qx = recs[ui + 1][0] if ui + 1 < len(recs) else []
            ny, nx = len(qy), len(qx)
            ix = 0
            for iy in range(ny):
                run(qy[iy:iy + 1])
                tgt = 0 if (cfg.get('nointerleave') or ulist[ui][3] == 's') else (iy + 1) * nx // ny
                while ix < tgt:
                    run(qx[ix:ix + 1]); ix += 1
            run(qx[ix:])
            S.flush()
        S.barrier()
        S.flush()


def phase2b(nc, S, T, P, cfg, tiles, G):
    C = cfg['C']
    NT = len(tiles)
    identf = G['identf']
    identb = G['identb']
    IOA = bass.IndirectOffsetOnAxis
    nq = 128
    with contextlib.ExitStack() as st:
        rw = T(st, "rw2", [128, 8, N_EXP], F32)
        rb = T(st, "rb2", [128, N_EXP], F32)
        trib = T(st, "trib2", [128, 128], BF16)
        onesb = T(st, "onesb2", [128, 128], BF16)
        iotae = T(st, "iotae2", [128, N_EXP], F32)
        pidx = T(st, "pidx2", [128, 1], F32)
        kconst = T(st, "kconst2", [128, 4], F32)
        tmpc = T(st, "tmpc2", [128, 128], F32)
        carry = T(st, "carry2", [128, N_EXP], F32)
        h1 = [T(st, "h1r%d" % i, [128, D], F32) for i in range(2)]
        hi_ = [T(st, "hir%d" % i, [128, D], BF16) for i in range(2)]
        lo_ = [T(st, "lor%d" % i, [128, D], BF16) for i in range(2)]
        hiT = [T(st, "hiTr%d" % i, [128, 8, 128], BF16) for i in range(2)]
        loT = [T(st, "loTr%d" % i, [128, 8, 128], BF16) for i in range(2)]
        rwh = T(st, "rwh", [128, 8, N_EXP], BF16)
        rwl = T(st, "rwl", [128, 8, N_EXP], BF16)
        lg = [T(st, "lgr%d" % i, [128, N_EXP], F32) for i in range(2)]
        mx8 = [T(st, "mx8r%d" % i, [128, 8], F32) for i in range(2)]
        ix8 = [T(st, "ix8r%d" % i, [128, 8], U32) for i in range(2)]
        rt = [T(st, "rtr%d" % i, [128, 64], F32) for i in range(2)]
        mask32 = [T(st, "mask32r%d" % i, [128, N_EXP], BF16) for i in range(2)]
        rk32 = [T(st, "rk32r%d" % i, [128, N_EXP], F32) for i in range(2)]
        junk32 = T(st, "junk32r", [128, N_EXP], F32)
        sloti = [T(st, "slotir%d" % i, [128, 4], I32) for i in range(2)]
        pl = [T(st, "plr%d" % i, [128, 4, 4], F32) for i in range(2)]
        bhr = [[P(st, "rbh%d_%d" % (i, q), [128, 8, 128], BF16) for q in range(2)] for i in range(2)]
        b1 = [P(st, "rb1_%d" % i, [128, 512], F32) for i in range(2)]
        S.dma('sp', rw[:], G['router_w'].rearrange("(k p) n -> p k n", p=128), (), ['rw'])
        S.dma('sp', rb[:], G['router_b'].partition_broadcast(128)[:, 0, :], (), ['rb'])
        S.copy('act', rwh[:], rw[:], ['rw'], ['rwh'])
        S.tt('dve', rwl[:], rw[:], rwh[:], ALU.subtract, ['rw', 'rwh'], ['rwl'])
        S.op('pool', 'iota', (), ['tmpc'], out=tmpc[:], pattern=[[1, 128]], base=0, channel_multiplier=-1,
             allow_small_or_imprecise_dtypes=True)
        S.ts('dve', trib[:], tmpc[:], 0.0, None, ALU.is_gt, None, ['tmpc'], ['trib'])
        S.memset('pool', onesb[:], 1.0, ['onesb'])
        S.op('pool', 'iota', (), ['iotae'], out=iotae[:], pattern=[[1, N_EXP]], base=0, channel_multiplier=0,
             allow_small_or_imprecise_dtypes=True)
        S.op('pool', 'iota', (), ['pidx'], out=pidx[:], pattern=[[0, 1]], base=0, channel_multiplier=1,
             allow_small_or_imprecise_dtypes=True)
        S.op('pool', 'iota', (), ['kconst'], out=kconst[:], pattern=[[1, 4]], base=0, channel_multiplier=0,
             allow_small_or_imprecise_dtypes=True)
        S.memset('pool', carry[:], 0.0, ['carry'])

        def load(ti):
            S.dma('sp', h1[ti % 2][:], G['s_h1'][ti * 128:(ti + 1) * 128, :], (), [('h1', ti % 2)])

        def tile_thunks(ti):
            b = ti % 2
            tokbase = ti * 128
            H1, LG, MX, IX, RT, M32, RK, SL, PL = h1[b], lg[b], mx8[b], ix8[b], rt[b], mask32[b], rk32[b], sloti[b], pl[b]
            HI, LO, HIT, LOT = hi_[b], lo_[b], hiT[b], loT[b]
            n = lambda x: (x, b)
            ps_, psn = b1[b], n('b1')
            R = n('rt')
            Q = []
            def _t0():
                S.copy('act', HI[:], H1[:], [n('h1')], [n('hi')])
                S.tt('dve', LO[:], H1[:], HI[:], ALU.subtract, [n('h1'), n('hi')], [n('lo')])
                for (src, sn_, dst, dn_, bank, bkn) in ((HI, n('hi'), HIT, n('hiT'), bhr[b][0], n('bhr0')),
                                                      (LO, n('lo'), LOT, n('loT'), bhr[b][1], n('bhr1'))):
                    for k in range(8):
                        S.tr(bank[:, k, :], src[:, k * 128:(k + 1) * 128], identb[:], [sn_, 'identb'], [bkn])
                    S.copy('act', dst[:], bank[:], [bkn], [dn_])
            Q.append(_t0)
            def _t1():
                terms = [(HIT, n('hiT'), rwh, 'rwh'), (HIT, n('hiT'), rwl, 'rwl'), (LOT, n('loT'), rwh, 'rwh')]
                cnt_ = 0
                for (xt_, xn_, w_, wn_) in terms:
                    for k in range(8):
                        S.mm(ps_[:, 0:N_EXP], xt_[:, k, :], w_[:, k, :], cnt_ == 0, cnt_ == 23, [xn_, wn_], [psn])
                        cnt_ += 1
            Q.append(_t1)
            def _t2():
                S.tt('dve', LG[:], ps_[:, 0:N_EXP], rb[:], ALU.add, [psn, 'rb'], [n('lg')])
            Q.append(_t2)
            def _t3():
                S.op('dve', 'max', [n('lg')], [n('mx8')], out=MX[:], in_=LG[:])
            Q.append(_t3)
            def _t4():
                S.op('dve', 'max_index', [n('lg'), n('mx8')], [n('ix8')], out=IX[:], in_max=MX[:], in_values=LG[:])
            Q.append(_t4)
            def _t5():
                S.ts('dve', RT[:, 0:1], MX[:, 0:1], -1.0, None, ALU.mult, None, [n('mx8')], [R])
            Q.append(_t5)
            def _t6():
                S.act(RT[:, 1:5], MX[:, 0:4], AF.Exp, [n('mx8'), R], [R], bias=RT[:, 0:1], scale=1.0, accum=RT[:, 5:6])
            Q.append(_t6)
            def _t7():
                S.op('dve', 'reciprocal', [R], [R], out=RT[:, 6:7], in_=RT[:, 5:6])
            Q.append(_t7)
            def _t8():
                S.ts('dve', RT[:, 8:12], RT[:, 1:5], RT[:, 6:7], None, ALU.mult, None, [R], [R])
            Q.append(_t8)
            def _t9():
                S.ts('dve', M32[:], LG[:], MX[:, 3:4], None, ALU.is_ge, None, [n('lg'), n('mx8')], [n('mask32')])
            Q.append(_t9)
            def _t10():
                S.mm(ps_[:, 64:64 + N_EXP], trib[:], M32[:], True, True, ['trib', n('mask32'), n('lg')], [psn])
            Q.append(_t10)
            def _t11():
                S.mm(ps_[:, 128:128 + N_EXP], onesb[:], M32[:], True, True, ['onesb', n('mask32')], [psn])
            Q.append(_t11)
            def _t12():
                S.tt('dve', RK[:], ps_[:, 64:64 + N_EXP], carry[:], ALU.add, [psn, 'carry'], [n('rk32')])
                S.tt('dve', carry[:], carry[:], ps_[:, 128:128 + N_EXP], ALU.add, [psn, 'carry', n('rk32')], ['carry'])
            Q.append(_t12)
            def _t13():
                S.copy('dve', RT[:, 12:16], IX[:, 0:4], [n('ix8'), R], [R])
            Q.append(_t13)
            def _t14():
                for k in range(4):
                    S.stt('dve', junk32[:], iotae[:], RT[:, 12 + k:13 + k], RK[:], ALU.is_equal, ALU.mult,
                          ['iotae', R, n('rk32')], ['junk32', R], accum=RT[:, 16 + k:17 + k])
            Q.append(_t14)
            def _t15():
                S.stt('dve', RT[:, 20:24], RT[:, 12:16], float(C), RT[:, 16:20], ALU.mult, ALU.add, [R], [R])
            Q.append(_t15)
            def _t16():
                S.ts('dve', RT[:, 24:28], RT[:, 16:20], float(C) - 0.5, None, ALU.is_ge, None, [R], [R])
            Q.append(_t16)
            def _t17():
                S.ts('dve', RT[:, 32:36], RT[:, 20:24], -1.0, float(N_EXP * C), ALU.mult, ALU.add, [R], [R])
            Q.append(_t17)
            def _t18():
                S.tt('dve', RT[:, 32:36], RT[:, 32:36], RT[:, 24:28], ALU.mult, [R], [R])
            Q.append(_t18)
            def _t19():
                S.tt('dve', RT[:, 20:24], RT[:, 20:24], RT[:, 32:36], ALU.add, [R], [R])
            Q.append(_t19)
            def _t20():
                S.copy('dve', SL[:], RT[:, 20:24], [R], [n('sloti')])
            Q.append(_t20)
            def _t21():
                S.ts('dve', RT[:, 28:29], pidx[:], float(tokbase), None, ALU.add, None, ['pidx', R], [R])
            Q.append(_t21)
            def _t22():
                S.copy('dve', PL[:, :, 0], RT[:, 28:29].broadcast_to([nq, 4]), [R], [n('pl')])
            Q.append(_t22)
            def _t23():
                S.stt('dve', PL[:, :, 1], RT[:, 28:29].broadcast_to([nq, 4]), 4.0, kconst[:], ALU.mult, ALU.add,
                      [R, 'kconst', n('pl')], [n('pl')])
            Q.append(_t23)
            def _t24():
                S.copy('dve', PL[:, :, 2], RT[:, 8:12], [R, n('pl')], [n('pl')])
            Q.append(_t24)
            def _t25():
                S.memset('pool', PL[:, :, 3], 0.0, [n('pl')])
            Q.append(_t25)
            def _t26():
                for k in range(4):
                    S.op('pool', 'indirect_dma_start', [n('pl'), n('sloti')], [('slots', ti, k)], dma=True,
                         out=G['slots'][:, :], out_offset=IOA(ap=SL[:, k:k + 1], axis=0),
                         in_=PL[:, k, :], in_offset=None)
            Q.append(_t26)
            return Q

        for p0 in range(0, NT, 2):
            pair = [t for t in (p0, p0 + 1) if t < NT]
            for t in pair:
                load(t)
            qs = [tile_thunks(t) for t in pair]
            for k in range(max(len(q) for q in qs)):
                for q in qs:
                    if k < len(q):
                        q[k]()
            S.flush()
        S.barrier()
        S.flush()


def ln_apply(S, nq, src, srcn, junk, junkn, st_, stn, g, gn, b, bn, dst, dstn):
    S.act(junk[0:nq, :], src[0:nq, :], AF.Identity, [srcn], [junkn, stn], accum=st_[0:nq, 24:25])
    S.act(junk[0:nq, :], src[0:nq, :], AF.Square, [srcn, junkn], [junkn, stn], accum=st_[0:nq, 25:26])
    S.ts('dve', st_[0:nq, 26:27], st_[0:nq, 24:25], 1.0 / D, None, ALU.mult, None, [stn, junkn], [stn])
    S.tt('dve', st_[0:nq, 27:28], st_[0:nq, 26:27], st_[0:nq, 26:27], ALU.mult, [stn], [stn])
    S.stt('dve', st_[0:nq, 28:29], st_[0:nq, 25:26], 1.0 / D, st_[0:nq, 27:28], ALU.mult, ALU.subtract, [stn], [stn])
    S.ts('dve', st_[0:nq, 28:29], st_[0:nq, 28:29], 1e-5, None, ALU.add, None, [stn], [stn])
    S.act(st_[0:nq, 28:29], st_[0:nq, 28:29], AF.Sqrt, [stn], [stn])
    S.op('dve', 'reciprocal', [stn], [stn], out=st_[0:nq, 28:29], in_=st_[0:nq, 28:29])
    S.stt('dve', st_[0:nq, 29:30], st_[0:nq, 26:27], -1.0, st_[0:nq, 28:29], ALU.mult, ALU.mult, [stn], [stn])
    S.act(junk[0:nq, :], src[0:nq, :], AF.Identity, [srcn, stn, junkn], [junkn], bias=st_[0:nq, 29:30], scale=st_[0:nq, 28:29])
    S.tt('dve', junk[0:nq, :], junk[0:nq, :], g[0:nq, :], ALU.mult, [junkn, gn], [junkn])
    S.tt('pool', dst[0:nq, :], junk[0:nq, :], b[0:nq, :], ALU.add, [junkn, bn], [dstn])


def phase0_init(nc, S, T, P, cfg, G, st):
    C = cfg['C']
    NTOK = G['NTOK']
    nsl = N_EXP * C // 128
    zt_ = T(st, "zero_t", [128, 2048], F32)
    zb_ = T(st, "zero_b", [128, D], BF16)
    sinit = T(st, "sinit", [128, nsl, 4], F32)
    S.memset('pool', zt_[:], 0.0, ['zero_t'])
    S.memset('pool', zb_[:], 0.0, ['zero_b'])
    S.memset('pool', sinit[:], 0.0, ['sinit'])
    S.memset('pool', sinit[:, :, 0], float(NTOK), ['sinit'])
    S.memset('pool', sinit[:, :, 1], float(NTOK * 4), ['sinit'])
    S.dma('act', G['slots'][0:N_EXP * C, :].rearrange("(n p) f -> p n f", p=128), sinit[:], ['sinit'], ['slots_init'])
    S.dma('act', G['s_h1b'][NTOK:NTOK + 128, :], zb_[:], ['zero_b'], ['h1b_init'])
    ykv = G['yk'][0:NTOK * 4, :].rearrange("(n p two) f -> n p (two f)", p=128, two=2)
    G['_ykz'] = [(n, ykv[n], zt_) for n in range(ykv.shape[0])]


def phase3(nc, S, T, P, cfg, tiles, G):
    C = cfg['C']
    NTOK = G['NTOK']
    NB = C // 128
    NCH = (C + 511) // 512
    CW = C // NCH
    identf, identb = G['identf'], G['identb']
    IOA = bass.IndirectOffsetOnAxis
    YSPLIT = cfg.get('YSPLIT', 2)
    with contextlib.ExitStack() as st:
        wu = [T(st, "wu%d" % i, [128, 8, 2 * D_FF], BF16) for i in range(2)]
        wd = [T(st, "wd%d" % i, [128, 8, D], BF16) for i in range(2)]
        bd = [T(st, "bd%d" % i, [1, D], F32) for i in range(2)]
        buT = T(st, "buT", [128, N_EXP * 16], F32)
        bul = T(st, "bul", [128, 4, 128], F32)
        onesf = T(st, "onesf", [1, 128], F32)
        slot_t = [T(st, "slot_t%d" % i, [128, NB, 4], F32) for i in range(4)]
        srci = [T(st, "srci%d" % i, [128, NB], I32) for i in range(4)]
        dsti = [T(st, "dsti%d" % i, [128, 2, NB], I32) for i in range(4)]
        dstf = [T(st, "dstf%d" % i, [128, 2, NB], F32) for i in range(2)]
        xg = [T(st, "xg%d" % i, [128, D], BF16) for i in range(NB)]
        xgT = [T(st, "xgT%d" % i, [128, 8, C], BF16) for i in range(2)]
        NGL = 4
        g_ = [T(st, "g_%d" % i, [128, CW], F32) for i in range(NGL)]
        s_ = [T(st, "s_%d" % i, [128, CW], F32) for i in range(NGL)]
        l_ = [T(st, "l_%d" % i, [128, CW], F32) for i in range(NGL)]
        aT = T(st, "aT", [128, 8, C], BF16)
        yo = [T(st, "yo%d" % i, [128, D], F32) for i in range(3)]
        bigA = P(st, "p3A", [128, 1024], F32)
        bigB = P(st, "p3B", [128, 1024], F32)
        b1 = [P(st, "p3b1_%d" % i, [128, 512], F32) for i in range(2)]
        bh = [P(st, "p3bh_%d" % i, [128, 8, 128], BF16) for i in range(2)]
        banks = [(b1[0][:, :], 'p3b1_0'), (b1[1][:, :], 'p3b1_1'), (bigA[:, 0:512], 'p3A0'), (bigA[:, 512:1024], 'p3A1'),
                 (bigB[:, 0:512], 'p3B'), (bigB[:, 512:1024], 'p3B_hi')]
        rr = {'bk': 0, 'bh': 0, 'xg': 0, 'gl': 0, 'yo': 0, 'ds': 0}

        S.memset('pool', onesf[:], 1.0, ['onesf'])
        buv = G['b_up'].rearrange("e (c p) -> (e c) p", p=128)
        S.dma('sp', bul[:], buv.rearrange("(t r) p -> r t p", r=128), (), ['bul'])
        for t in range(4):
            S.tr(bigB[:, t * 128:(t + 1) * 128], bul[:, t, :], identf[:], ['bul', 'identf'], ['p3B'])
        S.copy('act', buT[:], bigB[:, 0:512], ['p3B'], ['buT'])

        def load_weights(e):
            b = e % 2
            wv = G['w_up'][e].rearrange("(k p) n -> p k n", p=128)
            for hk in range(2):
                S.dma('pool', wu[b][:, hk * 4:(hk + 1) * 4, :], wv[:, hk * 4:(hk + 1) * 4, :], (), [('wu', b, hk)])
            S.dma('pool', wd[b][:], G['w_down'][e].rearrange("(k p) n -> p k n", p=128), (), [('wd', b)])
            S.dma('sp', bd[b][:], G['b_down'][e:e + 1, :], (), [('bd', b)])

        def load_slots(e):
            t4 = e % 4
            S.dma('sp', slot_t[t4][:], G['slots'][e * C:(e + 1) * C, :].rearrange("(n p) f -> p n f", p=128), (), [('slot_t', t4)])
            S.copy('dve', srci[t4][:], slot_t[t4][:, :, 0], [('slot_t', t4)], [('srci', t4)])
            S.ts('dve', dstf[t4 % 2][:, 0, :], slot_t[t4][:, :, 1], 2.0, None, ALU.mult, None, [('slot_t', t4)], [('dstf', t4 % 2)])
            S.ts('dve', dstf[t4 % 2][:, 1, :], slot_t[t4][:, :, 1], 2.0, 1.0, ALU.mult, ALU.add, [('slot_t', t4)], [('dstf', t4 % 2)])
            S.copy('dve', dsti[t4][:], dstf[t4 % 2][:], [('dstf', t4 % 2)], [('dsti', t4)])

        def gathers(e):
            t4 = e % 4
            for blk in range(NB):
                S.op('pool', 'indirect_dma_start', [('srci', t4)], [('xg', blk)], dma=True,
                     out=xg[blk][:, :], out_offset=None, in_=G['s_h1b'][:, :],
                     in_offset=IOA(ap=srci[t4][:, blk:blk + 1], axis=0))

        def xposes(e):
            xb = e % 2
            for blk in range(NB):
                hi = rr['bh'] % 2; rr['bh'] += 1
                for k in range(8):
                    S.tr(bh[hi][:, k, :], xg[blk][:, k * 128:(k + 1) * 128], identb[:], [('xg', blk), 'identb'], ['p3bh_%d' % hi])
                S.copy('act', xgT[xb][:, :, blk * 128:(blk + 1) * 128], bh[hi][:], ['p3bh_%d' % hi], [('xgT', xb, blk)])

        load_weights(0)
        load_slots(0)
        if N_EXP > 1:
            load_slots(1)
        gathers(0)
        xposes(0)
        for e in range(N_EXP):
            b = e % 2
            t4 = e % 4
            xb = e % 2
            if e + 1 < N_EXP:
                load_weights(e + 1)
            if e + 2 < N_EXP:
                load_slots(e + 2)
            if e + 1 < N_EXP:
                gathers(e + 1)
            xgr = [('xgT', xb, blk) for blk in range(NB)]
            for fc in range(8):
                for ch in range(NCH):
                    cs = slice(ch * CW, (ch + 1) * CW)
                    pg, pgn = banks[rr['bk'] % 6]; rr['bk'] += 1
                    plb, pln = banks[rr['bk'] % 6]; rr['bk'] += 1
                    for k in range(8):
                        S.mm(pg[:, 0:CW], wu[b][:, k, fc * 128:(fc + 1) * 128], xgT[xb][:, k, cs], k == 0, k == 7,
                             [('wu', b, k // 4)] + xgr, [pgn])
                    for k in range(8):
                        S.mm(plb[:, 0:CW], wu[b][:, k, D_FF + fc * 128:D_FF + (fc + 1) * 128], xgT[xb][:, k, cs], k == 0, k == 7,
                             [('wu', b, k // 4)] + xgr, [pln])
                    i = rr['gl'] % NGL; rr['gl'] += 1
                    gn_, sn_, ln_ = 'g_%d' % i, 's_%d' % i, 'l_%d' % i
                    S.ts('dve', g_[i][:], pg[:, 0:CW], buT[:, e * 16 + fc:e * 16 + fc + 1], 7.0, ALU.add, ALU.min,
                         [pgn, 'buT'], [gn_])
                    S.act(s_[i][:], g_[i][:], AF.Sigmoid, [gn_], [sn_], scale=1.702)
                    S.act(l_[i][:], plb[:, 0:CW], AF.Identity, [pln, 'buT'], [ln_],
                          bias=buT[:, e * 16 + 8 + fc:e * 16 + 8 + fc + 1], scale=1.0)
                    S.ts('dve', l_[i][:], l_[i][:], -7.0, 7.0, ALU.max, ALU.min, [ln_], [ln_])
                    S.tt('pool', g_[i][:], g_[i][:], s_[i][:], ALU.mult, [gn_, sn_], [gn_])
                    S.stt('dve', aT[:, fc, cs], l_[i][:], 1.0, g_[i][:], ALU.add, ALU.mult, [ln_, gn_], [('aT', fc, ch)])
            if e + 1 < N_EXP:
                xposes(e + 1)
            atr = [('aT', fc, ch) for fc in range(8) for ch in range(NCH)]
            ykh = G['yk'].rearrange("r (two f) -> (r two) f", two=2)
            dsets = [((bigB[:, 0:512], 'p3B'), (bigB[:, 512:1024], 'p3B_hi')),
                     ((bigA[:, 0:512], 'p3A0'), (bigA[:, 512:1024], 'p3A1')),
                     ((b1[0][:, :], 'p3b1_0'), (b1[1][:, :], 'p3b1_1'))]
            for blk in range(NB):
                dset = dsets[rr['ds'] % 3]; rr['ds'] += 1
                i = rr['yo'] % 3; rr['yo'] += 1
                for cb in range(2):
                    osl, osn = dset[cb]
                    for k in range(8):
                        S.mm(osl, aT[:, k, blk * 128:(blk + 1) * 128], wd[b][:, k, cb * 512:(cb + 1) * 512], k == 0, False,
                             atr + [('wd', b)], [osn])
                    S.mm(osl, onesf[0:1, :], bd[b][0:1, cb * 512:(cb + 1) * 512], False, True, ['onesf', ('bd', b)], [osn])
                for cb in range(2):
                    osl, osn = dset[cb]
                    S.act(yo[i][:, cb * 512:(cb + 1) * 512], osl, AF.Copy, [osn, ('slot_t', t4)], [('yo', i, cb)],
                          scale=slot_t[t4][:, blk, 2:3])
                for hc in range(2):
                    S.op('pool', 'indirect_dma_start', [('yo', i, hc), ('dsti', t4)], [('yk', e, blk, hc)], dma=True,
                         out=ykh[:, :], out_offset=IOA(ap=dsti[t4][:, hc, blk:blk + 1], axis=0),
                         in_=yo[i][:, hc * 512:(hc + 1) * 512], in_offset=None)
            S.flush()
        S.barrier()
        S.flush()


def phase4(nc, S, T, P, cfg, tiles, G):
    NPS, SEQ, NSS = cfg['NPS'], cfg['SEQ'], cfg['NSS']
    NT = len(tiles)
    with contextlib.ExitStack() as st:
        l2g = T(st, "l2g", [128, D], F32)
        l2b = T(st, "l2b", [128, D], F32)
        ykt = [T(st, "ykt%d" % i, [128, 4, D], F32) for i in range(3)]
        h1t = [T(st, "h1t%d" % i, [128, D], F32) for i in range(3)]
        sa_l = [T(st, "sa%d" % i, [128, D], F32) for i in range(2)]
        sb_l = [T(st, "sb_%d" % i, [128, D], F32) for i in range(2)]
        pre2_l = [T(st, "pre2%d" % i, [128, D], F32) for i in range(2)]
        junk4_l = [T(st, "junk4%d" % i, [128, D], F32) for i in range(2)]
        st4_l = [T(st, "st4%d" % i, [128, 32], F32) for i in range(2)]
        yout = [T(st, "yout%d" % i, [128, D], F32) for i in range(2)]
        S.dma('sp', l2g[:], G['ln2_g'].partition_broadcast(128)[:, 0, :], (), ['l2g'])
        S.dma('sp', l2b[:], G['ln2_b'].partition_broadcast(128)[:, 0, :], (), ['l2b'])
        ykv = G['yk'][0:G['NTOK'] * 4, :].rearrange("(t k) f -> t k f", k=4)

        def load(ti):
            b = ti % 3
            S.dma('sp', ykt[b][:, 0:2, :], ykv[ti * 128:(ti + 1) * 128, 0:2, :], (), [('ykt', b, 0)])
            S.dma('act', ykt[b][:, 2:4, :], ykv[ti * 128:(ti + 1) * 128, 2:4, :], (), [('ykt', b, 1)])
            S.dma('sp', h1t[b][:], G['s_h1'][ti * 128:(ti + 1) * 128, :], (), ['h1t%d' % b])

        load(0)
        if NT > 1:
            load(1)
        for ti in range(NT):
            kind, a, j = tiles[ti]
            b = ti % 3
            if ti + 2 < NT:
                load(ti + 2)
            yb = ti % 2
            sa, sb_, pre2, junk4, st4 = sa_l[yb], sb_l[yb], pre2_l[yb], junk4_l[yb], st4_l[yb]
            san, sbn, pn, jn, stn4 = 'sa%d' % yb, 'sb_%d' % yb, 'pre2%d' % yb, 'junk4%d' % yb, 'st4%d' % yb
            S.tt('dve', sa[:], ykt[b][:, 0, :], ykt[b][:, 1, :], ALU.add, [('ykt', b, 0)], [san])
            S.tt('dve', sb_[:], ykt[b][:, 2, :], ykt[b][:, 3, :], ALU.add, [('ykt', b, 1)], [sbn])
            S.tt('dve', sa[:], sa[:], sb_[:], ALU.add, [san, sbn], [san])
            S.stt('dve', pre2[:], h1t[b][:], float(ALPHA), sa[:], ALU.mult, ALU.add, ['h1t%d' % b, san], [pn])
            ln_apply(S, 128, pre2, pn, junk4, jn, st4, stn4, l2g, 'l2g', l2b, 'l2b', yout[yb], 'yout%d' % yb)
            if kind == 'p':
                S.dma('act', G['y_p'][a, j * 128:(j + 1) * 128, :], yout[yb][:], ['yout%d' % yb])
            else:
                for hh in range(2):
                    S.dma('act', G['y_s'][2 * a + hh], yout[yb][hh * 64:(hh + 1) * 64, :], ['yout%d' % yb])
        S.barrier()
        S.flush()


def prep_inputs(inputs, cfg, ncores):
    NPS, SEQ, NSS = cfg['NPS'], cfg['SEQ'], cfg['NSS']
    f = lambda a: np.ascontiguousarray(np.asarray(a, dtype=np.float32))
    consts = make_consts(SEQ)
    cp = col_perm()
    w_in = f(inputs['w_in'][0][:, cp])
    hperm = np.concatenate([np.arange(h * 64, (h + 1) * 64) for h in HEAD_ORDER])
    w_dsa_o = f(inputs['w_dsa_o'][0])
    ff = np.concatenate([np.arange(0, 2 * D_FF, 2), np.arange(1, 2 * D_FF, 2)])
    w_up = f(inputs['w_up'][0][:, :, ff])
    b_up = f(inputs['b_up'][0][:, ff])
    shared = dict(
        w_in=w_in, w_ret_o=f(inputs['w_ret_o'][0]), w_dsa_o=w_dsa_o, w_o=f(inputs['w_o'][0]),
        ln1_g=f(inputs['ln1_g']), ln1_b=f(inputs['ln1_b']), ln2_g=f(inputs['ln2_g']), ln2_b=f(inputs['ln2_b']),
        router_w=f(inputs['router_w'][0]), router_b=f(inputs['router_b']),
        w_up=w_up, b_up=b_up, w_down=f(inputs['w_down'][0]), b_down=f(inputs['b_down'][0]),
        c_rotp=consts['rotp'], c_rots=consts['rots'], c_dec128=consts['dec128'], c_dec64=consts['dec64'],
        c_xi128=consts['xi128'], c_xi64=consts['xi64'], c_zp=consts['zp'], c_zs=consts['zs'],
    )
    if cfg.get('phases', 4) < 3:
        for k in ('w_up', 'b_up', 'w_down', 'b_down'):
            shared[k] = np.ascontiguousarray(shared[k][0:1])
    maps = []
    for c in range(ncores):
        m = dict(shared)
        m['xp'] = f(inputs['x_prompt'][c * NPS:(c + 1) * NPS])
        m['xs'] = f(inputs['x_sample'][c * NSS:(c + 1) * NSS])
        m['state_ret'] = f(inputs['state_ret'][0][c * NSS:(c + 1) * NSS])
        m['cache_k'] = f(inputs['cache_k'][0][c * NSS:(c + 1) * NSS]).reshape(NSS, 1024, 128)
        m['cache_v'] = f(inputs['cache_v'][0][c * NSS:(c + 1) * NSS]).reshape(NSS, 1024, 128)
        m['cache_ik'] = f(inputs['cache_idx_k'][0][c * NSS:(c + 1) * NSS])
        maps.append(m)
    return maps


def assemble(results, cfg, ncores):
    NPS, SEQ, NSS = cfg['NPS'], cfg['SEQ'], cfg['NSS']
    cat = lambda k: np.concatenate([np.asarray(r[k]) for r in results], axis=0)
    y_p = cat('y_p'); y_s = cat('y_s')
    rs_p = cat('rs_p')[None]; rs_s = cat('rs_s')[None]
    k_p = cat('k_p').reshape(ncores * NPS, SEQ, 2, 64)[None]
    v_p = cat('v_p').reshape(ncores * NPS, SEQ, 2, 64)[None]
    ik_p = cat('ik_p')[None]
    k_s = cat('k_s').reshape(ncores * NSS, 64, 2, 64)[None]
    v_s = cat('v_s').reshape(ncores * NSS, 64, 2, 64)[None]
    ik_s = cat('ik_s')[None]
    return (y_p, y_s, rs_p, k_p, v_p, ik_p, rs_s, k_s, v_s, ik_s)


def run(inputs, cfg, ncores):
    nc = build(cfg)
    maps = prep_inputs(inputs, cfg, ncores)
    res = run_bass_kernel_spmd(nc, maps, core_ids=list(range(ncores)))
    return res


def kernel(**inputs):
    cfg = dict(NPS=2, SEQ=2048, NSS=4, C=768)
    res = run(inputs, cfg, 8)
    outs = assemble(res.results, cfg, 8)
    return tuple(np.ascontiguousarray(o.astype(np.float32)) for o in outs)
```
